# Optimizing a Trainium2 kernel written in Bass

```python
import math
import jax, jax.numpy as jnp
from jax import lax
import numpy as np

D_MODEL = 2048
BATCH = 8
SEQ = 2048
DEPTH = 2

N_MIXERS = 2
N_ATTN_LAYERS = (DEPTH + 1) // 2
N_SSM_LAYERS = DEPTH // 2

N_Q_HEADS = 32
N_KV_HEADS = 4
HEAD_DIM = 64
Q_PER_KV = N_Q_HEADS // N_KV_HEADS
Q_DIM = N_Q_HEADS * HEAD_DIM
KV_DIM = N_KV_HEADS * HEAD_DIM
QKV_DIM = Q_DIM + 2 * KV_DIM
WINDOW = 128
BLOCK_Q = WINDOW
NUM_BUCKETS = 32
MAX_DISTANCE = 128

SSM_WIDTH = D_MODEL
SSM_GROUP_CH = 16
SSM_GROUPS = SSM_WIDTH // SSM_GROUP_CH
SSM_STATE = 64
DT_MIN = 0.001
DT_MAX = 0.1

N_EXPERTS = 32
TOP_K = 4
D_EXPERT = D_MODEL
SWIGLU_ALPHA = 1.702
SWIGLU_LIMIT = 7.0
MOE_ROW_BLOCK = 256

NORM_EPS = 1e-5

kernel_name = 'hybrid_swa_s5_moe_adaln'


def _rms_norm(x, gain):
    x32 = x.astype(jnp.float32)
    y = x32 * lax.rsqrt(jnp.mean(x32 * x32, axis=-1, keepdims=True) + NORM_EPS)
    return (y * gain.astype(jnp.float32)).astype(x.dtype)


def _t5_bucket(dist):
    n = np.maximum(dist, 0)
    max_exact = NUM_BUCKETS // 2
    large = max_exact + (np.log(np.maximum(n, 1) / max_exact) / np.log(MAX_DISTANCE / max_exact)
                         * (NUM_BUCKETS - max_exact)).astype(np.int32)
    large = np.minimum(large, NUM_BUCKETS - 1)
    return np.where(n < max_exact, n, large).astype(np.int32)


def _band_bias_and_mask(n_blocks, rel_bias):
    ql = np.arange(BLOCK_Q)[:, None]
    kl = np.arange(2 * BLOCK_Q)[None, :]
    dist = ql + BLOCK_Q - kl
    k_abs = np.arange(n_blocks)[:, None, None] * BLOCK_Q - BLOCK_Q + kl[None]
    valid = (dist >= 0)[None] & (dist < WINDOW)[None] & (k_abs >= 0)
    bias = jnp.take(rel_bias.astype(jnp.float32), jnp.asarray(_t5_bucket(dist)), axis=0)
    bias = jnp.transpose(bias, (2, 0, 1)).reshape(N_KV_HEADS, Q_PER_KV, BLOCK_Q, 2 * BLOCK_Q)
    return bias, jnp.asarray(valid)


def _sliding_window_attention(h, w_qkv, b_qkv, q_gain, k_gain, sinks, w_o, b_o, rel_bias):
    bsz, seq, _ = h.shape
    nblk = seq // BLOCK_Q
    qkv = h @ w_qkv + b_qkv
    q = qkv[..., :Q_DIM].reshape(bsz, seq, N_KV_HEADS, Q_PER_KV, HEAD_DIM)
    k = qkv[..., Q_DIM:Q_DIM + KV_DIM].reshape(bsz, seq, N_KV_HEADS, HEAD_DIM)
    v = qkv[..., Q_DIM + KV_DIM:].reshape(bsz, seq, N_KV_HEADS, HEAD_DIM)
    q = _rms_norm(q, q_gain)
    k = _rms_norm(k, k_gain)
    pad = jnp.zeros((bsz, BLOCK_Q, N_KV_HEADS, HEAD_DIM), k.dtype)

    def band(t):
        prev = jnp.concatenate([pad, t[:, :-BLOCK_Q]], axis=1).reshape(bsz, nblk, BLOCK_Q, N_KV_HEADS, HEAD_DIM)
        cur = t.reshape(bsz, nblk, BLOCK_Q, N_KV_HEADS, HEAD_DIM)
        return jnp.concatenate([prev, cur], axis=2)

    kb, vb = band(k), band(v)
    qb = q.reshape(bsz, nblk, BLOCK_Q, N_KV_HEADS, Q_PER_KV, HEAD_DIM)
    bias, valid = _band_bias_and_mask(nblk, rel_bias)
    logits = jnp.einsum('bnqhgd,bnkhd->bnhgqk', qb, kb).astype(jnp.float32) * (1.0 / math.sqrt(HEAD_DIM))
    logits = jnp.where(valid[None, :, None, None], logits + bias[None, None], -jnp.inf)
    sink = sinks.astype(jnp.float32).reshape(N_KV_HEADS, Q_PER_KV, 1, 1)
    m = jnp.maximum(jnp.max(logits, axis=-1, keepdims=True), sink)
    p = jnp.exp(logits - m)
    probs = (p / (jnp.sum(p, axis=-1, keepdims=True) + jnp.exp(sink - m))).astype(v.dtype)
    o = jnp.einsum('bnhgqk,bnkhd->bnqhgd', probs, vb).reshape(bsz, seq, Q_DIM)
    return o @ w_o + b_o


def _s5_mixer(h, lam_re, lam_im, log_dt, b_re, b_im, c_re, c_im, d_skip, w_glu_a, b_glu_a, w_glu_b, b_glu_b):
    bsz, seq, _ = h.shape
    f32 = jnp.float32
    u = h.astype(f32).reshape(bsz, seq, SSM_GROUPS, SSM_GROUP_CH)
    lr, li = lam_re.astype(f32), lam_im.astype(f32)
    dt = jnp.exp(log_dt.astype(f32))[:, None]
    mag = jnp.exp(lr * dt)
    ab_re, ab_im = mag * jnp.cos(li * dt), mag * jnp.sin(li * dt)
    den = lr * lr + li * li
    nr, ni = ab_re - 1.0, ab_im
    f_re = (nr * lr + ni * li) / den
    f_im = (ni * lr - nr * li) / den
    br, bi = b_re.astype(f32), b_im.astype(f32)
    bb_re = f_re[..., None] * br - f_im[..., None] * bi
    bb_im = f_re[..., None] * bi + f_im[..., None] * br
    x_re = jnp.einsum('bsgc,gpc->bsgp', u, bb_re)
    x_im = jnp.einsum('bsgc,gpc->bsgp', u, bb_im)
    a_re = jnp.broadcast_to(ab_re[None, None], (1, seq, SSM_GROUPS, SSM_STATE))
    a_im = jnp.broadcast_to(ab_im[None, None], (1, seq, SSM_GROUPS, SSM_STATE))

    def combine(e1, e2):
        a1r, a1i, b1r, b1i = e1
        a2r, a2i, b2r, b2i = e2
        return (a2r * a1r - a2i * a1i, a2r * a1i + a2i * a1r,
                a2r * b1r - a2i * b1i + b2r, a2r * b1i + a2i * b1r + b2i)

    _, _, s_re, s_im = lax.associative_scan(combine, (a_re, a_im, x_re, x_im), axis=1)
    y = (jnp.einsum('bsgp,gcp->bsgc', s_re, c_re.astype(f32))
         - jnp.einsum('bsgp,gcp->bsgc', s_im, c_im.astype(f32))
         + d_skip.astype(f32).reshape(SSM_GROUPS, SSM_GROUP_CH) * u)
    y = jax.nn.gelu(y.reshape(bsz, seq, SSM_WIDTH)).astype(h.dtype)
    return (y @ w_glu_a + b_glu_a) * jax.nn.sigmoid(y @ w_glu_b + b_glu_b)


def _clamped_swiglu(gu):
    glu = jnp.minimum(gu[..., :D_EXPERT], SWIGLU_LIMIT)
    lin = jnp.clip(gu[..., D_EXPERT:], -SWIGLU_LIMIT, SWIGLU_LIMIT)
    return glu * jax.nn.sigmoid(SWIGLU_ALPHA * glu) * (lin + 1.0)


def _moe(h, layer, w_router, b_router, w_gate_up, b_gate_up, w_down, b_down):
    bsz, seq, dm = h.shape
    n_tok = bsz * seq
    n_assign = n_tok * TOP_K
    hf = h.reshape(n_tok, dm)
    logits = (hf @ w_router[layer] + b_router[layer]).astype(jnp.float32)
    top_logit, top_idx = lax.top_k(logits, TOP_K)
    gates = jax.nn.softmax(top_logit, axis=-1)
    e_flat = top_idx.reshape(n_assign).astype(jnp.int32)
    tok_flat = jnp.broadcast_to(jnp.arange(n_tok, dtype=jnp.int32)[:, None], (n_tok, TOP_K)).reshape(n_assign)
    g_flat = gates.reshape(n_assign)
    order = jnp.argsort(e_flat)
    e_sorted = e_flat[order]
    counts = jnp.bincount(e_flat, length=N_EXPERTS).astype(jnp.int32)
    padded = (counts + MOE_ROW_BLOCK - 1) // MOE_ROW_BLOCK * MOE_ROW_BLOCK
    pad_end = jnp.cumsum(padded)
    pad_start = pad_end - padded
    grp_start = jnp.cumsum(counts) - counts
    dest = pad_start[e_sorted] + jnp.arange(n_assign, dtype=jnp.int32) - grp_start[e_sorted]
    n_blocks = (n_assign + N_EXPERTS * (MOE_ROW_BLOCK - 1)) // MOE_ROW_BLOCK
    n_rows = n_blocks * MOE_ROW_BLOCK
    row_tok = jnp.full((n_rows,), n_tok, jnp.int32).at[dest].set(tok_flat[order])
    row_gate = jnp.zeros((n_rows,), jnp.float32).at[dest].set(g_flat[order])
    blk_expert = jnp.minimum(jnp.searchsorted(pad_end, jnp.arange(n_blocks, dtype=jnp.int32) * MOE_ROW_BLOCK,
                                              side='right'), N_EXPERTS - 1).astype(jnp.int32)
    h_pad = jnp.concatenate([hf, jnp.zeros((1, dm), hf.dtype)], axis=0)

    def expert_block(acc, blk):
        rows, g, e = blk
        xb = h_pad[rows]
        gu = xb @ w_gate_up[layer, e] + b_gate_up[layer, e]
        y = _clamped_swiglu(gu) @ w_down[layer, e] + b_down[layer, e]
        return acc.at[rows].add(y.astype(jnp.float32) * g[:, None]), None

    acc, _ = lax.scan(expert_block, jnp.zeros((n_tok + 1, dm), jnp.float32),
                      (row_tok.reshape(n_blocks, MOE_ROW_BLOCK), row_gate.reshape(n_blocks, MOE_ROW_BLOCK), blk_expert))
    return acc[:n_tok].reshape(bsz, seq, dm).astype(h.dtype)


def setup_inputs(seed: int = 0) -> dict:
    key = jax.random.key(seed)
    ks = iter(jax.random.split(key, 40))

    def nrm(shape, std):
        return jax.random.normal(next(ks), shape, jnp.float32) * std

    D = D_MODEL
    NA, NS = N_ATTN_LAYERS, N_SSM_LAYERS
    n_idx = jnp.arange(SSM_STATE, dtype=jnp.float32)
    return {
        'x': nrm((BATCH, SEQ, D), 1.0),
        'c': nrm((BATCH, D), 1.0),
        'rel_bias': nrm((NUM_BUCKETS, N_Q_HEADS), 0.5),
        'norm_gain': 1.0 + nrm((DEPTH, 2, D), 0.02),
        'ada_w': nrm((DEPTH, D, 6 * D), 0.5 * D ** -0.5),
        'ada_b': nrm((DEPTH, 6 * D), 0.02),
        'attn_w_qkv': nrm((NA, D, QKV_DIM), D ** -0.5),
        'attn_b_qkv': nrm((NA, QKV_DIM), 0.02),
        'attn_q_gain': 1.0 + nrm((NA, HEAD_DIM), 0.02),
        'attn_k_gain': 1.0 + nrm((NA, HEAD_DIM), 0.02),
        'attn_sinks': nrm((NA, N_Q_HEADS), 1.0),
        'attn_w_o': nrm((NA, Q_DIM, D), Q_DIM ** -0.5),
        'attn_b_o': nrm((NA, D), 0.02),
        'ssm_lam_re': -0.5 * jnp.exp(nrm((NS, SSM_GROUPS, SSM_STATE), 0.05)),
        'ssm_lam_im': math.pi * n_idx + nrm((NS, SSM_GROUPS, SSM_STATE), 0.01),
        'ssm_log_dt': jax.random.uniform(next(ks), (NS, SSM_GROUPS), jnp.float32,
                                         minval=math.log(DT_MIN), maxval=math.log(DT_MAX)),
        'ssm_b_re': nrm((NS, SSM_GROUPS, SSM_STATE, SSM_GROUP_CH), (2 * SSM_GROUP_CH) ** -0.5),
        'ssm_b_im': nrm((NS, SSM_GROUPS, SSM_STATE, SSM_GROUP_CH), (2 * SSM_GROUP_CH) ** -0.5),
        'ssm_c_re': nrm((NS, SSM_GROUPS, SSM_GROUP_CH, SSM_STATE), 2.0 * SSM_STATE ** -0.5),
        'ssm_c_im': nrm((NS, SSM_GROUPS, SSM_GROUP_CH, SSM_STATE), 2.0 * SSM_STATE ** -0.5),
        'ssm_d': nrm((NS, SSM_WIDTH), 1.0),
        'ssm_w_glu_a': nrm((NS, SSM_WIDTH, D), SSM_WIDTH ** -0.5),
        'ssm_b_glu_a': nrm((NS, D), 0.02),
        'ssm_w_glu_b': nrm((NS, SSM_WIDTH, D), SSM_WIDTH ** -0.5),
        'ssm_b_glu_b': nrm((NS, D), 0.02),
        'moe_w_router': nrm((DEPTH, D, N_EXPERTS), D ** -0.5),
        'moe_b_router': nrm((DEPTH, N_EXPERTS), 0.01),
        'moe_w_gate_up': nrm((DEPTH, N_EXPERTS, D, 2 * D_EXPERT), D ** -0.5),
        'moe_b_gate_up': nrm((DEPTH, N_EXPERTS, 2 * D_EXPERT), 0.02),
        'moe_w_down': nrm((DEPTH, N_EXPERTS, D_EXPERT, D), D_EXPERT ** -0.5),
        'moe_b_down': nrm((DEPTH, N_EXPERTS, D), 0.02),
    }


def reference(x, c, rel_bias, norm_gain, ada_w, ada_b,
              attn_w_qkv, attn_b_qkv, attn_q_gain, attn_k_gain, attn_sinks, attn_w_o, attn_b_o,
              ssm_lam_re, ssm_lam_im, ssm_log_dt, ssm_b_re, ssm_b_im, ssm_c_re, ssm_c_im, ssm_d,
              ssm_w_glu_a, ssm_b_glu_a, ssm_w_glu_b, ssm_b_glu_b,
              moe_w_router, moe_b_router, moe_w_gate_up, moe_b_gate_up, moe_w_down, moe_b_down):
    cond = jax.nn.silu(c)
    for layer in range(DEPTH):
        mod = cond @ ada_w[layer] + ada_b[layer]
        sh1, sc1, g1, sh2, sc2, g2 = [m[:, None, :] for m in jnp.split(mod, 6, axis=-1)]
        h = _rms_norm(x, norm_gain[layer, 0]) * (1.0 + sc1) + sh1
        i = layer // N_MIXERS
        if layer % N_MIXERS == 0:
            mix = _sliding_window_attention(h, attn_w_qkv[i], attn_b_qkv[i], attn_q_gain[i], attn_k_gain[i],
                                            attn_sinks[i], attn_w_o[i], attn_b_o[i], rel_bias)
        else:
            mix = _s5_mixer(h, ssm_lam_re[i], ssm_lam_im[i], ssm_log_dt[i], ssm_b_re[i], ssm_b_im[i],
                            ssm_c_re[i], ssm_c_im[i], ssm_d[i], ssm_w_glu_a[i], ssm_b_glu_a[i],
                            ssm_w_glu_b[i], ssm_b_glu_b[i])
        x = x + g1 * mix
        h = _rms_norm(x, norm_gain[layer, 1]) * (1.0 + sc2) + sh2
        x = x + g2 * _moe(h, layer, moe_w_router, moe_b_router, moe_w_gate_up, moe_b_gate_up, moe_w_down, moe_b_down)
    return x
```

```python
import math
import numpy as np
import concourse.bass as bass
import concourse.mybir as mybir
from concourse.bass_utils import run_bass_kernel_spmd

F32 = mybir.dt.float32
F32R = mybir.dt.float32r
I32 = mybir.dt.int32
U32 = mybir.dt.uint32
ALU = mybir.AluOpType
AF = mybir.ActivationFunctionType
AX = mybir.AxisListType

ENGS = ("pe", "act", "dve", "pool", "sp")
SAME_ENGINE_SYNC = {"pe": False, "act": True, "dve": True, "pool": True, "sp": False}


class Prog:
    def __init__(self, nc):
        self.nc = nc
        self.items = {e: [] for e in ENGS}
        self.count = {e: 0 for e in ENGS}
        self.sem = {}
        self.waited = {e: {} for e in ENGS}
        self.last_write = {}
        self.reads_since = {}
        self.dma_count = {}
        self.ctx = []
        for e in ENGS:
            self.sem[e] = self._newsem("eng_" + e)

    def _newsem(self, name):
        cm = self.nc.semaphore(name)
        s = cm.__enter__()
        self.ctx.append(cm)
        return s

    def sbuf(self, name, shape, dt):
        cm = self.nc.sbuf_tensor(getattr(self, 'prefix', '') + name, list(shape), dt)
        t = cm.__enter__()
        self.ctx.append(cm)
        return t

    def psum(self, name, shape, dt):
        cm = self.nc.psum_tensor(getattr(self, 'prefix', '') + name, list(shape), dt)
        t = cm.__enter__()
        self.ctx.append(cm)
        return t

    def _deps(self, reads, writes):
        deps = {}
        def add(k, v):
            if deps.get(k, 0) < v:
                deps[k] = v
        for r in reads:
            if r in self.last_write:
                add(*self.last_write[r])
        for w in writes:
            if w in self.last_write:
                add(*self.last_write[w])
            for k, v in self.reads_since.get(w, {}).items():
                add(k, v)
        return deps

    def _emit_waits(self, eng, deps):
        for k, v in deps.items():
            if k == eng and not SAME_ENGINE_SYNC[eng]:
                continue
            if self.waited[eng].get(k, 0) >= v:
                continue
            self.waited[eng][k] = v
            self.items[eng].append(("wait", self.sem[k], v))

    def _record(self, key, val, reads, writes):
        for r in reads:
            self.reads_since.setdefault(r, {})
            if self.reads_since[r].get(key, 0) < val:
                self.reads_since[r][key] = val
        for w in writes:
            self.last_write[w] = (key, val)
            self.reads_since[w] = {}

    def op(self, eng, fn, reads=(), writes=()):
        deps = self._deps(reads, writes)
        self._emit_waits(eng, deps)
        self.count[eng] += 1
        self.items[eng].append(("op", fn, self.sem[eng], 1))
        self._record(eng, self.count[eng], reads, writes)

    def dma(self, eng, chan, fn, reads=(), writes=()):
        key = "dma_" + chan
        if key not in self.sem:
            self.sem[key] = self._newsem(key)
            self.dma_count[key] = 0
        deps = self._deps(reads, writes)
        self._emit_waits(eng, deps)
        self.dma_count[key] += 16
        self.items[eng].append(("op", fn, self.sem[key], 16))
        self._record(key, self.dma_count[key], reads, writes)

    def raw(self, eng, fn, reads=(), writes=()):
        self.op(eng, fn, reads, writes)

    def barrier_all(self, engs=ENGS):
        allk = {}
        for e in ENGS:
            if self.count[e]:
                allk[e] = self.count[e]
        for k, v in self.dma_count.items():
            if v:
                allk[k] = v
        for e in engs:
            self._emit_waits(e, dict(allk))

    def emit(self):
        nc = self.nc
        items = self.items
        with nc.Block() as block:
            def run(engine, lst):
                for it in lst:
                    if it[0] == "wait":
                        engine.wait_ge(it[1], it[2])
                    else:
                        ins = it[1](engine)
                        ins.then_inc(it[2], it[3])

            @block.tensor
            def _(e):
                run(e, items["pe"])

            @block.scalar
            def _(e):
                run(e, items["act"])

            @block.vector
            def _(e):
                run(e, items["dve"])

            @block.gpsimd
            def _(e):
                run(e, items["pool"])

            @block.sync
            def _(e):
                run(e, items["sp"])

    def mark(self):
        return len(self.ctx)

    def release(self, m):
        while len(self.ctx) > m:
            self.ctx.pop().__exit__(None, None, None)

    def close(self):
        for cm in reversed(self.ctx):
            cm.__exit__(None, None, None)
        self.ctx = []

BF16 = mybir.dt.bfloat16

D = 2048
S = 2048
NT = 16
QKV = 2560


def emit_adaln(P, nc, c_ap, ada_w, ada_b, modrow):
    ident = P.ident
    c16 = P.sbuf("c16", [16, 128], F32)
    condT = P.sbuf("condT", [128, 16], F32)
    pc = P.psum("pc", [128, 16], F32)
    P.dma("sp", "c", lambda e: e.dma_start(out=c16[:], in_=c_ap.rearrange("(k p) -> k p", p=128)), writes=["c16"])
    P.op("pe", lambda e: e.transpose(pc[:], c16[:], ident[:16, :16]), reads=["c16", "ident"], writes=["pc"])
    P.op("act", lambda e: e.activation(out=condT[:], in_=pc[:], func=AF.Silu), reads=["pc"], writes=["condT"])
    wb = [P.sbuf(f"adaw{i}", [128, 16, 512], F32) for i in range(2)]
    pm = [P.psum(f"pm{i}", [1, 512], F32) for i in range(2)]
    bt = [P.sbuf(f"adab{i}", [1, 512], F32) for i in range(2)]
    mt = [P.sbuf(f"mrow{i}", [1, 512], F32) for i in range(2)]
    it = 0
    for l in range(2):
        for cb in range(24):
            b = it % 2
            cs = slice(cb * 512, (cb + 1) * 512)
            P.dma("sp", f"adaw{b}", lambda e, b=b, l=l, cs=cs: e.dma_start(
                out=wb[b][:], in_=ada_w[l, :, cs].rearrange("(k p) n -> p k n", p=128)),
                writes=[f"adaw{b}"])
            P.dma("sp", f"adab{b}", lambda e, b=b, l=l, cs=cs: e.dma_start(out=bt[b][:], in_=ada_b[l:l + 1, cs]), writes=[f"adab{b}"])
            for k in range(16):
                P.op("pe", lambda e, b=b, k=k: e.matmul(pm[b][:], condT[:, k:k + 1], wb[b][:, k, :], start=(k == 0), stop=(k == 15)),
                     reads=["condT", f"adaw{b}"], writes=[f"pm{b}"])
            P.op("dve", lambda e, b=b: e.tensor_tensor(mt[b][:], pm[b][:], bt[b][:], ALU.add),
                 reads=[f"pm{b}", f"adab{b}"], writes=[f"mrow{b}"])
            P.dma("sp", f"mrow{b}", lambda e, b=b, l=l, cs=cs: e.dma_start(out=modrow[l:l + 1, cs], in_=mt[b][:]), reads=[f"mrow{b}"], writes=["d_modrow"])
            it += 1


def emit_attn(P, nc, x, xa, modrow, norm_gain, w_qkv, b_qkv, q_gain, k_gain, sinks, w_o, b_o, biasmask):
    ident = P.ident
    A = P.sbuf("A", [128, D], F32)
    Bt = P.sbuf("Bt", [128, D], F32)
    G1 = P.sbuf("G1", [128, D], F32)
    bq = P.sbuf("bq", [128, QKV], F32)
    bo = P.sbuf("bo", [128, D], F32)
    qg = P.sbuf("qg", [128, 64], F32)
    kg = P.sbuf("kg", [128, 64], F32)
    snk = P.sbuf("snk", [128, 32], F32)
    bm = P.sbuf("bm", [128, 32, 256], F32)
    P.dma("sp", "c0", lambda e: e.dma_start(out=Bt[:], in_=modrow[0, 0:D].partition_broadcast(128)), reads=["d_modrow"], writes=["Bt"])
    P.dma("sp", "c1", lambda e: e.dma_start(out=A[:], in_=modrow[0, D:2 * D].partition_broadcast(128)), reads=["d_modrow"], writes=["A"])
    P.dma("sp", "c2", lambda e: e.dma_start(out=G1[:], in_=modrow[0, 2 * D:3 * D].partition_broadcast(128)), reads=["d_modrow"], writes=["G1"])
    P.dma("sp", "c3", lambda e: e.dma_start(out=bo[:], in_=norm_gain[0, 0, :].partition_broadcast(128)), writes=["bo"])
    P.op("dve", lambda e: e.scalar_tensor_tensor(A[:], A[:], 1.0, bo[:], ALU.add, ALU.mult), reads=["A", "bo"], writes=["A"])
    P.dma("sp", "c3", lambda e: e.dma_start(out=bo[:], in_=b_o[0, :].partition_broadcast(128)), reads=["bo"], writes=["bo"])
    P.dma("sp", "c4", lambda e: e.dma_start(out=bq[:], in_=b_qkv[0, :].partition_broadcast(128)), writes=["bq"])
    P.dma("sp", "c5", lambda e: e.dma_start(out=qg[:], in_=q_gain[0, :].partition_broadcast(128)), writes=["qg"])
    P.dma("sp", "c6", lambda e: e.dma_start(out=kg[:], in_=k_gain[0, :].partition_broadcast(128)), writes=["kg"])
    P.dma("sp", "c7", lambda e: e.dma_start(out=snk[:], in_=sinks[0, :].partition_broadcast(128)), writes=["snk"])
    P.dma("sp", "c8", lambda e: e.dma_start(out=bm[:], in_=biasmask.rearrange("h q k -> q h k")), writes=["bm"])
    P.op("dve", lambda e: e.tensor_scalar(qg[:], qg[:], 0.125, None, ALU.mult), reads=["qg"], writes=["qg"])

    xt = P.sbuf("xt", [128, D], F32)
    ht = P.sbuf("ht", [128, D], F32)
    junk = P.sbuf("junk", [128, QKV], F32)
    hT = P.sbuf("hT", [128, 16, 128], F32)
    ss = P.sbuf("ss", [128, 1], F32)
    rs = P.sbuf("rs", [128, 1], F32)
    qkv = P.sbuf("qkv", [128, QKV], F32)
    sq36 = P.sbuf("sq36", [128, 36], F32)
    kT = P.sbuf("kT", [64, 4, 256], F32)
    vv = P.sbuf("vv", [128, 2, 256], F32)
    qT = P.sbuf("qT", [64, 128], F32)
    pe_ = P.sbuf("pexp", [128, 256], F32)
    pT = P.sbuf("pT", [128, 2, 128], F32)
    mx = P.sbuf("mx", [128, 1], F32)
    nm = P.sbuf("nm", [128, 1], F32)
    sm = P.sbuf("sm", [128, 1], F32)
    es = P.sbuf("es", [128, 1], F32)
    osb = P.sbuf("osb", [128, D], F32)
    wch = [P.sbuf(f"wch{i}", [128, 16, 256], F32) for i in range(2)]
    ptr = P.psum("ptr", [128, 4, 128], F32)
    pmm = [P.psum(f"pmm{i}", [128, 256], F32) for i in range(2)]
    plg = P.psum("plg", [128, 256], F32)
    ppt = P.psum("ppt", [128, 2, 128], F32)
    po = P.psum("po", [128, 64], F32)
    pq = P.psum("pq", [64, 128], F32)

    P.op("dve", lambda e: e.memset(kT[:], 0.0), writes=["kT"])
    P.op("dve", lambda e: e.memset(vv[:], 0.0), writes=["vv"])
    wi = 0
    for t in range(NT):
        rows = slice(t * 128, (t + 1) * 128)
        P.dma("sp", "xt", lambda e, rows=rows: e.dma_start(out=xt[:], in_=x[rows, :]), writes=["xt"])
        P.op("act", lambda e: e.activation(out=junk[:, 0:D], in_=xt[:], func=AF.Square, accum_out=ss[:]), reads=["xt"], writes=["junk", "ss"])
        P.op("dve", lambda e: e.tensor_scalar(rs[:], ss[:], 1.0 / D, 1e-5, ALU.mult, ALU.add), reads=["ss"], writes=["rs"])
        P.op("act", lambda e: e.activation(out=rs[:], in_=rs[:], func=AF.Sqrt), reads=["rs"], writes=["rs"])
        P.op("dve", lambda e: e.reciprocal(rs[:], rs[:]), reads=["rs"], writes=["rs"])
        P.op("dve", lambda e: e.scalar_tensor_tensor(ht[:], xt[:], rs[:], A[:], ALU.mult, ALU.mult), reads=["xt", "rs", "A"], writes=["ht"])
        P.op("dve", lambda e: e.tensor_tensor(ht[:], ht[:], Bt[:], ALU.add), reads=["ht", "Bt"], writes=["ht"])
        for q in range(4):
            for i in range(4):
                k = q * 4 + i
                P.op("pe", lambda e, k=k, i=i: e.transpose(ptr[:, i, :], ht[:, k * 128:(k + 1) * 128], ident[:]), reads=["ht", "ident"], writes=["ptr"])
            P.op("act", lambda e, q=q: e.copy(out=hT[:, q * 4:(q + 1) * 4, :], in_=ptr[:]), reads=["ptr"], writes=["hT"])
        for cb in range(10):
            b = wi % 2
            wi += 1
            P.dma("sp", f"wch{b}", lambda e, b=b, cb=cb: e.dma_start(
                out=wch[b][:], in_=w_qkv[0, :, cb * 256:(cb + 1) * 256].rearrange("(k p) n -> p k n", p=128)), writes=[f"wch{b}"])
            for k in range(16):
                P.op("pe", lambda e, b=b, k=k: e.matmul(pmm[b][:], hT[:, k, :], wch[b][:, k, :], start=(k == 0), stop=(k == 15)),
                     reads=["hT", f"wch{b}"], writes=[f"pmm{b}"])
            P.op("dve", lambda e, b=b, cb=cb: e.tensor_tensor(qkv[:, cb * 256:(cb + 1) * 256], pmm[b][:], bq[:, cb * 256:(cb + 1) * 256], ALU.add),
                 reads=[f"pmm{b}", "bq"], writes=["qkv"])
        P.op("dve", lambda e: e.tensor_tensor(junk[:, 0:2304], qkv[:, 0:2304], qkv[:, 0:2304], ALU.mult), reads=["qkv"], writes=["junk"])
        P.op("dve", lambda e: e.tensor_reduce(sq36[:], junk[:, 0:2304].rearrange("p (h d) -> p h d", d=64), AX.X, ALU.add), reads=["junk"], writes=["sq36"])
        P.op("dve", lambda e: e.tensor_scalar(sq36[:], sq36[:], 1.0 / 64, 1e-5, ALU.mult, ALU.add), reads=["sq36"], writes=["sq36"])
        P.op("act", lambda e: e.activation(out=sq36[:], in_=sq36[:], func=AF.Sqrt), reads=["sq36"], writes=["sq36"])
        P.op("dve", lambda e: e.reciprocal(sq36[:], sq36[:]), reads=["sq36"], writes=["sq36"])
        for h in range(36):
            g = qg if h < 32 else kg
            gname = "qg" if h < 32 else "kg"
            P.op("dve", lambda e, h=h, g=g: e.scalar_tensor_tensor(qkv[:, h * 64:(h + 1) * 64], qkv[:, h * 64:(h + 1) * 64], sq36[:, h:h + 1], g[:], ALU.mult, ALU.mult),
                 reads=["qkv", "sq36", gname], writes=["qkv"])
        P.op("dve", lambda e: e.tensor_copy(kT[:, :, 0:128], kT[:, :, 128:256]), reads=["kT"], writes=["kT"])
        P.op("dve", lambda e: e.tensor_copy(vv[:, 0, :], vv[:, 1, :]), reads=["vv"], writes=["vv"])
        P.op("dve", lambda e: e.tensor_copy(vv[:, 1, :], qkv[:, 2304:2560]), reads=["qkv", "vv"], writes=["vv"])
        for kh in range(4):
            P.op("pe", lambda e, kh=kh: e.transpose(pq[:], qkv[:, 2048 + kh * 64:2048 + (kh + 1) * 64], ident[:]), reads=["qkv", "ident"], writes=["pq"])
            P.op("act", lambda e, kh=kh: e.copy(out=kT[:, kh, 128:256], in_=pq[:]), reads=["pq", "kT"], writes=["kT"])
        for h in range(32):
            kh = h // 8
            P.op("pe", lambda e, h=h: e.transpose(pq[:], qkv[:, h * 64:(h + 1) * 64], ident[:]), reads=["qkv", "ident"], writes=["pq"])
            P.op("act", lambda e: e.copy(out=qT[:], in_=pq[:]), reads=["pq"], writes=["qT"])
            P.op("pe", lambda e, kh=kh: e.matmul(plg[:], qT[:], kT[:, kh, :], start=True, stop=True), reads=["qT", "kT"], writes=["plg"])
            P.op("dve", lambda e, h=h: e.tensor_tensor(pe_[:], plg[:], bm[:, h, :], ALU.add), reads=["plg", "bm"], writes=["pexp"])
            if t == 0:
                P.op("dve", lambda e: e.memset(pe_[:, 0:128], -30000.0), reads=["pexp"], writes=["pexp"])
            P.op("dve", lambda e: e.reduce_max(mx[:], pe_[:], AX.X), reads=["pexp"], writes=["mx"])
            P.op("dve", lambda e, h=h: e.tensor_scalar(nm[:], mx[:], snk[:, h:h + 1], -1.0, ALU.max, ALU.mult), reads=["mx", "snk"], writes=["nm"])
            P.op("act", lambda e: e.activation(out=pe_[:], in_=pe_[:], func=AF.Exp, bias=nm[:], accum_out=sm[:]), reads=["pexp", "nm"], writes=["pexp", "sm"])
            P.op("act", lambda e, h=h: e.activation(out=es[:], in_=snk[:, h:h + 1], func=AF.Exp, bias=nm[:]), reads=["snk", "nm"], writes=["es"])
            P.op("dve", lambda e: e.tensor_tensor(sm[:], sm[:], es[:], ALU.add), reads=["sm", "es"], writes=["sm"])
            P.op("dve", lambda e: e.reciprocal(sm[:], sm[:]), reads=["sm"], writes=["sm"])
            for j in range(2):
                P.op("pe", lambda e, j=j: e.transpose(ppt[:, j, :], pe_[:, j * 128:(j + 1) * 128], ident[:]), reads=["pexp", "ident"], writes=["ppt"])
            P.op("act", lambda e: e.copy(out=pT[:], in_=ppt[:]), reads=["ppt"], writes=["pT"])
            for j in range(2):
                P.op("pe", lambda e, j=j, kh=kh: e.matmul(po[:], pT[:, j, :], vv[:, j, kh * 64:(kh + 1) * 64], start=(j == 0), stop=(j == 1)),
                     reads=["pT", "vv"], writes=["po"])
            P.op("dve", lambda e, h=h: e.tensor_scalar(osb[:, h * 64:(h + 1) * 64], po[:], sm[:], None, ALU.mult), reads=["po", "sm"], writes=["osb"])
        for q in range(4):
            for i in range(4):
                k = q * 4 + i
                P.op("pe", lambda e, k=k, i=i: e.transpose(ptr[:, i, :], osb[:, k * 128:(k + 1) * 128], ident[:]), reads=["osb", "ident"], writes=["ptr"])
            P.op("act", lambda e, q=q: e.copy(out=hT[:, q * 4:(q + 1) * 4, :], in_=ptr[:]), reads=["ptr"], writes=["hT"])
        for cb in range(8):
            b = wi % 2
            wi += 1
            P.dma("sp", f"wch{b}", lambda e, b=b, cb=cb: e.dma_start(
                out=wch[b][:], in_=w_o[0, :, cb * 256:(cb + 1) * 256].rearrange("(k p) n -> p k n", p=128)), writes=[f"wch{b}"])
            for k in range(16):
                P.op("pe", lambda e, b=b, k=k: e.matmul(pmm[b][:], hT[:, k, :], wch[b][:, k, :], start=(k == 0), stop=(k == 15)),
                     reads=["hT", f"wch{b}"], writes=[f"pmm{b}"])
            cs = slice(cb * 256, (cb + 1) * 256)
            P.op("dve", lambda e, b=b, cs=cs: e.tensor_tensor(ht[:, cs], pmm[b][:], bo[:, cs], ALU.add), reads=[f"pmm{b}", "bo"], writes=["ht"])
            P.op("dve", lambda e, cs=cs: e.tensor_tensor(ht[:, cs], ht[:, cs], G1[:, cs], ALU.mult), reads=["ht", "G1"], writes=["ht"])
            P.op("dve", lambda e, cs=cs: e.tensor_tensor(ht[:, cs], ht[:, cs], xt[:, cs], ALU.add), reads=["ht", "xt"], writes=["ht"])
        P.dma("sp", "xa", lambda e, rows=rows: e.dma_start(out=xa[rows, :], in_=ht[:]), reads=["ht"], writes=["d_xa"])


def make_biasmask(rel_bias):
    ql = np.arange(128)[:, None]
    kl = np.arange(256)[None, :]
    dist = ql + 128 - kl
    n = np.maximum(dist, 0)
    max_exact = 16
    large = max_exact + (np.log(np.maximum(n, 1) / max_exact) / np.log(128 / max_exact) * (32 - max_exact)).astype(np.int32)
    large = np.minimum(large, 31)
    bucket = np.where(n < max_exact, n, large).astype(np.int32)
    valid = (dist >= 0) & (dist < 128)
    bm = np.ascontiguousarray(np.transpose(rel_bias[bucket], (2, 0, 1))).astype(np.float32)
    bm[:, ~valid] = -30000.0
    return bm


STOP = 99
NP = 4
TP = 512


def emit_moe(P, nc, l, xin, xout, modrow, norm_gain, w_router, b_router, w_gu, b_gu, w_d, b_d, mode='full', nexp=32):
    ident = P.ident
    A = P.sbuf("mA", [128, D], F32)
    Bt = P.sbuf("mB", [128, D], F32)
    G2 = P.sbuf("mG2", [128, D], F32)
    brt = P.sbuf("brt", [128, 32], F32)
    wr = P.sbuf("wr", [128, 16, 32], F32)
    identb = P.sbuf("identb", [128, 128], BF16)
    ones_b = P.sbuf("ones_b", [1, 128], BF16)
    P.op("dve", lambda e: e.tensor_copy(identb[:], ident[:]), reads=["ident"], writes=["identb"])
    P.op("dve", lambda e: e.memset(ones_b[:], 1.0), writes=["ones_b"])
    P.dma("sp", "m0", lambda e: e.dma_start(out=Bt[:], in_=modrow[l, 3 * D:4 * D].partition_broadcast(128)), reads=["d_modrow"], writes=["mB"])
    P.dma("sp", "m1", lambda e: e.dma_start(out=A[:], in_=modrow[l, 4 * D:5 * D].partition_broadcast(128)), reads=["d_modrow"], writes=["mA"])
    P.dma("sp", "m2", lambda e: e.dma_start(out=G2[:], in_=modrow[l, 5 * D:6 * D].partition_broadcast(128)), reads=["d_modrow"], writes=["mG2"])
    P.dma("sp", "m3", lambda e: e.dma_start(out=brt[:], in_=b_router[l, :].partition_broadcast(128)), writes=["brt"])
    P.dma("sp", "m4", lambda e: e.dma_start(out=wr[:], in_=w_router[l].rearrange("(k p) n -> p k n", p=128)), writes=["wr"])
    xt = P.sbuf("mxt", [128, D], F32)
    ht = P.sbuf("mht", [128, D], F32)
    P.dma("sp", "m5", lambda e: e.dma_start(out=ht[:], in_=norm_gain[l, 1, :].partition_broadcast(128)), writes=["mht"])
    P.op("dve", lambda e: e.scalar_tensor_tensor(A[:], A[:], 1.0, ht[:], ALU.add, ALU.mult), reads=["mA", "mht"], writes=["mA"])
    ss = P.sbuf("mss", [128, 1], F32)
    rs = P.sbuf("mrs", [128, 1], F32)
    hTf = P.sbuf("hTf", [128, 16, 128], F32)
    hT = P.sbuf("hTb", [128, 16, TP], BF16)
    actT = P.sbuf("actT", [128, 16, TP], BF16)
    acc = P.sbuf("acc", [128, TP // 128, D], F32)
    Gm = P.sbuf("Gm", [128, TP // 128, 32], F32)
    lg = P.sbuf("lg", [128, 32], F32)
    mx8 = P.sbuf("mx8", [128, 8], F32)
    nmx = P.sbuf("nmx", [128, 1], F32)
    msk = P.sbuf("msk", [128, 32], F32)
    ssum = P.sbuf("ssum", [128, 1], F32)
    bg32 = P.sbuf("bg32", [32, 128], F32)
    bgT = [P.sbuf(f"bgT{i}", [128, 32], F32) for i in range(2)]
    bd_b = [P.sbuf(f"bd_b{i}", [1, D], BF16) for i in range(2)]
    wg = [P.sbuf(f"wg{i}", [128, 16, 256], BF16) for i in range(2)]
    wl = [P.sbuf(f"wl{i}", [128, 16, 256], BF16) for i in range(2)]
    wd = [P.sbuf(f"wd{i}", [128, 16, 512], BF16) for i in range(2)]
    t1 = [P.sbuf(f"t1_{i}", [128, TP], F32) for i in range(2)]
    t2 = [P.sbuf(f"t2_{i}", [128, TP], F32) for i in range(2)]
    sg = [P.sbuf(f"sg_{i}", [128, TP], F32) for i in range(2)]
    ptr = P.psum("mptr", [128, 4, 128], F32)
    pg = [P.psum(f"pg{i}", [128, TP], F32) for i in range(2)]
    pl = [P.psum(f"pl{i}", [128, TP], F32) for i in range(2)]
    pd = [P.psum(f"pd{i}", [128, 512], F32) for i in range(2)]
    psm = P.psum("mpsm", [128, 64], F32)
    plg = psm[:, 0:32]
    pbg = psm[:, 32:64]

    wi = 0; di = 0; ui = 0; pi = 0; ei = 0
    for ps_ in range(NP):
        for tt in range(TP // 128):
            rows = slice(ps_ * TP + tt * 128, ps_ * TP + (tt + 1) * 128)
            P.dma("sp", "mxt", lambda e, rows=rows: e.dma_start(out=xt[:], in_=xin[rows, :]), writes=["mxt"])
            P.op("act", lambda e: e.activation(out=ht[:], in_=xt[:], func=AF.Square, accum_out=ss[:]), reads=["mxt"], writes=["mht", "mss"])
            P.op("dve", lambda e: e.tensor_scalar(rs[:], ss[:], 1.0 / D, 1e-5, ALU.mult, ALU.add), reads=["mss"], writes=["mrs"])
            P.op("act", lambda e: e.activation(out=rs[:], in_=rs[:], func=AF.Sqrt), reads=["mrs"], writes=["mrs"])
            P.op("dve", lambda e: e.reciprocal(rs[:], rs[:]), reads=["mrs"], writes=["mrs"])
            P.op("dve", lambda e: e.scalar_tensor_tensor(ht[:], xt[:], rs[:], A[:], ALU.mult, ALU.mult), reads=["mxt", "mrs", "mA"], writes=["mht"])
            P.op("dve", lambda e: e.tensor_tensor(ht[:], ht[:], Bt[:], ALU.add), reads=["mht", "mB"], writes=["mht"])
            if STOP <= 1:
                P.dma("sp", "mxo", lambda e, rows=rows: e.dma_start(out=xout[rows, :], in_=ht[:]), reads=["mht"], writes=["d_xout"])
                continue
            for q in range(4):
                for i in range(4):
                    k = q * 4 + i
                    P.op("pe", lambda e, k=k, i=i: e.transpose(ptr[:, i, :], ht[:, k * 128:(k + 1) * 128], ident[:]), reads=["mht", "ident"], writes=["mptr"])
                P.op("act", lambda e, q=q: e.copy(out=hTf[:, q * 4:(q + 1) * 4, :], in_=ptr[:]), reads=["mptr"], writes=["hTf"])
                P.op("dve", lambda e, q=q, tt=tt: e.tensor_copy(hT[:, q * 4:(q + 1) * 4, tt * 128:(tt + 1) * 128], hTf[:, q * 4:(q + 1) * 4, :]), reads=["hTf"], writes=["hTb"])
            if STOP <= 2:
                P.dma("sp", "mxo", lambda e, rows=rows: e.dma_start(out=xout[rows, :], in_=hTf[:].rearrange("p k t -> p (k t)")), reads=["hTf", "hTb"], writes=["d_xout"])
                continue
            for k in range(16):
                P.op("pe", lambda e, k=k: e.matmul(plg, hTf[:, k, :], wr[:, k, :], start=(k == 0), stop=(k == 15)), reads=["hTf", "wr"], writes=["mpsm"])
            P.op("dve", lambda e: e.tensor_tensor(lg[:], plg, brt[:], ALU.add), reads=["mpsm", "brt"], writes=["lg"])
            if STOP <= 3:
                P.dma("sp", "mxo", lambda e, rows=rows: e.dma_start(out=xout[rows, 0:32], in_=lg[:]), reads=["lg"], writes=["d_xout"])
                continue
            P.op("dve", lambda e: e.max(mx8[:], lg[:]), reads=["lg"], writes=["mx8"])
            P.op("dve", lambda e: e.tensor_scalar(msk[:], lg[:], mx8[:, 3:4], None, ALU.is_ge), reads=["lg", "mx8"], writes=["msk"])
            P.op("dve", lambda e: e.tensor_scalar(nmx[:], mx8[:, 0:1], -1.0, None, ALU.mult), reads=["mx8"], writes=["nmx"])
            P.op("act", lambda e: e.activation(out=lg[:], in_=lg[:], func=AF.Exp, bias=nmx[:]), reads=["lg", "nmx"], writes=["lg"])
            P.op("dve", lambda e: e.tensor_tensor(lg[:], lg[:], msk[:], ALU.mult), reads=["lg", "msk"], writes=["lg"])
            P.op("dve", lambda e: e.reduce_sum(ssum[:], lg[:], AX.X), reads=["lg"], writes=["ssum"])
            P.op("dve", lambda e: e.reciprocal(ssum[:], ssum[:]), reads=["ssum"], writes=["ssum"])
            P.op("dve", lambda e, tt=tt: e.tensor_scalar(Gm[:, tt, :], lg[:], ssum[:], None, ALU.mult), reads=["lg", "ssum"], writes=["Gm"])
        if mode == 'route' and STOP <= 3:
            continue
        if mode == 'route':
            for tt in range(TP // 128):
                rows = slice(ps_ * TP + tt * 128, ps_ * TP + (tt + 1) * 128)
                P.dma("sp", "mxo", lambda e, rows=rows, tt=tt: e.dma_start(out=xout[rows, 0:32], in_=Gm[:, tt, :]), reads=["Gm"], writes=["d_xout"])
            continue
        P.op("pool", lambda e: e.memset(acc[:], 0.0), reads=["acc"], writes=["acc"])
        for ex in range(nexp):
            eb = ei % 2; ei += 1
            P.dma("sp", "bg32", lambda e, ex=ex: e.dma_start(out=bg32[:], in_=b_gu[l, ex, :].rearrange("(c p) -> c p", p=128)), writes=["bg32"])
            P.op("pe", lambda e: e.transpose(pbg, bg32[:], ident[:32, :32]), reads=["bg32", "ident"], writes=["mpsm"])
            P.op("act", lambda e, eb=eb: e.copy(out=bgT[eb][:], in_=pbg), reads=["mpsm"], writes=[f"bgT{eb}"])
            P.dma("pool", f"bd_b{eb}", lambda e, ex=ex, eb=eb: e.dma_start(out=bd_b[eb][:], in_=b_d[l, ex:ex + 1, :]), writes=[f"bd_b{eb}"])
            for c2 in range(8):
                b = wi % 2; wi += 1
                P.dma("pool", f"wg{b}", lambda e, b=b, ex=ex, c2=c2: e.dma_start(
                    out=wg[b][:], in_=w_gu[l, ex, :, c2 * 256:(c2 + 1) * 256].rearrange("(k p) n -> p k n", p=128)), writes=[f"wg{b}"])
                P.dma("pool", f"wl{b}", lambda e, b=b, ex=ex, c2=c2: e.dma_start(
                    out=wl[b][:], in_=w_gu[l, ex, :, D + c2 * 256:D + (c2 + 1) * 256].rearrange("(k p) n -> p k n", p=128)), writes=[f"wl{b}"])
                for sub in range(2):
                    u = ui % 2; ui += 1
                    ch = c2 * 2 + sub
                    for k in range(16):
                        P.op("pe", lambda e, u=u, b=b, k=k, sub=sub: e.matmul(pg[u][:], wg[b][:, k, sub * 128:(sub + 1) * 128], hT[:, k, :], start=(k == 0), stop=(k == 15)),
                             reads=[f"wg{b}", "hTb"], writes=[f"pg{u}"])
                    for k in range(16):
                        P.op("pe", lambda e, u=u, b=b, k=k, sub=sub: e.matmul(pl[u][:], wl[b][:, k, sub * 128:(sub + 1) * 128], hT[:, k, :], start=(k == 0), stop=(k == 15)),
                             reads=[f"wl{b}", "hTb"], writes=[f"pl{u}"])
                    P.op("dve", lambda e, u=u, eb=eb, ch=ch: e.tensor_scalar(t1[u][:], pg[u][:], bgT[eb][:, ch:ch + 1], 7.0, ALU.add, ALU.min),
                         reads=[f"pg{u}", f"bgT{eb}"], writes=[f"t1_{u}"])
                    P.op("act", lambda e, u=u: e.activation(out=sg[u][:], in_=t1[u][:], func=AF.Sigmoid, scale=1.702), reads=[f"t1_{u}"], writes=[f"sg_{u}"])
                    P.op("dve", lambda e, u=u, eb=eb, ch=ch: e.tensor_scalar(t2[u][:], pl[u][:], bgT[eb][:, 16 + ch:16 + ch + 1], 7.0, ALU.add, ALU.min),
                         reads=[f"pl{u}", f"bgT{eb}"], writes=[f"t2_{u}"])
                    P.op("dve", lambda e, u=u: e.tensor_scalar(t2[u][:], t2[u][:], -7.0, 1.0, ALU.max, ALU.add), reads=[f"t2_{u}"], writes=[f"t2_{u}"])
                    P.op("dve", lambda e, u=u: e.tensor_tensor(t1[u][:], t1[u][:], sg[u][:], ALU.mult), reads=[f"t1_{u}", f"sg_{u}"], writes=[f"t1_{u}"])
                    P.op("dve", lambda e, u=u, ch=ch: e.tensor_tensor(actT[:, ch, :], t1[u][:], t2[u][:], ALU.mult), reads=[f"t1_{u}", f"t2_{u}"], writes=["actT"])
            for cc in range(4):
                b = di % 2; di += 1
                P.dma("pool", f"wd{b}", lambda e, b=b, ex=ex, cc=cc: e.dma_start(
                    out=wd[b][:], in_=w_d[l, ex, :, cc * 512:(cc + 1) * 512].rearrange("(k p) n -> p k n", p=128)), writes=[f"wd{b}"])
                for tt in range(TP // 128):
                    u = pi % 2; pi += 1
                    for i in range(16):
                        P.op("pe", lambda e, u=u, b=b, i=i, tt=tt: e.matmul(pd[u][:], actT[:, i, tt * 128:(tt + 1) * 128], wd[b][:, i, :], start=(i == 0), stop=False),
                             reads=["actT", f"wd{b}"], writes=[f"pd{u}"])
                    P.op("pe", lambda e, u=u, eb=eb, cc=cc: e.matmul(pd[u][:], ones_b[:], bd_b[eb][:, cc * 512:(cc + 1) * 512], start=False, stop=True),
                         reads=["ones_b", f"bd_b{eb}"], writes=[f"pd{u}"])
                    P.op("dve", lambda e, u=u, tt=tt, cc=cc, ex=ex: e.scalar_tensor_tensor(acc[:, tt, cc * 512:(cc + 1) * 512], pd[u][:], Gm[:, tt, ex:ex + 1],
                                                                                     acc[:, tt, cc * 512:(cc + 1) * 512], ALU.mult, ALU.add),
                         reads=[f"pd{u}", "Gm", "acc"], writes=["acc"])
        for tt in range(TP // 128):
            rows = slice(ps_ * TP + tt * 128, ps_ * TP + (tt + 1) * 128)
            P.dma("sp", "mxt", lambda e, rows=rows: e.dma_start(out=xt[:], in_=xin[rows, :]), writes=["mxt"])
            P.op("dve", lambda e, tt=tt: e.tensor_tensor(ht[:], acc[:, tt, :], G2[:], ALU.mult), reads=["acc", "mG2"], writes=["mht"])
            P.op("dve", lambda e: e.tensor_tensor(ht[:], ht[:], xt[:], ALU.add), reads=["mht", "mxt"], writes=["mht"])
            P.dma("sp", "mxo", lambda e, rows=rows: e.dma_start(out=xout[rows, :], in_=ht[:]), reads=["mht"], writes=["d_xout%d" % l])

TWO_PI = 2.0 * math.pi


def _wrap_sincos(P, frac_src, n, tagp, tmp_i, tmp_f, g, sin_out, cos_out, u, u_name=None, ti_name=None):
    u_name = u_name or (tagp + "u"); ti_name = ti_name or (tagp + "ti")
    P.op("dve", lambda e: e.tensor_copy(tmp_i, u), reads=[u_name], writes=[ti_name])
    P.op("dve", lambda e: e.tensor_copy(tmp_f, tmp_i), reads=[ti_name], writes=[tagp + "tf"])
    P.op("dve", lambda e: e.tensor_tensor(tmp_f, u, tmp_f, ALU.subtract), reads=[u_name, tagp + "tf"], writes=[tagp + "tf"])
    P.op("dve", lambda e: e.scalar_tensor_tensor(g, tmp_f, 0.5, tmp_f, ALU.is_gt, ALU.subtract), reads=[tagp + "tf"], writes=[tagp + "g"])
    P.op("dve", lambda e: e.scalar_tensor_tensor(tmp_f, g, 0.5, g, ALU.is_gt, ALU.subtract), reads=[tagp + "g"], writes=[tagp + "tf"])
    P.op("act", lambda e: e.activation(out=sin_out, in_=tmp_f, func=AF.Sin, scale=TWO_PI), reads=[tagp + "tf"], writes=[tagp + "sin"])
    P.op("dve", lambda e: e.tensor_scalar(tmp_f, tmp_f, 0.25, None, ALU.add), reads=[tagp + "tf", tagp + "sin"], writes=[tagp + "tf"])
    P.op("dve", lambda e: e.scalar_tensor_tensor(g, tmp_f, 0.5, tmp_f, ALU.is_gt, ALU.subtract), reads=[tagp + "tf"], writes=[tagp + "g"])
    P.op("act", lambda e: e.activation(out=cos_out, in_=g, func=AF.Sin, scale=-TWO_PI), reads=[tagp + "g"], writes=[tagp + "cos"])


def emit_ssm(P, nc, xin, xout, hbuf, modrow, norm_gain, lam_re, lam_im, log_dt, b_re, b_im, c_re, c_im, d_skip,
             w_a, b_a, w_b, b_b, debug_y=None):
    ident = P.ident
    m_pre = P.mark()
    A = P.sbuf("sA", [128, D], F32)
    Bt = P.sbuf("sB", [128, D], F32)
    xt = P.sbuf("sxt", [128, D], F32)
    ht = P.sbuf("sht", [128, D], F32)
    ss = P.sbuf("sss", [128, 1], F32)
    rs = P.sbuf("srs", [128, 1], F32)
    P.dma("sp", "s0", lambda e: e.dma_start(out=Bt[:], in_=modrow[1, 0:D].partition_broadcast(128)), reads=["d_modrow"], writes=["sB"])
    P.dma("sp", "s1", lambda e: e.dma_start(out=A[:], in_=modrow[1, D:2 * D].partition_broadcast(128)), reads=["d_modrow"], writes=["sA"])
    P.dma("sp", "s2", lambda e: e.dma_start(out=ht[:], in_=norm_gain[1, 0, :].partition_broadcast(128)), writes=["sht"])
    P.op("dve", lambda e: e.scalar_tensor_tensor(A[:], A[:], 1.0, ht[:], ALU.add, ALU.mult), reads=["sA", "sht"], writes=["sA"])
    for t in range(16):
        rows = slice(t * 128, (t + 1) * 128)
        P.dma("sp", "sxt", lambda e, rows=rows: e.dma_start(out=xt[:], in_=xin[rows, :]), writes=["sxt"])
        P.op("act", lambda e: e.activation(out=ht[:], in_=xt[:], func=AF.Square, accum_out=ss[:]), reads=["sxt"], writes=["sht", "sss"])
        P.op("dve", lambda e: e.tensor_scalar(rs[:], ss[:], 1.0 / D, 1e-5, ALU.mult, ALU.add), reads=["sss"], writes=["srs"])
        P.op("act", lambda e: e.activation(out=rs[:], in_=rs[:], func=AF.Sqrt), reads=["srs"], writes=["srs"])
        P.op("dve", lambda e: e.reciprocal(rs[:], rs[:]), reads=["srs"], writes=["srs"])
        P.op("dve", lambda e: e.scalar_tensor_tensor(ht[:], xt[:], rs[:], A[:], ALU.mult, ALU.mult), reads=["sxt", "srs", "sA"], writes=["sht"])
        P.op("dve", lambda e: e.tensor_tensor(ht[:], ht[:], Bt[:], ALU.add), reads=["sht", "sB"], writes=["sht"])
        P.dma("sp", "shb", lambda e, rows=rows: e.dma_start(out=hbuf[rows, :], in_=ht[:]), reads=["sht"], writes=["d_hbuf"])
    P.barrier_all()
    P.release(m_pre)

    ptr = P.psum("sptr", [128, 4, 128], F32)
    pxr = [P.psum(f"pxr{i}", [128, 512], F32) for i in range(2)]
    pxi = [P.psum(f"pxi{i}", [128, 512], F32) for i in range(2)]
    pyc = [P.psum(f"pyc{i}", [128, 512], F32) for i in range(2)]

    l64 = P.sbuf("l64", [64, 3, 128], F32)
    dt2 = P.sbuf("dt2", [64, 2], F32)
    P.dma("sp", "p0", lambda e: e.dma_start(out=l64[:, 0, :], in_=lam_re[0].rearrange("(j a) p -> j (a p)", a=2)), writes=["l64"])
    P.dma("sp", "p1", lambda e: e.dma_start(out=l64[:, 1, :], in_=lam_im[0].rearrange("(j a) p -> j (a p)", a=2)), writes=["l64"])
    P.dma("sp", "p2", lambda e: e.dma_start(out=dt2[:], in_=log_dt[0].rearrange("(j a) -> j a", a=2)), writes=["dt2"])
    P.op("dve", lambda e: e.tensor_copy(l64[:, 2, 0:64], dt2[:, 0:1].to_broadcast([64, 64])), reads=["dt2", "l64"], writes=["l64"])
    P.op("dve", lambda e: e.tensor_copy(l64[:, 2, 64:128], dt2[:, 1:2].to_broadcast([64, 64])), reads=["dt2", "l64"], writes=["l64"])
    prm = P.sbuf("prm", [128, 3, 64], F32)
    for i in range(3):
        P.op("pe", lambda e, i=i: e.transpose(ptr[:, i, 0:64], l64[:, i, :], ident[:64, :64]), reads=["l64", "ident"], writes=["sptr"])
    P.op("act", lambda e: e.copy(out=prm[:], in_=ptr[:, 0:3, 0:64]), reads=["sptr"], writes=["prm"])
    lr = prm[:, 0, :]; li = prm[:, 1, :]
    q = P.sbuf("q", [128, 12, 64], F32)
    qi = P.sbuf("qi", [128, 64], I32)
    dtc, rr, thf, sn, cs, abr, abi, den, fre, fim, tA, tB = [q[:, i, :] for i in range(12)]
    P.op("act", lambda e: e.activation(out=dtc, in_=prm[:, 2, :], func=AF.Exp), reads=["prm"], writes=["q_dt"])
    P.op("dve", lambda e: e.tensor_tensor(rr, lr, dtc, ALU.mult), reads=["prm", "q_dt"], writes=["q_r"])
    P.op("act", lambda e: e.activation(out=rr, in_=rr, func=AF.Exp), reads=["q_r"], writes=["q_r"])
    P.op("dve", lambda e: e.scalar_tensor_tensor(thf, li, 1.0 / TWO_PI, dtc, ALU.mult, ALU.mult), reads=["prm", "q_dt"], writes=["pp_u"])
    _wrap_sincos(P, None, 64, "pp_", qi[:], tA, tB, sn, cs, thf)
    P.op("dve", lambda e: e.tensor_tensor(abr, rr, cs, ALU.mult), reads=["q_r", "pp_cos"], writes=["q_abr"])
    P.op("dve", lambda e: e.tensor_tensor(abi, rr, sn, ALU.mult), reads=["q_r", "pp_sin"], writes=["q_abi"])
    P.op("dve", lambda e: e.tensor_tensor(den, lr, lr, ALU.mult), reads=["prm"], writes=["q_den"])
    P.op("dve", lambda e: e.tensor_tensor(tA, li, li, ALU.mult), reads=["prm", "pp_tf"], writes=["pp_tf"])
    P.op("dve", lambda e: e.tensor_tensor(den, den, tA, ALU.add), reads=["q_den", "pp_tf"], writes=["q_den"])
    P.op("dve", lambda e: e.reciprocal(den, den), reads=["q_den"], writes=["q_den"])
    P.op("dve", lambda e: e.tensor_scalar(abr, abr, -1.0, None, ALU.add), reads=["q_abr"], writes=["q_abr"])
    P.op("dve", lambda e: e.tensor_tensor(tA, abr, lr, ALU.mult), reads=["q_abr", "prm", "pp_tf"], writes=["pp_tf"])
    P.op("dve", lambda e: e.tensor_tensor(tB, abi, li, ALU.mult), reads=["q_abi", "prm", "pp_g"], writes=["pp_g"])
    P.op("dve", lambda e: e.tensor_tensor(tA, tA, tB, ALU.add), reads=["pp_tf", "pp_g"], writes=["pp_tf"])
    P.op("dve", lambda e: e.tensor_tensor(fre, tA, den, ALU.mult), reads=["pp_tf", "q_den"], writes=["q_fre"])
    P.op("dve", lambda e: e.tensor_tensor(tA, abi, lr, ALU.mult), reads=["q_abi", "prm", "pp_tf"], writes=["pp_tf"])
    P.op("dve", lambda e: e.tensor_tensor(tB, abr, li, ALU.mult), reads=["q_abr", "prm", "pp_g"], writes=["pp_g"])
    P.op("dve", lambda e: e.tensor_tensor(tA, tA, tB, ALU.subtract), reads=["pp_tf", "pp_g"], writes=["pp_tf"])
    P.op("dve", lambda e: e.tensor_tensor(fim, tA, den, ALU.mult), reads=["pp_tf", "q_den"], writes=["q_fim"])
    P.op("dve", lambda e: e.scalar_tensor_tensor(thf, li, 1.0 / TWO_PI, dtc, ALU.mult, ALU.mult), reads=["prm", "q_dt", "pp_u"], writes=["pp_u"])

    Bre = P.sbuf("Bre", [128, 64, 16], F32)
    Bim = P.sbuf("Bim", [128, 64, 16], F32)
    bbr = P.sbuf("bbr", [128, 64, 16], F32)
    bbi = P.sbuf("bbi", [128, 64, 16], F32)
    for a in range(2):
        P.dma("sp", f"pb{a}", lambda e, a=a: e.dma_start(out=Bre[a * 64:(a + 1) * 64, :, :], in_=b_re[0].rearrange("(j a) p c -> a p j c", a=2)[a]), writes=["Bre"])
        P.dma("sp", f"pc{a}", lambda e, a=a: e.dma_start(out=Bim[a * 64:(a + 1) * 64, :, :], in_=b_im[0].rearrange("(j a) p c -> a p j c", a=2)[a]), writes=["Bim"])
    freb = fre.unsqueeze(2).to_broadcast([128, 64, 16])
    fimb = fim.unsqueeze(2).to_broadcast([128, 64, 16])
    P.op("dve", lambda e: e.tensor_tensor(bbr[:], Bre[:], freb, ALU.mult), reads=["Bre", "q_fre"], writes=["bbr"])
    P.op("dve", lambda e: e.tensor_tensor(bbi[:], Bim[:], fimb, ALU.mult), reads=["Bim", "q_fim"], writes=["bbi"])
    P.op("dve", lambda e: e.tensor_tensor(bbr[:], bbr[:], bbi[:], ALU.subtract), reads=["bbr", "bbi"], writes=["bbr"])
    P.op("dve", lambda e: e.tensor_tensor(bbi[:], Bre[:], fimb, ALU.mult), reads=["Bre", "q_fim", "bbi"], writes=["bbi"])
    P.op("dve", lambda e: e.tensor_tensor(Bre[:], Bim[:], freb, ALU.mult), reads=["Bim", "q_fre", "Bre"], writes=["Bre"])
    P.op("dve", lambda e: e.tensor_tensor(bbi[:], bbi[:], Bre[:], ALU.add), reads=["bbi", "Bre"], writes=["bbi"])

    d16 = P.sbuf("d16", [16, 128], F32)
    dT = P.sbuf("dT", [128, 16], F32)
    P.dma("sp", "p3", lambda e: e.dma_start(out=d16[:], in_=d_skip[0].rearrange("(k p) -> k p", p=128)), writes=["d16"])
    P.op("pe", lambda e: e.transpose(ptr[:, 3, 0:16], d16[:], ident[:16, :16]), reads=["d16", "ident"], writes=["sptr"])
    P.op("act", lambda e: e.copy(out=dT[:], in_=ptr[:, 3, 0:16]), reads=["sptr"], writes=["dT"])

    iot = P.sbuf("iot", [128, S], F32)
    P.op("pool", lambda e: e.iota(iot[:], [[1, S]], base=0, channel_multiplier=0, allow_small_or_imprecise_dtypes=True), writes=["iot"])

    XR = P.sbuf("XR", [128, 4, 128], F32)
    XI = P.sbuf("XI", [128, 4, 128], F32)
    BBr = P.sbuf("BBr", [128, 4, 128], F32)
    BBi = P.sbuf("BBi", [128, 4, 128], F32)
    CCr = P.sbuf("CCr", [128, 4, 128], F32)
    CCi = P.sbuf("CCi", [128, 4, 128], F32)
    for tname, tt_ in (("XR", XR), ("XI", XI), ("CCr", CCr), ("CCi", CCi)):
        P.op("pool", lambda e, tt_=tt_: e.memset(tt_[:], 0.0), writes=[tname])
    Cld = P.sbuf("Cld", [128, 2, 2, 64], F32)
    Td = P.sbuf("Td", [128, 2, 128], F32)
    hld = P.sbuf("hld", [128, 16, 128], F32)
    uT = P.sbuf("uT", [128, S], F32)
    yacc = P.sbuf("yacc", [128, S], F32)
    ygT = P.sbuf("ygT", [128, 16, S], BF16)
    cosT = P.sbuf("cosT", [128, S], F32)
    sinT = P.sbuf("sinT", [128, S], F32)
    tf_ = P.sbuf("tf_", [128, S], F32)
    gg = P.sbuf("gg", [128, S], F32)
    wre = P.sbuf("wre", [128, S], F32)
    wim = P.sbuf("wim", [128, S], F32)
    qre = P.sbuf("qre", [128, S], F32)
    qim = P.sbuf("qim", [128, S], F32)

    bi = 0; ci = 0
    for jk in range(16):
        for jj in range(4):
            j = jk * 4 + jj
            r0 = jj * 32
            P.op("dve", lambda e, j=j, jj=jj, r0=r0: e.tensor_copy(XR[0:64, jj, r0:r0 + 16], bbr[0:64, j, :]), reads=["bbr", "XR"], writes=["XR"])
            P.op("dve", lambda e, j=j, jj=jj, r0=r0: e.tensor_copy(XR[64:128, jj, r0 + 16:r0 + 32], bbr[64:128, j, :]), reads=["bbr", "XR"], writes=["XR"])
            P.op("dve", lambda e, j=j, jj=jj, r0=r0: e.tensor_copy(XI[0:64, jj, r0:r0 + 16], bbi[0:64, j, :]), reads=["bbi", "XI"], writes=["XI"])
            P.op("dve", lambda e, j=j, jj=jj, r0=r0: e.tensor_copy(XI[64:128, jj, r0 + 16:r0 + 32], bbi[64:128, j, :]), reads=["bbi", "XI"], writes=["XI"])
        for jj in range(4):
            P.op("pe", lambda e, jj=jj: e.transpose(ptr[:, jj, :], XR[:, jj, :], ident[:]), reads=["XR", "ident"], writes=["sptr"])
        P.op("act", lambda e: e.copy(out=BBr[:], in_=ptr[:]), reads=["sptr"], writes=["BBr"])
        for jj in range(4):
            P.op("pe", lambda e, jj=jj: e.transpose(ptr[:, jj, :], XI[:, jj, :], ident[:]), reads=["XI", "ident"], writes=["sptr"])
        P.op("act", lambda e: e.copy(out=BBi[:], in_=ptr[:]), reads=["sptr"], writes=["BBi"])
        crows = slice(jk * 128, (jk + 1) * 128)
        for dup in range(2):
            P.dma("sp", "cld", lambda e, dup=dup, crows=crows: e.dma_start(out=Cld[:, 0, dup, :], in_=c_re[0].rearrange("g c p -> (g c) p")[crows, :]), writes=["Cld"])
            P.dma("sp", "cld", lambda e, dup=dup, crows=crows: e.dma_start(out=Cld[:, 1, dup, :], in_=c_im[0].rearrange("g c p -> (g c) p")[crows, :]), writes=["Cld"])
        for ri in range(2):
            P.op("pe", lambda e, ri=ri: e.transpose(ptr[:, ri, :], Cld[:, ri, :, :].rearrange("p d q -> p (d q)"), ident[:]), reads=["Cld", "ident"], writes=["sptr"])
        P.op("act", lambda e: e.copy(out=Td[:], in_=ptr[:, 0:2, :]), reads=["sptr"], writes=["Td"])
        for jj in range(4):
            r0 = jj * 32
            P.op("dve", lambda e, jj=jj, r0=r0: e.tensor_copy(CCr[0:64, jj, r0:r0 + 16], Td[0:64, 0, r0:r0 + 16]), reads=["Td", "CCr"], writes=["CCr"])
            P.op("dve", lambda e, jj=jj, r0=r0: e.tensor_copy(CCr[64:128, jj, r0 + 16:r0 + 32], Td[64:128, 0, r0 + 16:r0 + 32]), reads=["Td", "CCr"], writes=["CCr"])
            P.op("dve", lambda e, jj=jj, r0=r0: e.tensor_scalar(CCi[0:64, jj, r0:r0 + 16], Td[0:64, 1, r0:r0 + 16], -1.0, None, ALU.mult), reads=["Td", "CCi"], writes=["CCi"])
            P.op("dve", lambda e, jj=jj, r0=r0: e.tensor_scalar(CCi[64:128, jj, r0 + 16:r0 + 32], Td[64:128, 1, r0 + 16:r0 + 32], -1.0, None, ALU.mult), reads=["Td", "CCi"], writes=["CCi"])
        P.dma("sp", "hld", lambda e, jk=jk: e.dma_start(out=hld[:], in_=hbuf[:, jk * 128:(jk + 1) * 128].rearrange("(t p) f -> p t f", p=128)), reads=["d_hbuf"], writes=["hld"])
        for qd in range(4):
            for i in range(4):
                t = qd * 4 + i
                P.op("pe", lambda e, t=t, i=i: e.transpose(ptr[:, i, :], hld[:, t, :], ident[:]), reads=["hld", "ident"], writes=["sptr"])
            P.op("act", lambda e, qd=qd: e.copy(out=uT[:, qd * 512:(qd + 1) * 512], in_=ptr[:].rearrange("p a b -> p (a b)")), reads=["sptr"], writes=["uT"])
        P.op("dve", lambda e, jk=jk: e.tensor_scalar(yacc[:], uT[:], dT[:, jk:jk + 1], None, ALU.mult), reads=["uT", "dT"], writes=["yacc"])
        for jj in range(4):
            j = jk * 4 + jj
            r0 = jj * 32
            P.op("dve", lambda e, j=j: e.tensor_scalar(qre[:], iot[:], thf[:, j:j + 1], None, ALU.mult), reads=["iot", "pp_u"], writes=["qre"])
            _wrap_sincos(P, None, S, "tt_", qim[:].bitcast(I32), tf_[:], gg[:], sinT[:], cosT[:], qre[:], u_name="qre", ti_name="qim")
            for n in range(4):
                b = bi % 2; bi += 1
                ns = slice(n * 512, (n + 1) * 512)
                P.op("pe", lambda e, b=b, jj=jj, r0=r0, ns=ns: e.matmul(pxr[b][:], BBr[min(r0, 64):r0 + 32, jj, :], uT[min(r0, 64):r0 + 32, ns], start=True, stop=True),
                     reads=["BBr", "uT"], writes=[f"pxr{b}"])
                P.op("pe", lambda e, b=b, jj=jj, r0=r0, ns=ns: e.matmul(pxi[b][:], BBi[min(r0, 64):r0 + 32, jj, :], uT[min(r0, 64):r0 + 32, ns], start=True, stop=True),
                     reads=["BBi", "uT"], writes=[f"pxi{b}"])
                P.op("dve", lambda e, b=b, ns=ns: e.tensor_tensor(wre[:, ns], pxr[b][:], cosT[:, ns], ALU.mult), reads=[f"pxr{b}", "tt_cos"], writes=["wre"])
                P.op("dve", lambda e, b=b, ns=ns: e.tensor_tensor(gg[:, ns], pxi[b][:], sinT[:, ns], ALU.mult), reads=[f"pxi{b}", "tt_sin", "tt_g"], writes=["tt_g"])
                P.op("dve", lambda e, ns=ns: e.tensor_tensor(wre[:, ns], wre[:, ns], gg[:, ns], ALU.add), reads=["wre", "tt_g"], writes=["wre"])
                P.op("dve", lambda e, b=b, ns=ns: e.tensor_tensor(wim[:, ns], pxi[b][:], cosT[:, ns], ALU.mult), reads=[f"pxi{b}", "tt_cos"], writes=["wim"])
                P.op("dve", lambda e, b=b, ns=ns: e.tensor_tensor(gg[:, ns], pxr[b][:], sinT[:, ns], ALU.mult), reads=[f"pxr{b}", "tt_sin", "tt_g"], writes=["tt_g"])
                P.op("dve", lambda e, ns=ns: e.tensor_tensor(wim[:, ns], wim[:, ns], gg[:, ns], ALU.subtract), reads=["wim", "tt_g"], writes=["wim"])
            rb = rr[:, j:j + 1].to_broadcast([128, S])
            P.op("dve", lambda e, rb=rb: e.tensor_tensor_scan(qre[:], rb, wre[:], 0.0, ALU.mult, ALU.add), reads=["wre", "q_r"], writes=["qre"])
            P.op("dve", lambda e, rb=rb: e.tensor_tensor_scan(qim[:], rb, wim[:], 0.0, ALU.mult, ALU.add), reads=["wim", "q_r"], writes=["qim"])
            P.op("dve", lambda e: e.tensor_tensor(wre[:], qre[:], cosT[:], ALU.mult), reads=["qre", "tt_cos", "wre"], writes=["wre"])
            P.op("dve", lambda e: e.tensor_tensor(gg[:], qim[:], sinT[:], ALU.mult), reads=["qim", "tt_sin", "tt_g"], writes=["tt_g"])
            P.op("dve", lambda e: e.tensor_tensor(wre[:], wre[:], gg[:], ALU.subtract), reads=["wre", "tt_g"], writes=["wre"])
            P.op("dve", lambda e: e.tensor_tensor(wim[:], qre[:], sinT[:], ALU.mult), reads=["qre", "tt_sin", "wim"], writes=["wim"])
            P.op("dve", lambda e: e.tensor_tensor(gg[:], qim[:], cosT[:], ALU.mult), reads=["qim", "tt_cos", "tt_g"], writes=["tt_g"])
            P.op("dve", lambda e: e.tensor_tensor(wim[:], wim[:], gg[:], ALU.add), reads=["wim", "tt_g"], writes=["wim"])
            for n in range(4):
                c = ci % 2; ci += 1
                ns = slice(n * 512, (n + 1) * 512)
                P.op("pe", lambda e, c=c, jj=jj, ns=ns: e.matmul(pyc[c][:], CCr[:, jj, :], wre[:, ns], start=True, stop=False), reads=["CCr", "wre"], writes=[f"pyc{c}"])
                P.op("pe", lambda e, c=c, jj=jj, ns=ns: e.matmul(pyc[c][:], CCi[:, jj, :], wim[:, ns], start=False, stop=True), reads=["CCi", "wim"], writes=[f"pyc{c}"])
                P.op("dve", lambda e, c=c, ns=ns: e.tensor_tensor(yacc[:, ns], yacc[:, ns], pyc[c][:], ALU.add), reads=["yacc", f"pyc{c}"], writes=["yacc"])
        if debug_y is not None:
            P.dma("sp", "dbg", lambda e, jk=jk: e.dma_start(out=debug_y[jk * 128:(jk + 1) * 128, :], in_=yacc[:]), reads=["yacc"], writes=["d_dbg"])
        P.op("dve", lambda e: e.tensor_tensor(gg[:], yacc[:], yacc[:], ALU.mult), reads=["yacc", "tt_g"], writes=["tt_g"])
        P.op("dve", lambda e: e.tensor_scalar(gg[:], gg[:], 0.044715, 1.0, ALU.mult, ALU.add), reads=["tt_g"], writes=["tt_g"])
        P.op("dve", lambda e: e.tensor_tensor(gg[:], gg[:], yacc[:], ALU.mult), reads=["tt_g", "yacc"], writes=["tt_g"])
        P.op("act", lambda e: e.activation(out=gg[:], in_=gg[:], func=AF.Tanh, scale=0.7978845608028654), reads=["tt_g"], writes=["tt_g"])
        P.op("dve", lambda e: e.tensor_scalar(gg[:], gg[:], 1.0, 0.5, ALU.add, ALU.mult), reads=["tt_g"], writes=["tt_g"])
        P.op("dve", lambda e, jk=jk: e.tensor_tensor(ygT[:, jk, :], gg[:], yacc[:], ALU.mult), reads=["tt_g", "yacc"], writes=["ygT"])

    P.barrier_all()
    G1 = wre; ba_t = wim; bb_t = gg; xcol = [cosT, sinT]; ocol = [qre, qim]
    P.dma("sp", "g0", lambda e: e.dma_start(out=G1[:], in_=modrow[1, 2 * D:3 * D].partition_broadcast(128)), reads=["d_modrow", "wre"], writes=["wre"])
    P.dma("sp", "g1", lambda e: e.dma_start(out=ba_t[:], in_=b_a[0, :].partition_broadcast(128)), reads=["wim"], writes=["wim"])
    P.dma("sp", "g2", lambda e: e.dma_start(out=bb_t[:], in_=b_b[0, :].partition_broadcast(128)), reads=["tt_g"], writes=["tt_g"])
    wa = [tf_[:].bitcast(BF16).rearrange("p (k n) -> p k n", k=16)]
    wb = [iot[:].bitcast(BF16).rearrange("p (k n) -> p k n", k=16)]
    gi = 0
    CW = 256
    for cb in range(D // CW):
        cs_ = slice(cb * CW, (cb + 1) * CW)
        P.dma("pool", "wa0", lambda e, cs_=cs_: e.dma_start(out=wa[0], in_=w_a[0, :, cs_].rearrange("(k p) n -> p k n", p=128)), writes=["wa0"])
        P.dma("pool", "wb0", lambda e, cs_=cs_: e.dma_start(out=wb[0], in_=w_b[0, :, cs_].rearrange("(k p) n -> p k n", p=128)), writes=["wb0"])
        for t in range(16):
            b = gi % 2; gi += 1
            rows = slice(t * 128, (t + 1) * 128)
            xc = xcol[b]; oc = ocol[b]
            P.dma("sp", f"gx{b}", lambda e, rows=rows, cs_=cs_, xc=xc: e.dma_start(out=xc[:, 0:CW], in_=xin[rows, cs_]), reads=[f"xc{b}"], writes=[f"xc{b}"])
            for k in range(16):
                P.op("pe", lambda e, b=b, k=k, rows=rows: e.matmul(pxr[b][:, 0:CW], ygT[:, k, rows], wa[0][:, k, :], start=(k == 0), stop=(k == 15)), reads=["ygT", "wa0"], writes=[f"pxr{b}"])
            for k in range(16):
                P.op("pe", lambda e, b=b, k=k, rows=rows: e.matmul(pxi[b][:, 0:CW], ygT[:, k, rows], wb[0][:, k, :], start=(k == 0), stop=(k == 15)), reads=["ygT", "wb0"], writes=[f"pxi{b}"])
            P.op("dve", lambda e, b=b, cs_=cs_, oc=oc: e.tensor_tensor(oc[:, 512:512 + CW], pxi[b][:, 0:CW], bb_t[:, cs_], ALU.add), reads=[f"pxi{b}", "tt_g"], writes=[f"oc{b}"])
            P.op("act", lambda e, oc=oc, b=b: e.activation(out=oc[:, 512:512 + CW], in_=oc[:, 512:512 + CW], func=AF.Sigmoid), reads=[f"oc{b}"], writes=[f"oc{b}"])
            P.op("dve", lambda e, b=b, cs_=cs_, oc=oc: e.tensor_tensor(oc[:, 0:CW], pxr[b][:, 0:CW], ba_t[:, cs_], ALU.add), reads=[f"pxr{b}", "wim", f"oc{b}"], writes=[f"oc{b}"])
            P.op("dve", lambda e, oc=oc, b=b: e.tensor_tensor(oc[:, 0:CW], oc[:, 0:CW], oc[:, 512:512 + CW], ALU.mult), reads=[f"oc{b}"], writes=[f"oc{b}"])
            P.op("dve", lambda e, oc=oc, b=b, cs_=cs_: e.tensor_tensor(oc[:, 0:CW], oc[:, 0:CW], G1[:, cs_], ALU.mult), reads=[f"oc{b}", "wre"], writes=[f"oc{b}"])
            P.op("dve", lambda e, oc=oc, xc=xc, b=b: e.tensor_tensor(oc[:, 0:CW], oc[:, 0:CW], xc[:, 0:CW], ALU.add), reads=[f"oc{b}", f"xc{b}"], writes=[f"oc{b}"])
            P.dma("sp", f"go{b}", lambda e, rows=rows, cs_=cs_, oc=oc: e.dma_start(out=xout[rows, cs_], in_=oc[:, 0:CW]), reads=[f"oc{b}"], writes=["d_xout_ssm"])


_REP = ["norm_gain", "ada_w", "ada_b", "attn_w_qkv", "attn_b_qkv", "attn_q_gain", "attn_k_gain", "attn_sinks",
        "attn_w_o", "attn_b_o", "ssm_lam_re", "ssm_lam_im", "ssm_log_dt", "ssm_b_re", "ssm_b_im", "ssm_c_re",
        "ssm_c_im", "ssm_d", "ssm_w_glu_a", "ssm_b_glu_a", "ssm_w_glu_b", "ssm_b_glu_b", "moe_w_router",
        "moe_b_router", "moe_w_gate_up", "moe_b_gate_up", "moe_w_down", "moe_b_down"]
_SHAPES = {"norm_gain": [2, 2, D], "ada_w": [2, D, 6 * D], "ada_b": [2, 6 * D], "attn_w_qkv": [1, D, QKV],
           "attn_b_qkv": [1, QKV], "attn_q_gain": [1, 64], "attn_k_gain": [1, 64], "attn_sinks": [1, 32],
           "attn_w_o": [1, D, D], "attn_b_o": [1, D], "ssm_lam_re": [1, 128, 64], "ssm_lam_im": [1, 128, 64],
           "ssm_log_dt": [1, 128], "ssm_b_re": [1, 128, 64, 16], "ssm_b_im": [1, 128, 64, 16],
           "ssm_c_re": [1, 128, 16, 64], "ssm_c_im": [1, 128, 16, 64], "ssm_d": [1, D], "ssm_w_glu_a": [1, D, D],
           "ssm_b_glu_a": [1, D], "ssm_w_glu_b": [1, D, D], "ssm_b_glu_b": [1, D], "moe_w_router": [2, D, 32],
           "moe_b_router": [2, 32], "moe_w_gate_up": [2, 32, D, 2 * D], "moe_b_gate_up": [2, 32, 2 * D],
           "moe_w_down": [2, 32, D, D], "moe_b_down": [2, 32, D]}


def build_full():
    nc = bass.Bass("TRN2", target_bir_lowering=False)
    dt = lambda name, shape, kind="ExternalInput": nc.dram_tensor(name, list(shape), F32, kind=kind).ap()
    x = dt("x", [S, D]); c = dt("c", [D]); biasmask = dt("biasmask", [32, 128, 256])
    a = {n: dt(n, _SHAPES[n]) for n in _REP}
    out = dt("out", [S, D], kind="ExternalOutput")
    scr = lambda name, shape: nc.dram_tensor(name, list(shape), F32, kind="Internal").ap()
    modrow = scr("modrow", [2, 6 * D]); xa0 = scr("xa0", [S, D]); xb0 = scr("xb0", [S, D]); xa1 = scr("xa1", [S, D]); hbuf = scr("hbuf", [S, D])
    P = Prog(nc)
    P.ident = P.sbuf("ident", [128, 128], F32)
    P.op("pool", lambda e: e.memset(P.ident[:], 1.0), writes=["ident"])
    P.op("pool", lambda e: e.affine_select(P.ident[:], P.ident[:], [[-1, 128]], ALU.is_equal, 0.0, base=0, channel_multiplier=1), reads=["ident"], writes=["ident"])
    P.barrier_all()
    m0 = P.mark()
    emit_adaln(P, nc, c, a["ada_w"], a["ada_b"], modrow)
    P.barrier_all(); P.release(m0)
    emit_attn(P, nc, x, xa0, modrow, a["norm_gain"], a["attn_w_qkv"], a["attn_b_qkv"], a["attn_q_gain"], a["attn_k_gain"],
              a["attn_sinks"], a["attn_w_o"], a["attn_b_o"], biasmask)
    P.barrier_all(); P.release(m0)
    P.prefix = 'm0_'
    emit_moe(P, nc, 0, xa0, xb0, modrow, a["norm_gain"], a["moe_w_router"], a["moe_b_router"], a["moe_w_gate_up"],
             a["moe_b_gate_up"], a["moe_w_down"], a["moe_b_down"])
    P.barrier_all(); P.release(m0)
    P.prefix = 's_'
    emit_ssm(P, nc, xb0, xa1, hbuf, modrow, a["norm_gain"], a["ssm_lam_re"], a["ssm_lam_im"], a["ssm_log_dt"], a["ssm_b_re"],
             a["ssm_b_im"], a["ssm_c_re"], a["ssm_c_im"], a["ssm_d"], a["ssm_w_glu_a"], a["ssm_b_glu_a"], a["ssm_w_glu_b"], a["ssm_b_glu_b"])
    P.barrier_all(); P.release(m0)
    P.prefix = 'm1_'
    emit_moe(P, nc, 1, xa1, out, modrow, a["norm_gain"], a["moe_w_router"], a["moe_b_router"], a["moe_w_gate_up"],
             a["moe_b_gate_up"], a["moe_w_down"], a["moe_b_down"])
    P.barrier_all()
    P.emit()
    P.close()
    return nc


def kernel(**inputs):
    n_cores = int(inputs.pop("_n_cores", 8)) if "_n_cores" in inputs else 8
    x = np.asarray(inputs["x"], dtype=np.float32)
    c = np.asarray(inputs["c"], dtype=np.float32)
    base = {n: np.ascontiguousarray(np.asarray(inputs[n], dtype=np.float32)) for n in _REP}
    base["biasmask"] = make_biasmask(np.asarray(inputs["rel_bias"], dtype=np.float32))
    nc = build_full()
    in_maps = [dict(base, x=np.ascontiguousarray(x[b]), c=np.ascontiguousarray(c[b])) for b in range(n_cores)]
    res = run_bass_kernel_spmd(nc, in_maps, core_ids=list(range(n_cores)))
    return np.stack([res.results[b]["out"] for b in range(n_cores)], axis=0).astype(np.float32)
```

```python
import math
import copy
import numpy as np
import concourse.bass as bass
import concourse.mybir as mybir
from concourse.bass_utils import run_bass_kernel_spmd

F32 = mybir.dt.float32
F32R = mybir.dt.float32r
I32 = mybir.dt.int32
U32 = mybir.dt.uint32
ALU = mybir.AluOpType
AF = mybir.ActivationFunctionType
AX = mybir.AxisListType

ENGS = ("pe", "act", "dve", "pool", "sp")
SAME_ENGINE_SYNC = {"pe": False, "act": True, "dve": True, "pool": True, "sp": False}


class Prog:
    def __init__(self, nc):
        self.nc = nc
        self.items = {e: [] for e in ENGS}
        self.count = {e: 0 for e in ENGS}
        self.sem = {}
        self.waited = {e: {} for e in ENGS}
        self.last_write = {}
        self.reads_since = {}
        self.dma_count = {}
        self.ctx = []
        for e in ENGS:
            self.sem[e] = self._newsem("eng_" + e)

    def _newsem(self, name):
        cm = self.nc.semaphore(name)
        s = cm.__enter__()
        self.ctx.append(cm)
        return s

    def sbuf(self, name, shape, dt):
        cm = self.nc.sbuf_tensor(getattr(self, 'prefix', '') + name, list(shape), dt)
        t = cm.__enter__()
        self.ctx.append(cm)
        return t

    def psum(self, name, shape, dt):
        cm = self.nc.psum_tensor(getattr(self, 'prefix', '') + name, list(shape), dt)
        t = cm.__enter__()
        self.ctx.append(cm)
        return t

    def _deps(self, reads, writes):
        deps = {}
        def add(k, v):
            if deps.get(k, 0) < v:
                deps[k] = v
        for r in reads:
            if r in self.last_write:
                add(*self.last_write[r])
        for w in writes:
            if w in self.last_write:
                add(*self.last_write[w])
            for k, v in self.reads_since.get(w, {}).items():
                add(k, v)
        return deps

    def _emit_waits(self, eng, deps):
        for k, v in deps.items():
            if k == eng and not SAME_ENGINE_SYNC[eng]:
                continue
            if self.waited[eng].get(k, 0) >= v:
                continue
            self.waited[eng][k] = v
            self.items[eng].append(("wait", self.sem[k], v))

    def _record(self, key, val, reads, writes):
        for r in reads:
            self.reads_since.setdefault(r, {})
            if self.reads_since[r].get(key, 0) < val:
                self.reads_since[r][key] = val
        for w in writes:
            self.last_write[w] = (key, val)
            self.reads_since[w] = {}

    def op(self, eng, fn, reads=(), writes=()):
        deps = self._deps(reads, writes)
        self._emit_waits(eng, deps)
        self.count[eng] += 1
        self.items[eng].append(("op", fn, self.sem[eng], 1))
        self._record(eng, self.count[eng], reads, writes)

    def dma(self, eng, chan, fn, reads=(), writes=()):
        key = "dma_" + chan
        if key not in self.sem:
            self.sem[key] = self._newsem(key)
            self.dma_count[key] = 0
        deps = self._deps(reads, writes)
        self._emit_waits(eng, deps)
        self.dma_count.setdefault(key, 0)
        self.dma_count[key] += 16
        self.items[eng].append(("op", fn, self.sem[key], 16))
        self._record(key, self.dma_count[key], reads, writes)

    def raw(self, eng, fn, reads=(), writes=()):
        self.op(eng, fn, reads, writes)

    def _snap(self):
        return copy.deepcopy((self.count, self.waited, self.last_write, self.reads_since, self.dma_count))

    def _restore(self, st):
        self.count, self.waited, self.last_write, self.reads_since, self.dma_count = copy.deepcopy(st)

    def guard(self, cnt_ap, thresh, fnA, fnB, cnt_res):
        self.barrier_all()
        for e in ENGS:
            self._emit_waits(e, self._deps([cnt_res], []))
        st0 = self._snap()
        main_items = self.items
        self.items = {e: [] for e in ENGS}
        fnA()
        itA = self.items; cA = dict(self.count); dA = dict(self.dma_count)
        self._restore(st0)
        self.items = {e: [] for e in ENGS}
        fnB()
        itB = self.items; cB = dict(self.count); dB = dict(self.dma_count)
        assert dA == dB, "branches must issue identical DMA counts per channel"
        self.items = main_items
        start = st0[0]
        for e in ENGS:
            m = max(cA[e], cB[e])
            for its_all, c in ((itA, cA[e]), (itB, cB[e])):
                d = m - c
                if d > 0:
                    its = its_all[e]
                    idx = min(i for i, it in enumerate(its) if it[0] == "op" and it[3] == 1)
                    it = its[idx]
                    its[idx] = ("op", it[1], it[2], 1 + d)
                    sem_e = self.sem[e]
                    for e2 in ENGS:
                        l2 = its_all[e2]
                        for i, it2 in enumerate(l2):
                            if it2[0] == "wait" and it2[1] is sem_e and it2[2] > start[e]:
                                l2[i] = ("wait", it2[1], it2[2] + d)
            self.count[e] = m
        for e in ENGS:
            self.items[e].append(("if", cnt_ap, thresh, itA[e], itB[e]))
        self.barrier_all()

    def barrier_all(self, engs=ENGS):
        allk = {}
        for e in ENGS:
            if self.count[e]:
                allk[e] = self.count[e]
        for k, v in self.dma_count.items():
            if v:
                allk[k] = v
        for e in engs:
            self._emit_waits(e, dict(allk))

    def emit(self):
        nc = self.nc
        items = self.items
        with nc.Block() as block:
            def run(engine, lst):
                for it in lst:
                    if it[0] == "wait":
                        engine.wait_ge(it[1], it[2])
                    elif it[0] == "if":
                        self._gr = getattr(self, "_gr", 0) + 1
                        with engine.register("gr%d" % self._gr) as gr:
                            engine.reg_load(gr, it[1])
                            with engine.If_lt(gr, it[2]):
                                run(engine, it[3])
                            with engine.Else():
                                run(engine, it[4])
                    else:
                        ins = it[1](engine)
                        ins.then_inc(it[2], it[3])

            @block.tensor
            def _(e):
                run(e, items["pe"])

            @block.scalar
            def _(e):
                run(e, items["act"])

            @block.vector
            def _(e):
                run(e, items["dve"])

            @block.gpsimd
            def _(e):
                run(e, items["pool"])

            @block.sync
            def _(e):
                run(e, items["sp"])

    def mark(self):
        return len(self.ctx)

    def release(self, m):
        while len(self.ctx) > m:
            self.ctx.pop().__exit__(None, None, None)

    def close(self):
        for cm in reversed(self.ctx):
            cm.__exit__(None, None, None)
        self.ctx = []

BF16 = mybir.dt.bfloat16

D = 2048
S = 2048
NT = 16
QKV = 2560


def emit_adaln(P, nc, c_ap, ada_w, ada_b, modrow):
    ident = P.ident
    c16 = P.sbuf("c16", [16, 128], F32)
    condT = P.sbuf("condT", [128, 16], F32)
    pc = P.psum("pc", [128, 16], F32)
    P.dma("sp", "c", lambda e: e.dma_start(out=c16[:], in_=c_ap.rearrange("(k p) -> k p", p=128)), writes=["c16"])
    P.op("pe", lambda e: e.transpose(pc[:], c16[:], ident[:16, :16]), reads=["c16", "ident"], writes=["pc"])
    P.op("act", lambda e: e.activation(out=condT[:], in_=pc[:], func=AF.Silu), reads=["pc"], writes=["condT"])
    wb = [P.sbuf(f"adaw{i}", [128, 16, 512], F32) for i in range(2)]
    pm = [P.psum(f"pm{i}", [1, 512], F32) for i in range(2)]
    bt = [P.sbuf(f"adab{i}", [1, 512], F32) for i in range(2)]
    mt = [P.sbuf(f"mrow{i}", [1, 512], F32) for i in range(2)]
    it = 0
    for l in range(2):
        for cb in range(24):
            b = it % 2
            cs = slice(cb * 512, (cb + 1) * 512)
            P.dma("sp", f"adaw{b}", lambda e, b=b, l=l, cs=cs: e.dma_start(
                out=wb[b][:], in_=ada_w[l, :, cs].rearrange("(k p) n -> p k n", p=128)),
                writes=[f"adaw{b}"])
            P.dma("sp", f"adab{b}", lambda e, b=b, l=l, cs=cs: e.dma_start(out=bt[b][:], in_=ada_b[l:l + 1, cs]), writes=[f"adab{b}"])
            for k in range(16):
                P.op("pe", lambda e, b=b, k=k: e.matmul(pm[b][:], condT[:, k:k + 1], wb[b][:, k, :], start=(k == 0), stop=(k == 15)),
                     reads=["condT", f"adaw{b}"], writes=[f"pm{b}"])
            P.op("dve", lambda e, b=b: e.tensor_tensor(mt[b][:], pm[b][:], bt[b][:], ALU.add),
                 reads=[f"pm{b}", f"adab{b}"], writes=[f"mrow{b}"])
            P.dma("sp", f"mrow{b}", lambda e, b=b, l=l, cs=cs: e.dma_start(out=modrow[l:l + 1, cs], in_=mt[b][:]), reads=[f"mrow{b}"], writes=["d_modrow"])
            it += 1


def emit_attn(P, nc, x, xa, modrow, norm_gain, w_qkv, b_qkv, q_gain, k_gain, sinks, w_o, b_o, biasmask):
    ident = P.ident
    A = P.sbuf("A", [128, D], F32)
    Bt = P.sbuf("Bt", [128, D], F32)
    G1 = P.sbuf("G1", [128, D], F32)
    bq = P.sbuf("bq", [128, QKV], F32)
    bo = P.sbuf("bo", [128, D], F32)
    qg = P.sbuf("qg", [128, 64], F32)
    kg = P.sbuf("kg", [128, 64], F32)
    snk = P.sbuf("snk", [128, 32], F32)
    bm = P.sbuf("bm", [128, 32, 256], F32)
    P.dma("sp", "c0", lambda e: e.dma_start(out=Bt[:], in_=modrow[0, 0:D].partition_broadcast(128)), reads=["d_modrow"], writes=["Bt"])
    P.dma("sp", "c1", lambda e: e.dma_start(out=A[:], in_=modrow[0, D:2 * D].partition_broadcast(128)), reads=["d_modrow"], writes=["A"])
    P.dma("sp", "c2", lambda e: e.dma_start(out=G1[:], in_=modrow[0, 2 * D:3 * D].partition_broadcast(128)), reads=["d_modrow"], writes=["G1"])
    P.dma("sp", "c3", lambda e: e.dma_start(out=bo[:], in_=norm_gain[0, 0, :].partition_broadcast(128)), writes=["bo"])
    P.op("dve", lambda e: e.scalar_tensor_tensor(A[:], A[:], 1.0, bo[:], ALU.add, ALU.mult), reads=["A", "bo"], writes=["A"])
    P.dma("sp", "c3", lambda e: e.dma_start(out=bo[:], in_=b_o[0, :].partition_broadcast(128)), reads=["bo"], writes=["bo"])
    P.dma("sp", "c4", lambda e: e.dma_start(out=bq[:], in_=b_qkv[0, :].partition_broadcast(128)), writes=["bq"])
    P.dma("sp", "c5", lambda e: e.dma_start(out=qg[:], in_=q_gain[0, :].partition_broadcast(128)), writes=["qg"])
    P.dma("sp", "c6", lambda e: e.dma_start(out=kg[:], in_=k_gain[0, :].partition_broadcast(128)), writes=["kg"])
    P.dma("sp", "c7", lambda e: e.dma_start(out=snk[:], in_=sinks[0, :].partition_broadcast(128)), writes=["snk"])
    P.dma("sp", "c8", lambda e: e.dma_start(out=bm[:], in_=biasmask.rearrange("h q k -> q h k")), writes=["bm"])
    P.op("dve", lambda e: e.tensor_scalar(qg[:], qg[:], 0.125, None, ALU.mult), reads=["qg"], writes=["qg"])

    xt = P.sbuf("xt", [128, D], F32)
    ht = P.sbuf("ht", [128, D], F32)
    junk = P.sbuf("junk", [128, QKV], F32)
    hT = P.sbuf("hT", [128, 16, 128], F32)
    ss = P.sbuf("ss", [128, 1], F32)
    rs = P.sbuf("rs", [128, 1], F32)
    qkv = P.sbuf("qkv", [128, QKV], F32)
    sq36 = P.sbuf("sq36", [128, 36], F32)
    kT = P.sbuf("kT", [64, 4, 256], F32)
    vv = P.sbuf("vv", [128, 2, 256], F32)
    qT = P.sbuf("qT", [64, 128], F32)
    pe_ = P.sbuf("pexp", [128, 256], F32)
    pT = P.sbuf("pT", [128, 2, 128], F32)
    mx = P.sbuf("mx", [128, 1], F32)
    nm = P.sbuf("nm", [128, 1], F32)
    sm = P.sbuf("sm", [128, 1], F32)
    es = P.sbuf("es", [128, 1], F32)
    osb = P.sbuf("osb", [128, D], F32)
    wch = [P.sbuf(f"wch{i}", [128, 16, 256], F32) for i in range(2)]
    ptr = P.psum("ptr", [128, 4, 128], F32)
    pmm = [P.psum(f"pmm{i}", [128, 256], F32) for i in range(2)]
    plg = P.psum("plg", [128, 256], F32)
    ppt = P.psum("ppt", [128, 2, 128], F32)
    po = P.psum("po", [128, 64], F32)
    pq = P.psum("pq", [64, 128], F32)

    P.op("dve", lambda e: e.memset(kT[:], 0.0), writes=["kT"])
    P.op("dve", lambda e: e.memset(vv[:], 0.0), writes=["vv"])
    wi = 0
    for t in range(NT):
        rows = slice(t * 128, (t + 1) * 128)
        P.dma("sp", "xt", lambda e, rows=rows: e.dma_start(out=xt[:], in_=x[rows, :]), writes=["xt"])
        P.op("act", lambda e: e.activation(out=junk[:, 0:D], in_=xt[:], func=AF.Square, accum_out=ss[:]), reads=["xt"], writes=["junk", "ss"])
        P.op("dve", lambda e: e.tensor_scalar(rs[:], ss[:], 1.0 / D, 1e-5, ALU.mult, ALU.add), reads=["ss"], writes=["rs"])
        P.op("act", lambda e: e.activation(out=rs[:], in_=rs[:], func=AF.Sqrt), reads=["rs"], writes=["rs"])
        P.op("dve", lambda e: e.reciprocal(rs[:], rs[:]), reads=["rs"], writes=["rs"])
        P.op("dve", lambda e: e.scalar_tensor_tensor(ht[:], xt[:], rs[:], A[:], ALU.mult, ALU.mult), reads=["xt", "rs", "A"], writes=["ht"])
        P.op("dve", lambda e: e.tensor_tensor(ht[:], ht[:], Bt[:], ALU.add), reads=["ht", "Bt"], writes=["ht"])
        for q in range(4):
            for i in range(4):
                k = q * 4 + i
                P.op("pe", lambda e, k=k, i=i: e.transpose(ptr[:, i, :], ht[:, k * 128:(k + 1) * 128], ident[:]), reads=["ht", "ident"], writes=["ptr"])
            P.op("act", lambda e, q=q: e.copy(out=hT[:, q * 4:(q + 1) * 4, :], in_=ptr[:]), reads=["ptr"], writes=["hT"])
        for cb in range(10):
            b = wi % 2
            wi += 1
            P.dma("sp", f"wch{b}", lambda e, b=b, cb=cb: e.dma_start(
                out=wch[b][:], in_=w_qkv[0, :, cb * 256:(cb + 1) * 256].rearrange("(k p) n -> p k n", p=128)), writes=[f"wch{b}"])
            for k in range(16):
                P.op("pe", lambda e, b=b, k=k: e.matmul(pmm[b][:], hT[:, k, :], wch[b][:, k, :], start=(k == 0), stop=(k == 15)),
                     reads=["hT", f"wch{b}"], writes=[f"pmm{b}"])
            P.op("dve", lambda e, b=b, cb=cb: e.tensor_tensor(qkv[:, cb * 256:(cb + 1) * 256], pmm[b][:], bq[:, cb * 256:(cb + 1) * 256], ALU.add),
                 reads=[f"pmm{b}", "bq"], writes=["qkv"])
        P.op("dve", lambda e: e.tensor_tensor(junk[:, 0:2304], qkv[:, 0:2304], qkv[:, 0:2304], ALU.mult), reads=["qkv"], writes=["junk"])
        P.op("dve", lambda e: e.tensor_reduce(sq36[:], junk[:, 0:2304].rearrange("p (h d) -> p h d", d=64), AX.X, ALU.add), reads=["junk"], writes=["sq36"])
        P.op("dve", lambda e: e.tensor_scalar(sq36[:], sq36[:], 1.0 / 64, 1e-5, ALU.mult, ALU.add), reads=["sq36"], writes=["sq36"])
        P.op("act", lambda e: e.activation(out=sq36[:], in_=sq36[:], func=AF.Sqrt), reads=["sq36"], writes=["sq36"])
        P.op("dve", lambda e: e.reciprocal(sq36[:], sq36[:]), reads=["sq36"], writes=["sq36"])
        for h in range(36):
            g = qg if h < 32 else kg
            gname = "qg" if h < 32 else "kg"
            P.op("dve", lambda e, h=h, g=g: e.scalar_tensor_tensor(qkv[:, h * 64:(h + 1) * 64], qkv[:, h * 64:(h + 1) * 64], sq36[:, h:h + 1], g[:], ALU.mult, ALU.mult),
                 reads=["qkv", "sq36", gname], writes=["qkv"])
        P.op("dve", lambda e: e.tensor_copy(kT[:, :, 0:128], kT[:, :, 128:256]), reads=["kT"], writes=["kT"])
        P.op("dve", lambda e: e.tensor_copy(vv[:, 0, :], vv[:, 1, :]), reads=["vv"], writes=["vv"])
        P.op("dve", lambda e: e.tensor_copy(vv[:, 1, :], qkv[:, 2304:2560]), reads=["qkv", "vv"], writes=["vv"])
        for kh in range(4):
            P.op("pe", lambda e, kh=kh: e.transpose(pq[:], qkv[:, 2048 + kh * 64:2048 + (kh + 1) * 64], ident[:]), reads=["qkv", "ident"], writes=["pq"])
            P.op("act", lambda e, kh=kh: e.copy(out=kT[:, kh, 128:256], in_=pq[:]), reads=["pq", "kT"], writes=["kT"])
        for h in range(32):
            kh = h // 8
            P.op("pe", lambda e, h=h: e.transpose(pq[:], qkv[:, h * 64:(h + 1) * 64], ident[:]), reads=["qkv", "ident"], writes=["pq"])
            P.op("act", lambda e: e.copy(out=qT[:], in_=pq[:]), reads=["pq"], writes=["qT"])
            P.op("pe", lambda e, kh=kh: e.matmul(plg[:], qT[:], kT[:, kh, :], start=True, stop=True), reads=["qT", "kT"], writes=["plg"])
            P.op("dve", lambda e, h=h: e.tensor_tensor(pe_[:], plg[:], bm[:, h, :], ALU.add), reads=["plg", "bm"], writes=["pexp"])
            if t == 0:
                P.op("dve", lambda e: e.memset(pe_[:, 0:128], -30000.0), reads=["pexp"], writes=["pexp"])
            P.op("dve", lambda e: e.reduce_max(mx[:], pe_[:], AX.X), reads=["pexp"], writes=["mx"])
            P.op("dve", lambda e, h=h: e.tensor_scalar(nm[:], mx[:], snk[:, h:h + 1], -1.0, ALU.max, ALU.mult), reads=["mx", "snk"], writes=["nm"])
            P.op("act", lambda e: e.activation(out=pe_[:], in_=pe_[:], func=AF.Exp, bias=nm[:], accum_out=sm[:]), reads=["pexp", "nm"], writes=["pexp", "sm"])
            P.op("act", lambda e, h=h: e.activation(out=es[:], in_=snk[:, h:h + 1], func=AF.Exp, bias=nm[:]), reads=["snk", "nm"], writes=["es"])
            P.op("dve", lambda e: e.tensor_tensor(sm[:], sm[:], es[:], ALU.add), reads=["sm", "es"], writes=["sm"])
            P.op("dve", lambda e: e.reciprocal(sm[:], sm[:]), reads=["sm"], writes=["sm"])
            for j in range(2):
                P.op("pe", lambda e, j=j: e.transpose(ppt[:, j, :], pe_[:, j * 128:(j + 1) * 128], ident[:]), reads=["pexp", "ident"], writes=["ppt"])
            P.op("act", lambda e: e.copy(out=pT[:], in_=ppt[:]), reads=["ppt"], writes=["pT"])
            for j in range(2):
                P.op("pe", lambda e, j=j, kh=kh: e.matmul(po[:], pT[:, j, :], vv[:, j, kh * 64:(kh + 1) * 64], start=(j == 0), stop=(j == 1)),
                     reads=["pT", "vv"], writes=["po"])
            P.op("dve", lambda e, h=h: e.tensor_scalar(osb[:, h * 64:(h + 1) * 64], po[:], sm[:], None, ALU.mult), reads=["po", "sm"], writes=["osb"])
        for q in range(4):
            for i in range(4):
                k = q * 4 + i
                P.op("pe", lambda e, k=k, i=i: e.transpose(ptr[:, i, :], osb[:, k * 128:(k + 1) * 128], ident[:]), reads=["osb", "ident"], writes=["ptr"])
            P.op("act", lambda e, q=q: e.copy(out=hT[:, q * 4:(q + 1) * 4, :], in_=ptr[:]), reads=["ptr"], writes=["hT"])
        for cb in range(8):
            b = wi % 2
            wi += 1
            P.dma("sp", f"wch{b}", lambda e, b=b, cb=cb: e.dma_start(
                out=wch[b][:], in_=w_o[0, :, cb * 256:(cb + 1) * 256].rearrange("(k p) n -> p k n", p=128)), writes=[f"wch{b}"])
            for k in range(16):
                P.op("pe", lambda e, b=b, k=k: e.matmul(pmm[b][:], hT[:, k, :], wch[b][:, k, :], start=(k == 0), stop=(k == 15)),
                     reads=["hT", f"wch{b}"], writes=[f"pmm{b}"])
            cs = slice(cb * 256, (cb + 1) * 256)
            P.op("dve", lambda e, b=b, cs=cs: e.tensor_tensor(ht[:, cs], pmm[b][:], bo[:, cs], ALU.add), reads=[f"pmm{b}", "bo"], writes=["ht"])
            P.op("dve", lambda e, cs=cs: e.tensor_tensor(ht[:, cs], ht[:, cs], G1[:, cs], ALU.mult), reads=["ht", "G1"], writes=["ht"])
            P.op("dve", lambda e, cs=cs: e.tensor_tensor(ht[:, cs], ht[:, cs], xt[:, cs], ALU.add), reads=["ht", "xt"], writes=["ht"])
        P.dma("sp", "xa", lambda e, rows=rows: e.dma_start(out=xa[rows, :], in_=ht[:]), reads=["ht"], writes=["d_xa"])


def make_biasmask(rel_bias):
    ql = np.arange(128)[:, None]
    kl = np.arange(256)[None, :]
    dist = ql + 128 - kl
    n = np.maximum(dist, 0)
    max_exact = 16
    large = max_exact + (np.log(np.maximum(n, 1) / max_exact) / np.log(128 / max_exact) * (32 - max_exact)).astype(np.int32)
    large = np.minimum(large, 31)
    bucket = np.where(n < max_exact, n, large).astype(np.int32)
    valid = (dist >= 0) & (dist < 128)
    bm = np.ascontiguousarray(np.transpose(rel_bias[bucket], (2, 0, 1))).astype(np.float32)
    bm[:, ~valid] = -30000.0
    return bm


STOP = 99
NP = 4
TP = 512


GUARD = True


def emit_moe(P, nc, l, xin, xout, modrow, norm_gain, w_router, b_router, w_gu, b_gu, w_d, b_d, mode='full', nexp=32):
    ident = P.ident
    A = P.sbuf("mA", [128, D], F32)
    brt = P.sbuf("brt", [128, 32], F32)
    wr = P.sbuf("wr", [128, 16, 32], F32)
    identb = P.sbuf("identb", [128, 128], BF16)
    ones_b = P.sbuf("ones_b", [1, 128], BF16)
    P.op("dve", lambda e: e.tensor_copy(identb[:], ident[:]), reads=["ident"], writes=["identb"])
    P.op("dve", lambda e: e.memset(ones_b[:], 1.0), writes=["ones_b"])
    P.dma("sp", "m3", lambda e: e.dma_start(out=brt[:], in_=b_router[l, :].partition_broadcast(128)), writes=["brt"])
    P.dma("sp", "m4", lambda e: e.dma_start(out=wr[:], in_=w_router[l].rearrange("(k p) n -> p k n", p=128)), writes=["wr"])
    xt = P.sbuf("mxt", [128, D], F32)
    ht = P.sbuf("mht", [128, D], F32)
    ss = P.sbuf("mss", [128, 1], F32)
    rs = P.sbuf("mrs", [128, 1], F32)
    hT = P.sbuf("hTb", [128, 16, TP], BF16)
    actT = P.sbuf("actT", [128, 16, TP], BF16)
    acc = P.sbuf("acc", [128, TP // 128, D], F32)
    hTf = acc[:, 0, :].rearrange("p (k t) -> p k t", k=16)
    Gm = P.sbuf("Gm", [128, TP // 128, 32], F32)
    lg = P.sbuf("lg", [128, 32], F32)
    mx8 = P.sbuf("mx8", [128, 8], F32)
    nmx = P.sbuf("nmx", [128, 1], F32)
    msk = P.sbuf("msk", [128, 32], F32)
    ssum = P.sbuf("ssum", [128, 1], F32)
    bg32 = P.sbuf("bg32", [32, 128], F32)
    bgT = [P.sbuf(f"bgT{i}", [128, 32], F32) for i in range(2)]
    bd_b = [P.sbuf(f"bd_b{i}", [1, D], BF16) for i in range(2)]
    wg = [P.sbuf(f"wg{i}", [128, 16, 256], BF16) for i in range(2)]
    wl = [P.sbuf(f"wl{i}", [128, 16, 256], BF16) for i in range(2)]
    wd = [P.sbuf(f"wd{i}", [128, 16, 256], BF16) for i in range(2)]
    CAP = 256
    hTok = P.sbuf("hTok", [128, TP // 128, D], BF16)
    Mall = P.sbuf("Mall", [128, TP // 128, 32], F32)
    rank = P.sbuf("rank", [128, TP // 128, 32], F32)
    cnt_i = P.sbuf("cnt_i", [128, 32], I32)
    onesf = P.sbuf("onesf", [128, 128], F32)
    Lst = P.sbuf("Lst", [128, 128], F32)
    iota_c = P.sbuf("iota_c", [128, CAP], F32)
    Sm = P.sbuf("Sm", [128, TP // 128, CAP], BF16)
    SGm = P.sbuf("SGm", [128, TP // 128, CAP], BF16)
    STm = P.sbuf("STm", [128, CAP // 128, TP // 128, 128], BF16)
    hTc = P.sbuf("hTc", [128, 16, CAP], BF16)
    Bt = hTc[:].rearrange("p k n -> p (k n)").bitcast(F32)
    yc = P.sbuf("yc", [128, CAP // 128, 256], BF16)
    P.op("pool", lambda e: e.memset(onesf[:], 1.0), writes=["onesf"])
    P.op("pool", lambda e: e.memset(Lst[:], 1.0), writes=["Lst"])
    P.op("pool", lambda e: e.affine_select(Lst[:], Lst[:], [[1, 128]], ALU.is_gt, 0.0, base=0, channel_multiplier=-1), reads=["Lst"], writes=["Lst"])
    P.op("pool", lambda e: e.iota(iota_c[:], [[1, CAP]], base=0, channel_multiplier=0, allow_small_or_imprecise_dtypes=True), writes=["iota_c"])
    t1 = [P.sbuf(f"t1_{i}", [128, TP], F32) for i in range(2)]
    t2 = [P.sbuf(f"t2_{i}", [128, TP], F32) for i in range(2)]
    sg = [P.sbuf(f"sg_{i}", [128, TP], F32) for i in range(2)]
    ptr = P.psum("mptr", [128, 4, 128], F32)
    pg = [P.psum(f"pg{i}", [128, TP], F32) for i in range(2)]
    pl = [P.psum(f"pl{i}", [128, TP], F32) for i in range(2)]
    pd = [P.psum(f"pd{i}", [128, 512], F32) for i in range(2)]
    psm = P.psum("mpsm", [128, 64], F32)
    plg = psm[:, 0:32]
    pbg = psm[:, 32:64]

    wi = 0; di = 0; ui = 0; pi = 0; ei = 0
    for ps_ in range(NP):
        P.dma("sp", "m1", lambda e: e.dma_start(out=A[:], in_=modrow[l, 4 * D:5 * D].partition_broadcast(128)), reads=["d_modrow", "mA"], writes=["mA"])
        P.dma("sp", "m0", lambda e: e.dma_start(out=Bt, in_=modrow[l, 3 * D:4 * D].partition_broadcast(128)), reads=["d_modrow", "hTc"], writes=["hTc"])
        P.dma("sp", "m5", lambda e: e.dma_start(out=ht[:], in_=norm_gain[l, 1, :].partition_broadcast(128)), reads=["mht"], writes=["mht"])
        P.op("dve", lambda e: e.scalar_tensor_tensor(A[:], A[:], 1.0, ht[:], ALU.add, ALU.mult), reads=["mA", "mht"], writes=["mA"])
        for tt in range(TP // 128):
            rows = slice(ps_ * TP + tt * 128, ps_ * TP + (tt + 1) * 128)
            P.dma("sp", "mxt", lambda e, rows=rows: e.dma_start(out=xt[:], in_=xin[rows, :]), writes=["mxt"])
            P.op("act", lambda e: e.activation(out=ht[:], in_=xt[:], func=AF.Square, accum_out=ss[:]), reads=["mxt"], writes=["mht", "mss"])
            P.op("dve", lambda e: e.tensor_scalar(rs[:], ss[:], 1.0 / D, 1e-5, ALU.mult, ALU.add), reads=["mss"], writes=["mrs"])
            P.op("act", lambda e: e.activation(out=rs[:], in_=rs[:], func=AF.Sqrt), reads=["mrs"], writes=["mrs"])
            P.op("dve", lambda e: e.reciprocal(rs[:], rs[:]), reads=["mrs"], writes=["mrs"])
            P.op("dve", lambda e: e.scalar_tensor_tensor(ht[:], xt[:], rs[:], A[:], ALU.mult, ALU.mult), reads=["mxt", "mrs", "mA"], writes=["mht"])
            P.op("dve", lambda e: e.tensor_tensor(ht[:], ht[:], Bt, ALU.add), reads=["mht", "hTc"], writes=["mht"])
            P.op("pool", lambda e, tt=tt: e.tensor_copy(hTok[:, tt, :], ht[:]), reads=["mht"], writes=["hTok"])
            if STOP <= 1:
                P.dma("sp", "mxo", lambda e, rows=rows: e.dma_start(out=xout[rows, :], in_=ht[:]), reads=["mht"], writes=["d_xout"])
                continue
            for q in range(4):
                for i in range(4):
                    k = q * 4 + i
                    P.op("pe", lambda e, k=k, i=i: e.transpose(ptr[:, i, :], ht[:, k * 128:(k + 1) * 128], ident[:]), reads=["mht", "ident"], writes=["mptr"])
                P.op("act", lambda e, q=q: e.copy(out=hTf[:, q * 4:(q + 1) * 4, :], in_=ptr[:]), reads=["mptr"], writes=["acc"])
                P.op("dve", lambda e, q=q, tt=tt: e.tensor_copy(hT[:, q * 4:(q + 1) * 4, tt * 128:(tt + 1) * 128], hTf[:, q * 4:(q + 1) * 4, :]), reads=["acc"], writes=["hTb"])
            if STOP <= 2:
                P.dma("sp", "mxo", lambda e, rows=rows: e.dma_start(out=xout[rows, :], in_=hTf[:].rearrange("p k t -> p (k t)")), reads=["acc", "hTb"], writes=["d_xout"])
                continue
            for k in range(16):
                P.op("pe", lambda e, k=k: e.matmul(plg, hTf[:, k, :], wr[:, k, :], start=(k == 0), stop=(k == 15)), reads=["acc", "wr"], writes=["mpsm"])
            P.op("dve", lambda e: e.tensor_tensor(lg[:], plg, brt[:], ALU.add), reads=["mpsm", "brt"], writes=["lg"])
            if STOP <= 3:
                P.dma("sp", "mxo", lambda e, rows=rows: e.dma_start(out=xout[rows, 0:32], in_=lg[:]), reads=["lg"], writes=["d_xout"])
                continue
            P.op("dve", lambda e: e.max(mx8[:], lg[:]), reads=["lg"], writes=["mx8"])
            P.op("dve", lambda e: e.tensor_scalar(msk[:], lg[:], mx8[:, 3:4], None, ALU.is_ge), reads=["lg", "mx8"], writes=["msk"])
            P.op("dve", lambda e, tt=tt: e.tensor_copy(Mall[:, tt, :], msk[:]), reads=["msk"], writes=["Mall"])
            P.op("dve", lambda e: e.tensor_scalar(nmx[:], mx8[:, 0:1], -1.0, None, ALU.mult), reads=["mx8"], writes=["nmx"])
            P.op("act", lambda e: e.activation(out=lg[:], in_=lg[:], func=AF.Exp, bias=nmx[:]), reads=["lg", "nmx"], writes=["lg"])
            P.op("dve", lambda e: e.tensor_tensor(lg[:], lg[:], msk[:], ALU.mult), reads=["lg", "msk"], writes=["lg"])
            P.op("dve", lambda e: e.reduce_sum(ssum[:], lg[:], AX.X), reads=["lg"], writes=["ssum"])
            P.op("dve", lambda e: e.reciprocal(ssum[:], ssum[:]), reads=["ssum"], writes=["ssum"])
            P.op("dve", lambda e, tt=tt: e.tensor_scalar(Gm[:, tt, :], lg[:], ssum[:], None, ALU.mult), reads=["lg", "ssum"], writes=["Gm"])
        if mode == 'route' and STOP <= 3:
            continue
        if mode == 'route':
            for tt in range(TP // 128):
                rows = slice(ps_ * TP + tt * 128, ps_ * TP + (tt + 1) * 128)
                P.dma("sp", "mxo", lambda e, rows=rows, tt=tt: e.dma_start(out=xout[rows, 0:32], in_=Gm[:, tt, :]), reads=["Gm"], writes=["d_xout"])
            continue
        P.op("pool", lambda e: e.memset(acc[:], 0.0), reads=["acc"], writes=["acc"])
        NTL = TP // 128
        for tt in range(NTL):
            for t2_ in range(tt):
                P.op("pe", lambda e, t2_=t2_: e.matmul(plg, onesf[:], Mall[:, t2_, :], start=(t2_ == 0), stop=False), reads=["onesf", "Mall"], writes=["mpsm"])
            P.op("pe", lambda e, tt=tt: e.matmul(plg, Lst[:], Mall[:, tt, :], start=(tt == 0), stop=True), reads=["Lst", "Mall"], writes=["mpsm"])
            P.op("dve", lambda e, tt=tt: e.tensor_copy(rank[:, tt, :], plg), reads=["mpsm"], writes=["rank"])
        for tt in range(NTL):
            P.op("pe", lambda e, tt=tt: e.matmul(plg, onesf[:], Mall[:, tt, :], start=(tt == 0), stop=(tt == NTL - 1)), reads=["onesf", "Mall"], writes=["mpsm"])
        P.op("dve", lambda e: e.tensor_copy(cnt_i[:], plg), reads=["mpsm"], writes=["cnt_i"])
        for ex in range(nexp):
            eb = ei % 2; ei += 1
            P.dma("sp", "bg32", lambda e, ex=ex: e.dma_start(out=bg32[:], in_=b_gu[l, ex, :].rearrange("(c p) -> c p", p=128)), writes=["bg32"])
            P.op("pe", lambda e: e.transpose(pbg, bg32[:], ident[:32, :32]), reads=["bg32", "ident"], writes=["mpsm"])
            P.op("act", lambda e, eb=eb: e.copy(out=bgT[eb][:], in_=pbg), reads=["mpsm"], writes=[f"bgT{eb}"])
            P.dma("pool", f"bd_b{eb}", lambda e, ex=ex, eb=eb: e.dma_start(out=bd_b[eb][:], in_=b_d[l, ex:ex + 1, :]), writes=[f"bd_b{eb}"])
            ctr = {"wi": wi, "di": di, "ui": ui, "pi": pi}

            def ffn(compact, ex=ex, eb=eb, ctr=ctr):
                wi = ctr["wi"]; di = ctr["di"]; ui = ctr["ui"]; pi = ctr["pi"]
                N = CAP if compact else TP
                hsrc = hTc if compact else hT
                if compact:
                    for tt in range(NTL):
                        P.op("dve", lambda e, tt=tt: e.tensor_scalar(Sm[:, tt, :], iota_c[:], rank[:, tt, ex:ex + 1], Mall[:, tt, ex:ex + 1], ALU.is_equal, ALU.mult),
                             reads=["iota_c", "rank", "Mall"], writes=["Sm"])
                        P.op("dve", lambda e, tt=tt: e.tensor_scalar(SGm[:, tt, :], Sm[:, tt, :], Gm[:, tt, ex:ex + 1], None, ALU.mult), reads=["Sm", "Gm"], writes=["SGm"])
                    ptb = ptr[:].rearrange("p a b -> p (a b)").bitcast(BF16).rearrange("p (a b) -> p a b", b=128)
                    for sb in range(CAP // 128):
                        for tt in range(NTL):
                            P.op("pe", lambda e, sb=sb, tt=tt: e.transpose(ptb[:, sb * NTL + tt, :], SGm[:, tt, sb * 128:(sb + 1) * 128], identb[:]), reads=["SGm", "identb"], writes=["mptr"])
                    P.op("act", lambda e: e.copy(out=STm[:].rearrange("p s t k -> p (s t) k"), in_=ptb[:, 0:(CAP // 128) * NTL, :]), reads=["mptr"], writes=["STm"])
                    for k2 in range(8):
                        u = ui % 2; ui += 1
                        for kk in range(2):
                            k = k2 * 2 + kk
                            for tt in range(NTL):
                                P.op("pe", lambda e, u=u, k=k, kk=kk, tt=tt: e.matmul(pg[u][:, kk * CAP:(kk + 1) * CAP], hTok[:, tt, k * 128:(k + 1) * 128], Sm[:, tt, :], start=(tt == 0), stop=(tt == NTL - 1)),
                                     reads=["hTok", "Sm"], writes=[f"pg{u}"])
                        P.op("act", lambda e, u=u, k2=k2: e.copy(out=hTc[:, k2 * 2:k2 * 2 + 2, :], in_=pg[u][:, 0:2 * CAP].rearrange("p (a b) -> p a b", a=2)), reads=[f"pg{u}"], writes=["hTc"])
                for c2 in range(8):
                    b = wi % 2; wi += 1
                    P.dma("pool", f"wg{b}", lambda e, b=b, c2=c2: e.dma_start(
                        out=wg[b][:], in_=w_gu[l, ex, :, c2 * 256:(c2 + 1) * 256].rearrange("(k p) n -> p k n", p=128)), writes=[f"wg{b}"])
                    P.dma("pool", f"wl{b}", lambda e, b=b, c2=c2: e.dma_start(
                        out=wl[b][:], in_=w_gu[l, ex, :, D + c2 * 256:D + (c2 + 1) * 256].rearrange("(k p) n -> p k n", p=128)), writes=[f"wl{b}"])
                    for sub in range(2):
                        u = ui % 2; ui += 1
                        ch = c2 * 2 + sub
                        hname = "hTc" if compact else "hTb"
                        for k in range(16):
                            P.op("pe", lambda e, u=u, b=b, k=k, sub=sub: e.matmul(pg[u][:, 0:N], wg[b][:, k, sub * 128:(sub + 1) * 128], hsrc[:, k, :], start=(k == 0), stop=(k == 15)),
                                 reads=[f"wg{b}", hname], writes=[f"pg{u}"])
                        for k in range(16):
                            P.op("pe", lambda e, u=u, b=b, k=k, sub=sub: e.matmul(pl[u][:, 0:N], wl[b][:, k, sub * 128:(sub + 1) * 128], hsrc[:, k, :], start=(k == 0), stop=(k == 15)),
                                 reads=[f"wl{b}", hname], writes=[f"pl{u}"])
                        P.op("dve", lambda e, u=u, ch=ch: e.tensor_scalar(t1[u][:, 0:N], pg[u][:, 0:N], bgT[eb][:, ch:ch + 1], 7.0, ALU.add, ALU.min),
                             reads=[f"pg{u}", f"bgT{eb}"], writes=[f"t1_{u}"])
                        P.op("act", lambda e, u=u: e.activation(out=sg[u][:, 0:N], in_=t1[u][:, 0:N], func=AF.Sigmoid, scale=1.702), reads=[f"t1_{u}"], writes=[f"sg_{u}"])
                        P.op("dve", lambda e, u=u, ch=ch: e.tensor_scalar(t2[u][:, 0:N], pl[u][:, 0:N], bgT[eb][:, 16 + ch:16 + ch + 1], 7.0, ALU.add, ALU.min),
                             reads=[f"pl{u}", f"bgT{eb}"], writes=[f"t2_{u}"])
                        P.op("dve", lambda e, u=u: e.tensor_scalar(t2[u][:, 0:N], t2[u][:, 0:N], -7.0, 1.0, ALU.max, ALU.add), reads=[f"t2_{u}"], writes=[f"t2_{u}"])
                        P.op("dve", lambda e, u=u: e.tensor_tensor(t1[u][:, 0:N], t1[u][:, 0:N], sg[u][:, 0:N], ALU.mult), reads=[f"t1_{u}", f"sg_{u}"], writes=[f"t1_{u}"])
                        P.op("dve", lambda e, u=u, ch=ch: e.tensor_tensor(actT[:, ch, 0:N], t1[u][:, 0:N], t2[u][:, 0:N], ALU.mult), reads=[f"t1_{u}", f"t2_{u}"], writes=["actT"])
                for cc in range(8):
                    b = di % 2; di += 1
                    P.dma("pool", f"wd{b}", lambda e, b=b, cc=cc: e.dma_start(
                        out=wd[b][:], in_=w_d[l, ex, :, cc * 256:(cc + 1) * 256].rearrange("(k p) n -> p k n", p=128)), writes=[f"wd{b}"])
                    ccs = slice(cc * 256, (cc + 1) * 256)
                    if not compact:
                        for tt in range(NTL):
                            u = pi % 2; pi += 1
                            for i in range(16):
                                P.op("pe", lambda e, u=u, b=b, i=i, tt=tt: e.matmul(pd[u][:, 0:256], actT[:, i, tt * 128:(tt + 1) * 128], wd[b][:, i, :], start=(i == 0), stop=False),
                                     reads=["actT", f"wd{b}"], writes=[f"pd{u}"])
                            P.op("pe", lambda e, u=u, ccs=ccs: e.matmul(pd[u][:, 0:256], ones_b[:], bd_b[eb][:, ccs], start=False, stop=True),
                                 reads=["ones_b", f"bd_b{eb}"], writes=[f"pd{u}"])
                            P.op("dve", lambda e, u=u, tt=tt, ccs=ccs: e.scalar_tensor_tensor(acc[:, tt, ccs], pd[u][:, 0:256], Gm[:, tt, ex:ex + 1], acc[:, tt, ccs], ALU.mult, ALU.add),
                                 reads=[f"pd{u}", "Gm", "acc"], writes=["acc"])
                    else:
                        for sb in range(CAP // 128):
                            u = pi % 2; pi += 1
                            for i in range(16):
                                P.op("pe", lambda e, u=u, b=b, i=i, sb=sb: e.matmul(pd[u][:, 0:256], actT[:, i, sb * 128:(sb + 1) * 128], wd[b][:, i, :], start=(i == 0), stop=False),
                                     reads=["actT", f"wd{b}"], writes=[f"pd{u}"])
                            P.op("pe", lambda e, u=u, ccs=ccs: e.matmul(pd[u][:, 0:256], ones_b[:], bd_b[eb][:, ccs], start=False, stop=True),
                                 reads=["ones_b", f"bd_b{eb}"], writes=[f"pd{u}"])
                            P.op("act", lambda e, u=u, sb=sb: e.copy(out=yc[:, sb, :], in_=pd[u][:, 0:256]), reads=[f"pd{u}"], writes=["yc"])
                        for tt in range(NTL):
                            u = ui % 2; ui += 1
                            for sb in range(CAP // 128):
                                P.op("pe", lambda e, u=u, sb=sb, tt=tt: e.matmul(pg[u][:, 0:256], STm[:, sb, tt, :], yc[:, sb, :], start=(sb == 0), stop=(sb == CAP // 128 - 1)),
                                     reads=["STm", "yc"], writes=[f"pg{u}"])
                            P.op("dve", lambda e, u=u, tt=tt, ccs=ccs: e.tensor_tensor(acc[:, tt, ccs], acc[:, tt, ccs], pg[u][:, 0:256], ALU.add), reads=[f"pg{u}", "acc"], writes=["acc"])
                ctr2 = {"wi": wi, "di": di, "ui": ui, "pi": pi}
                return ctr2

            res = {}
            def fa():
                res["a"] = ffn(True)
            def fb():
                res["b"] = ffn(False)
            if GUARD:
                P.guard(cnt_i[0:1, ex:ex + 1], CAP + 1, fa, fb, "cnt_i")
            else:
                fb()
                res["a"] = res["b"]
            wi = max(res["a"]["wi"], res["b"]["wi"]); di = max(res["a"]["di"], res["b"]["di"])
            ui = 0; pi = 0
        P.dma("sp", "m1", lambda e: e.dma_start(out=A[:], in_=modrow[l, 5 * D:6 * D].partition_broadcast(128)), reads=["d_modrow", "mA"], writes=["mA"])
        for tt in range(TP // 128):
            rows = slice(ps_ * TP + tt * 128, ps_ * TP + (tt + 1) * 128)
            P.dma("sp", "mxt", lambda e, rows=rows: e.dma_start(out=xt[:], in_=xin[rows, :]), writes=["mxt"])
            P.op("dve", lambda e, tt=tt: e.tensor_tensor(ht[:], acc[:, tt, :], A[:], ALU.mult), reads=["acc", "mA"], writes=["mht"])
            P.op("dve", lambda e: e.tensor_tensor(ht[:], ht[:], xt[:], ALU.add), reads=["mht", "mxt"], writes=["mht"])
            P.dma("sp", "mxo", lambda e, rows=rows: e.dma_start(out=xout[rows, :], in_=ht[:]), reads=["mht"], writes=["d_xout%d" % l])

TWO_PI = 2.0 * math.pi


def _wrap_sincos(P, frac_src, n, tagp, tmp_i, tmp_f, g, sin_out, cos_out, u, u_name=None, ti_name=None):
    u_name = u_name or (tagp + "u"); ti_name = ti_name or (tagp + "ti")
    P.op("dve", lambda e: e.tensor_copy(tmp_i, u), reads=[u_name], writes=[ti_name])
    P.op("dve", lambda e: e.tensor_copy(tmp_f, tmp_i), reads=[ti_name], writes=[tagp + "tf"])
    P.op("dve", lambda e: e.tensor_tensor(tmp_f, u, tmp_f, ALU.subtract), reads=[u_name, tagp + "tf"], writes=[tagp + "tf"])
    P.op("dve", lambda e: e.scalar_tensor_tensor(g, tmp_f, 0.5, tmp_f, ALU.is_gt, ALU.subtract), reads=[tagp + "tf"], writes=[tagp + "g"])
    P.op("dve", lambda e: e.scalar_tensor_tensor(tmp_f, g, 0.5, g, ALU.is_gt, ALU.subtract), reads=[tagp + "g"], writes=[tagp + "tf"])
    P.op("act", lambda e: e.activation(out=sin_out, in_=tmp_f, func=AF.Sin, scale=TWO_PI), reads=[tagp + "tf"], writes=[tagp + "sin"])
    P.op("dve", lambda e: e.tensor_scalar(tmp_f, tmp_f, 0.25, None, ALU.add), reads=[tagp + "tf", tagp + "sin"], writes=[tagp + "tf"])
    P.op("dve", lambda e: e.scalar_tensor_tensor(g, tmp_f, 0.5, tmp_f, ALU.is_gt, ALU.subtract), reads=[tagp + "tf"], writes=[tagp + "g"])
    P.op("act", lambda e: e.activation(out=cos_out, in_=g, func=AF.Sin, scale=-TWO_PI), reads=[tagp + "g"], writes=[tagp + "cos"])


def emit_ssm(P, nc, xin, xout, hbuf, modrow, norm_gain, lam_re, lam_im, log_dt, b_re, b_im, c_re, c_im, d_skip,
             w_a, b_a, w_b, b_b, debug_y=None):
    ident = P.ident
    m_pre = P.mark()
    A = P.sbuf("sA", [128, D], F32)
    Bt = P.sbuf("sB", [128, D], F32)
    xt = P.sbuf("sxt", [128, D], F32)
    ht = P.sbuf("sht", [128, D], F32)
    ss = P.sbuf("sss", [128, 1], F32)
    rs = P.sbuf("srs", [128, 1], F32)
    P.dma("sp", "s0", lambda e: e.dma_start(out=Bt[:], in_=modrow[1, 0:D].partition_broadcast(128)), reads=["d_modrow"], writes=["sB"])
    P.dma("sp", "s1", lambda e: e.dma_start(out=A[:], in_=modrow[1, D:2 * D].partition_broadcast(128)), reads=["d_modrow"], writes=["sA"])
    P.dma("sp", "s2", lambda e: e.dma_start(out=ht[:], in_=norm_gain[1, 0, :].partition_broadcast(128)), writes=["sht"])
    P.op("dve", lambda e: e.scalar_tensor_tensor(A[:], A[:], 1.0, ht[:], ALU.add, ALU.mult), reads=["sA", "sht"], writes=["sA"])
    for t in range(16):
        rows = slice(t * 128, (t + 1) * 128)
        P.dma("sp", "sxt", lambda e, rows=rows: e.dma_start(out=xt[:], in_=xin[rows, :]), writes=["sxt"])
        P.op("act", lambda e: e.activation(out=ht[:], in_=xt[:], func=AF.Square, accum_out=ss[:]), reads=["sxt"], writes=["sht", "sss"])
        P.op("dve", lambda e: e.tensor_scalar(rs[:], ss[:], 1.0 / D, 1e-5, ALU.mult, ALU.add), reads=["sss"], writes=["srs"])
        P.op("act", lambda e: e.activation(out=rs[:], in_=rs[:], func=AF.Sqrt), reads=["srs"], writes=["srs"])
        P.op("dve", lambda e: e.reciprocal(rs[:], rs[:]), reads=["srs"], writes=["srs"])
        P.op("dve", lambda e: e.scalar_tensor_tensor(ht[:], xt[:], rs[:], A[:], ALU.mult, ALU.mult), reads=["sxt", "srs", "sA"], writes=["sht"])
        P.op("dve", lambda e: e.tensor_tensor(ht[:], ht[:], Bt[:], ALU.add), reads=["sht", "sB"], writes=["sht"])
        P.dma("sp", "shb", lambda e, rows=rows: e.dma_start(out=hbuf[rows, :], in_=ht[:]), reads=["sht"], writes=["d_hbuf"])
    P.barrier_all()
    P.release(m_pre)

    ptr = P.psum("sptr", [128, 4, 128], F32)
    pxr = [P.psum(f"pxr{i}", [128, 512], F32) for i in range(2)]
    pxi = [P.psum(f"pxi{i}", [128, 512], F32) for i in range(2)]
    pyc = [P.psum(f"pyc{i}", [128, 512], F32) for i in range(2)]

    l64 = P.sbuf("l64", [64, 3, 128], F32)
    dt2 = P.sbuf("dt2", [64, 2], F32)
    P.dma("sp", "p0", lambda e: e.dma_start(out=l64[:, 0, :], in_=lam_re[0].rearrange("(j a) p -> j (a p)", a=2)), writes=["l64"])
    P.dma("sp", "p1", lambda e: e.dma_start(out=l64[:, 1, :], in_=lam_im[0].rearrange("(j a) p -> j (a p)", a=2)), writes=["l64"])
    P.dma("sp", "p2", lambda e: e.dma_start(out=dt2[:], in_=log_dt[0].rearrange("(j a) -> j a", a=2)), writes=["dt2"])
    P.op("dve", lambda e: e.tensor_copy(l64[:, 2, 0:64], dt2[:, 0:1].to_broadcast([64, 64])), reads=["dt2", "l64"], writes=["l64"])
    P.op("dve", lambda e: e.tensor_copy(l64[:, 2, 64:128], dt2[:, 1:2].to_broadcast([64, 64])), reads=["dt2", "l64"], writes=["l64"])
    prm = P.sbuf("prm", [128, 3, 64], F32)
    for i in range(3):
        P.op("pe", lambda e, i=i: e.transpose(ptr[:, i, 0:64], l64[:, i, :], ident[:64, :64]), reads=["l64", "ident"], writes=["sptr"])
    P.op("act", lambda e: e.copy(out=prm[:], in_=ptr[:, 0:3, 0:64]), reads=["sptr"], writes=["prm"])
    lr = prm[:, 0, :]; li = prm[:, 1, :]
    q = P.sbuf("q", [128, 12, 64], F32)
    qi = P.sbuf("qi", [128, 64], I32)
    dtc, rr, thf, sn, cs, abr, abi, den, fre, fim, tA, tB = [q[:, i, :] for i in range(12)]
    P.op("act", lambda e: e.activation(out=dtc, in_=prm[:, 2, :], func=AF.Exp), reads=["prm"], writes=["q_dt"])
    P.op("dve", lambda e: e.tensor_tensor(rr, lr, dtc, ALU.mult), reads=["prm", "q_dt"], writes=["q_r"])
    P.op("act", lambda e: e.activation(out=rr, in_=rr, func=AF.Exp), reads=["q_r"], writes=["q_r"])
    P.op("dve", lambda e: e.scalar_tensor_tensor(thf, li, 1.0 / TWO_PI, dtc, ALU.mult, ALU.mult), reads=["prm", "q_dt"], writes=["pp_u"])
    _wrap_sincos(P, None, 64, "pp_", qi[:], tA, tB, sn, cs, thf)
    P.op("dve", lambda e: e.tensor_tensor(abr, rr, cs, ALU.mult), reads=["q_r", "pp_cos"], writes=["q_abr"])
    P.op("dve", lambda e: e.tensor_tensor(abi, rr, sn, ALU.mult), reads=["q_r", "pp_sin"], writes=["q_abi"])
    P.op("dve", lambda e: e.tensor_tensor(den, lr, lr, ALU.mult), reads=["prm"], writes=["q_den"])
    P.op("dve", lambda e: e.tensor_tensor(tA, li, li, ALU.mult), reads=["prm", "pp_tf"], writes=["pp_tf"])
    P.op("dve", lambda e: e.tensor_tensor(den, den, tA, ALU.add), reads=["q_den", "pp_tf"], writes=["q_den"])
    P.op("dve", lambda e: e.reciprocal(den, den), reads=["q_den"], writes=["q_den"])
    P.op("dve", lambda e: e.tensor_scalar(abr, abr, -1.0, None, ALU.add), reads=["q_abr"], writes=["q_abr"])
    P.op("dve", lambda e: e.tensor_tensor(tA, abr, lr, ALU.mult), reads=["q_abr", "prm", "pp_tf"], writes=["pp_tf"])
    P.op("dve", lambda e: e.tensor_tensor(tB, abi, li, ALU.mult), reads=["q_abi", "prm", "pp_g"], writes=["pp_g"])
    P.op("dve", lambda e: e.tensor_tensor(tA, tA, tB, ALU.add), reads=["pp_tf", "pp_g"], writes=["pp_tf"])
    P.op("dve", lambda e: e.tensor_tensor(fre, tA, den, ALU.mult), reads=["pp_tf", "q_den"], writes=["q_fre"])
    P.op("dve", lambda e: e.tensor_tensor(tA, abi, lr, ALU.mult), reads=["q_abi", "prm", "pp_tf"], writes=["pp_tf"])
    P.op("dve", lambda e: e.tensor_tensor(tB, abr, li, ALU.mult), reads=["q_abr", "prm", "pp_g"], writes=["pp_g"])
    P.op("dve", lambda e: e.tensor_tensor(tA, tA, tB, ALU.subtract), reads=["pp_tf", "pp_g"], writes=["pp_tf"])
    P.op("dve", lambda e: e.tensor_tensor(fim, tA, den, ALU.mult), reads=["pp_tf", "q_den"], writes=["q_fim"])
    P.op("dve", lambda e: e.scalar_tensor_tensor(thf, li, 1.0 / TWO_PI, dtc, ALU.mult, ALU.mult), reads=["prm", "q_dt", "pp_u"], writes=["pp_u"])

    Bre = P.sbuf("Bre", [128, 64, 16], F32)
    Bim = P.sbuf("Bim", [128, 64, 16], F32)
    bbr = P.sbuf("bbr", [128, 64, 16], F32)
    bbi = P.sbuf("bbi", [128, 64, 16], F32)
    for a in range(2):
        P.dma("sp", f"pb{a}", lambda e, a=a: e.dma_start(out=Bre[a * 64:(a + 1) * 64, :, :], in_=b_re[0].rearrange("(j a) p c -> a p j c", a=2)[a]), writes=["Bre"])
        P.dma("sp", f"pc{a}", lambda e, a=a: e.dma_start(out=Bim[a * 64:(a + 1) * 64, :, :], in_=b_im[0].rearrange("(j a) p c -> a p j c", a=2)[a]), writes=["Bim"])
    freb = fre.unsqueeze(2).to_broadcast([128, 64, 16])
    fimb = fim.unsqueeze(2).to_broadcast([128, 64, 16])
    P.op("dve", lambda e: e.tensor_tensor(bbr[:], Bre[:], freb, ALU.mult), reads=["Bre", "q_fre"], writes=["bbr"])
    P.op("dve", lambda e: e.tensor_tensor(bbi[:], Bim[:], fimb, ALU.mult), reads=["Bim", "q_fim"], writes=["bbi"])
    P.op("dve", lambda e: e.tensor_tensor(bbr[:], bbr[:], bbi[:], ALU.subtract), reads=["bbr", "bbi"], writes=["bbr"])
    P.op("dve", lambda e: e.tensor_tensor(bbi[:], Bre[:], fimb, ALU.mult), reads=["Bre", "q_fim", "bbi"], writes=["bbi"])
    P.op("dve", lambda e: e.tensor_tensor(Bre[:], Bim[:], freb, ALU.mult), reads=["Bim", "q_fre", "Bre"], writes=["Bre"])
    P.op("dve", lambda e: e.tensor_tensor(bbi[:], bbi[:], Bre[:], ALU.add), reads=["bbi", "Bre"], writes=["bbi"])

    d16 = P.sbuf("d16", [16, 128], F32)
    dT = P.sbuf("dT", [128, 16], F32)
    P.dma("sp", "p3", lambda e: e.dma_start(out=d16[:], in_=d_skip[0].rearrange("(k p) -> k p", p=128)), writes=["d16"])
    P.op("pe", lambda e: e.transpose(ptr[:, 3, 0:16], d16[:], ident[:16, :16]), reads=["d16", "ident"], writes=["sptr"])
    P.op("act", lambda e: e.copy(out=dT[:], in_=ptr[:, 3, 0:16]), reads=["sptr"], writes=["dT"])

    iot = P.sbuf("iot", [128, S], F32)
    P.op("pool", lambda e: e.iota(iot[:], [[1, S]], base=0, channel_multiplier=0, allow_small_or_imprecise_dtypes=True), writes=["iot"])

    XR = P.sbuf("XR", [128, 4, 128], F32)
    XI = P.sbuf("XI", [128, 4, 128], F32)
    BBr = P.sbuf("BBr", [128, 4, 128], F32)
    BBi = P.sbuf("BBi", [128, 4, 128], F32)
    CCr = P.sbuf("CCr", [128, 4, 128], F32)
    CCi = P.sbuf("CCi", [128, 4, 128], F32)
    for tname, tt_ in (("XR", XR), ("XI", XI), ("CCr", CCr), ("CCi", CCi)):
        P.op("pool", lambda e, tt_=tt_: e.memset(tt_[:], 0.0), writes=[tname])
    Cld = P.sbuf("Cld", [128, 2, 2, 64], F32)
    Td = P.sbuf("Td", [128, 2, 128], F32)
    hld = P.sbuf("hld", [128, 16, 128], F32)
    uT = P.sbuf("uT", [128, S], F32)
    yacc = P.sbuf("yacc", [128, S], F32)
    ygT = P.sbuf("ygT", [128, 16, S], BF16)
    cosT = P.sbuf("cosT", [128, S], F32)
    sinT = P.sbuf("sinT", [128, S], F32)
    tf_ = P.sbuf("tf_", [128, S], F32)
    gg = P.sbuf("gg", [128, S], F32)
    wre = P.sbuf("wre", [128, S], F32)
    wim = P.sbuf("wim", [128, S], F32)
    qre = P.sbuf("qre", [128, S], F32)
    qim = P.sbuf("qim", [128, S], F32)

    bi = 0; ci = 0
    for jk in range(16):
        for jj in range(4):
            j = jk * 4 + jj
            r0 = jj * 32
            P.op("dve", lambda e, j=j, jj=jj, r0=r0: e.tensor_copy(XR[0:64, jj, r0:r0 + 16], bbr[0:64, j, :]), reads=["bbr", "XR"], writes=["XR"])
            P.op("dve", lambda e, j=j, jj=jj, r0=r0: e.tensor_copy(XR[64:128, jj, r0 + 16:r0 + 32], bbr[64:128, j, :]), reads=["bbr", "XR"], writes=["XR"])
            P.op("dve", lambda e, j=j, jj=jj, r0=r0: e.tensor_copy(XI[0:64, jj, r0:r0 + 16], bbi[0:64, j, :]), reads=["bbi", "XI"], writes=["XI"])
            P.op("dve", lambda e, j=j, jj=jj, r0=r0: e.tensor_copy(XI[64:128, jj, r0 + 16:r0 + 32], bbi[64:128, j, :]), reads=["bbi", "XI"], writes=["XI"])
        for jj in range(4):
            P.op("pe", lambda e, jj=jj: e.transpose(ptr[:, jj, :], XR[:, jj, :], ident[:]), reads=["XR", "ident"], writes=["sptr"])
        P.op("act", lambda e: e.copy(out=BBr[:], in_=ptr[:]), reads=["sptr"], writes=["BBr"])
        for jj in range(4):
            P.op("pe", lambda e, jj=jj: e.transpose(ptr[:, jj, :], XI[:, jj, :], ident[:]), reads=["XI", "ident"], writes=["sptr"])
        P.op("act", lambda e: e.copy(out=BBi[:], in_=ptr[:]), reads=["sptr"], writes=["BBi"])
        crows = slice(jk * 128, (jk + 1) * 128)
        for dup in range(2):
            P.dma("sp", "cld", lambda e, dup=dup, crows=crows: e.dma_start(out=Cld[:, 0, dup, :], in_=c_re[0].rearrange("g c p -> (g c) p")[crows, :]), writes=["Cld"])
            P.dma("sp", "cld", lambda e, dup=dup, crows=crows: e.dma_start(out=Cld[:, 1, dup, :], in_=c_im[0].rearrange("g c p -> (g c) p")[crows, :]), writes=["Cld"])
        for ri in range(2):
            P.op("pe", lambda e, ri=ri: e.transpose(ptr[:, ri, :], Cld[:, ri, :, :].rearrange("p d q -> p (d q)"), ident[:]), reads=["Cld", "ident"], writes=["sptr"])
        P.op("act", lambda e: e.copy(out=Td[:], in_=ptr[:, 0:2, :]), reads=["sptr"], writes=["Td"])
        for jj in range(4):
            r0 = jj * 32
            P.op("dve", lambda e, jj=jj, r0=r0: e.tensor_copy(CCr[0:64, jj, r0:r0 + 16], Td[0:64, 0, r0:r0 + 16]), reads=["Td", "CCr"], writes=["CCr"])
            P.op("dve", lambda e, jj=jj, r0=r0: e.tensor_copy(CCr[64:128, jj, r0 + 16:r0 + 32], Td[64:128, 0, r0 + 16:r0 + 32]), reads=["Td", "CCr"], writes=["CCr"])
            P.op("dve", lambda e, jj=jj, r0=r0: e.tensor_scalar(CCi[0:64, jj, r0:r0 + 16], Td[0:64, 1, r0:r0 + 16], -1.0, None, ALU.mult), reads=["Td", "CCi"], writes=["CCi"])
            P.op("dve", lambda e, jj=jj, r0=r0: e.tensor_scalar(CCi[64:128, jj, r0 + 16:r0 + 32], Td[64:128, 1, r0 + 16:r0 + 32], -1.0, None, ALU.mult), reads=["Td", "CCi"], writes=["CCi"])
        P.dma("sp", "hld", lambda e, jk=jk: e.dma_start(out=hld[:], in_=hbuf[:, jk * 128:(jk + 1) * 128].rearrange("(t p) f -> p t f", p=128)), reads=["d_hbuf"], writes=["hld"])
        for qd in range(4):
            for i in range(4):
                t = qd * 4 + i
                P.op("pe", lambda e, t=t, i=i: e.transpose(ptr[:, i, :], hld[:, t, :], ident[:]), reads=["hld", "ident"], writes=["sptr"])
            P.op("act", lambda e, qd=qd: e.copy(out=uT[:, qd * 512:(qd + 1) * 512], in_=ptr[:].rearrange("p a b -> p (a b)")), reads=["sptr"], writes=["uT"])
        P.op("dve", lambda e, jk=jk: e.tensor_scalar(yacc[:], uT[:], dT[:, jk:jk + 1], None, ALU.mult), reads=["uT", "dT"], writes=["yacc"])
        for jj in range(4):
            j = jk * 4 + jj
            r0 = jj * 32
            P.op("dve", lambda e, j=j: e.tensor_scalar(qre[:], iot[:], thf[:, j:j + 1], None, ALU.mult), reads=["iot", "pp_u"], writes=["qre"])
            _wrap_sincos(P, None, S, "tt_", qim[:].bitcast(I32), tf_[:], gg[:], sinT[:], cosT[:], qre[:], u_name="qre", ti_name="qim")
            for n in range(4):
                b = bi % 2; bi += 1
                ns = slice(n * 512, (n + 1) * 512)
                P.op("pe", lambda e, b=b, jj=jj, r0=r0, ns=ns: e.matmul(pxr[b][:], BBr[min(r0, 64):r0 + 32, jj, :], uT[min(r0, 64):r0 + 32, ns], start=True, stop=True),
                     reads=["BBr", "uT"], writes=[f"pxr{b}"])
                P.op("pe", lambda e, b=b, jj=jj, r0=r0, ns=ns: e.matmul(pxi[b][:], BBi[min(r0, 64):r0 + 32, jj, :], uT[min(r0, 64):r0 + 32, ns], start=True, stop=True),
                     reads=["BBi", "uT"], writes=[f"pxi{b}"])
                P.op("dve", lambda e, b=b, ns=ns: e.tensor_tensor(wre[:, ns], pxr[b][:], cosT[:, ns], ALU.mult), reads=[f"pxr{b}", "tt_cos"], writes=["wre"])
                P.op("dve", lambda e, b=b, ns=ns: e.tensor_tensor(gg[:, ns], pxi[b][:], sinT[:, ns], ALU.mult), reads=[f"pxi{b}", "tt_sin", "tt_g"], writes=["tt_g"])
                P.op("dve", lambda e, ns=ns: e.tensor_tensor(wre[:, ns], wre[:, ns], gg[:, ns], ALU.add), reads=["wre", "tt_g"], writes=["wre"])
                P.op("dve", lambda e, b=b, ns=ns: e.tensor_tensor(wim[:, ns], pxi[b][:], cosT[:, ns], ALU.mult), reads=[f"pxi{b}", "tt_cos"], writes=["wim"])
                P.op("dve", lambda e, b=b, ns=ns: e.tensor_tensor(gg[:, ns], pxr[b][:], sinT[:, ns], ALU.mult), reads=[f"pxr{b}", "tt_sin", "tt_g"], writes=["tt_g"])
                P.op("dve", lambda e, ns=ns: e.tensor_tensor(wim[:, ns], wim[:, ns], gg[:, ns], ALU.subtract), reads=["wim", "tt_g"], writes=["wim"])
            rb = rr[:, j:j + 1].to_broadcast([128, S])
            P.op("dve", lambda e, rb=rb: e.tensor_tensor_scan(qre[:], rb, wre[:], 0.0, ALU.mult, ALU.add), reads=["wre", "q_r"], writes=["qre"])
            P.op("dve", lambda e, rb=rb: e.tensor_tensor_scan(qim[:], rb, wim[:], 0.0, ALU.mult, ALU.add), reads=["wim", "q_r"], writes=["qim"])
            P.op("dve", lambda e: e.tensor_tensor(wre[:], qre[:], cosT[:], ALU.mult), reads=["qre", "tt_cos", "wre"], writes=["wre"])
            P.op("dve", lambda e: e.tensor_tensor(gg[:], qim[:], sinT[:], ALU.mult), reads=["qim", "tt_sin", "tt_g"], writes=["tt_g"])
            P.op("dve", lambda e: e.tensor_tensor(wre[:], wre[:], gg[:], ALU.subtract), reads=["wre", "tt_g"], writes=["wre"])
            P.op("dve", lambda e: e.tensor_tensor(wim[:], qre[:], sinT[:], ALU.mult), reads=["qre", "tt_sin", "wim"], writes=["wim"])
            P.op("dve", lambda e: e.tensor_tensor(gg[:], qim[:], cosT[:], ALU.mult), reads=["qim", "tt_cos", "tt_g"], writes=["tt_g"])
            P.op("dve", lambda e: e.tensor_tensor(wim[:], wim[:], gg[:], ALU.add), reads=["wim", "tt_g"], writes=["wim"])
            for n in range(4):
                c = ci % 2; ci += 1
                ns = slice(n * 512, (n + 1) * 512)
                P.op("pe", lambda e, c=c, jj=jj, ns=ns: e.matmul(pyc[c][:], CCr[:, jj, :], wre[:, ns], start=True, stop=False), reads=["CCr", "wre"], writes=[f"pyc{c}"])
                P.op("pe", lambda e, c=c, jj=jj, ns=ns: e.matmul(pyc[c][:], CCi[:, jj, :], wim[:, ns], start=False, stop=True), reads=["CCi", "wim"], writes=[f"pyc{c}"])
                P.op("dve", lambda e, c=c, ns=ns: e.tensor_tensor(yacc[:, ns], yacc[:, ns], pyc[c][:], ALU.add), reads=["yacc", f"pyc{c}"], writes=["yacc"])
        if debug_y is not None:
            P.dma("sp", "dbg", lambda e, jk=jk: e.dma_start(out=debug_y[jk * 128:(jk + 1) * 128, :], in_=yacc[:]), reads=["yacc"], writes=["d_dbg"])
        P.op("dve", lambda e: e.tensor_tensor(gg[:], yacc[:], yacc[:], ALU.mult), reads=["yacc", "tt_g"], writes=["tt_g"])
        P.op("dve", lambda e: e.tensor_scalar(gg[:], gg[:], 0.044715, 1.0, ALU.mult, ALU.add), reads=["tt_g"], writes=["tt_g"])
        P.op("dve", lambda e: e.tensor_tensor(gg[:], gg[:], yacc[:], ALU.mult), reads=["tt_g", "yacc"], writes=["tt_g"])
        P.op("act", lambda e: e.activation(out=gg[:], in_=gg[:], func=AF.Tanh, scale=0.7978845608028654), reads=["tt_g"], writes=["tt_g"])
        P.op("dve", lambda e: e.tensor_scalar(gg[:], gg[:], 1.0, 0.5, ALU.add, ALU.mult), reads=["tt_g"], writes=["tt_g"])
        P.op("dve", lambda e, jk=jk: e.tensor_tensor(ygT[:, jk, :], gg[:], yacc[:], ALU.mult), reads=["tt_g", "yacc"], writes=["ygT"])

    P.barrier_all()
    G1 = wre; ba_t = wim; bb_t = gg; xcol = [cosT, sinT]; ocol = [qre, qim]
    P.dma("sp", "g0", lambda e: e.dma_start(out=G1[:], in_=modrow[1, 2 * D:3 * D].partition_broadcast(128)), reads=["d_modrow", "wre"], writes=["wre"])
    P.dma("sp", "g1", lambda e: e.dma_start(out=ba_t[:], in_=b_a[0, :].partition_broadcast(128)), reads=["wim"], writes=["wim"])
    P.dma("sp", "g2", lambda e: e.dma_start(out=bb_t[:], in_=b_b[0, :].partition_broadcast(128)), reads=["tt_g"], writes=["tt_g"])
    wa = [tf_[:].bitcast(BF16).rearrange("p (k n) -> p k n", k=16)]
    wb = [iot[:].bitcast(BF16).rearrange("p (k n) -> p k n", k=16)]
    gi = 0
    CW = 256
    for cb in range(D // CW):
        cs_ = slice(cb * CW, (cb + 1) * CW)
        P.dma("pool", "wa0", lambda e, cs_=cs_: e.dma_start(out=wa[0], in_=w_a[0, :, cs_].rearrange("(k p) n -> p k n", p=128)), writes=["wa0"])
        P.dma("pool", "wb0", lambda e, cs_=cs_: e.dma_start(out=wb[0], in_=w_b[0, :, cs_].rearrange("(k p) n -> p k n", p=128)), writes=["wb0"])
        for t in range(16):
            b = gi % 2; gi += 1
            rows = slice(t * 128, (t + 1) * 128)
            xc = xcol[b]; oc = ocol[b]
            P.dma("sp", f"gx{b}", lambda e, rows=rows, cs_=cs_, xc=xc: e.dma_start(out=xc[:, 0:CW], in_=xin[rows, cs_]), reads=[f"xc{b}"], writes=[f"xc{b}"])
            for k in range(16):
                P.op("pe", lambda e, b=b, k=k, rows=rows: e.matmul(pxr[b][:, 0:CW], ygT[:, k, rows], wa[0][:, k, :], start=(k == 0), stop=(k == 15)), reads=["ygT", "wa0"], writes=[f"pxr{b}"])
            for k in range(16):
                P.op("pe", lambda e, b=b, k=k, rows=rows: e.matmul(pxi[b][:, 0:CW], ygT[:, k, rows], wb[0][:, k, :], start=(k == 0), stop=(k == 15)), reads=["ygT", "wb0"], writes=[f"pxi{b}"])
            P.op("dve", lambda e, b=b, cs_=cs_, oc=oc: e.tensor_tensor(oc[:, 512:512 + CW], pxi[b][:, 0:CW], bb_t[:, cs_], ALU.add), reads=[f"pxi{b}", "tt_g"], writes=[f"oc{b}"])
            P.op("act", lambda e, oc=oc, b=b: e.activation(out=oc[:, 512:512 + CW], in_=oc[:, 512:512 + CW], func=AF.Sigmoid), reads=[f"oc{b}"], writes=[f"oc{b}"])
            P.op("dve", lambda e, b=b, cs_=cs_, oc=oc: e.tensor_tensor(oc[:, 0:CW], pxr[b][:, 0:CW], ba_t[:, cs_], ALU.add), reads=[f"pxr{b}", "wim", f"oc{b}"], writes=[f"oc{b}"])
            P.op("dve", lambda e, oc=oc, b=b: e.tensor_tensor(oc[:, 0:CW], oc[:, 0:CW], oc[:, 512:512 + CW], ALU.mult), reads=[f"oc{b}"], writes=[f"oc{b}"])
            P.op("dve", lambda e, oc=oc, b=b, cs_=cs_: e.tensor_tensor(oc[:, 0:CW], oc[:, 0:CW], G1[:, cs_], ALU.mult), reads=[f"oc{b}", "wre"], writes=[f"oc{b}"])
            P.op("dve", lambda e, oc=oc, xc=xc, b=b: e.tensor_tensor(oc[:, 0:CW], oc[:, 0:CW], xc[:, 0:CW], ALU.add), reads=[f"oc{b}", f"xc{b}"], writes=[f"oc{b}"])
            P.dma("sp", f"go{b}", lambda e, rows=rows, cs_=cs_, oc=oc: e.dma_start(out=xout[rows, cs_], in_=oc[:, 0:CW]), reads=[f"oc{b}"], writes=["d_xout_ssm"])


_REP = ["norm_gain", "ada_w", "ada_b", "attn_w_qkv", "attn_b_qkv", "attn_q_gain", "attn_k_gain", "attn_sinks",
        "attn_w_o", "attn_b_o", "ssm_lam_re", "ssm_lam_im", "ssm_log_dt", "ssm_b_re", "ssm_b_im", "ssm_c_re",
        "ssm_c_im", "ssm_d", "ssm_w_glu_a", "ssm_b_glu_a", "ssm_w_glu_b", "ssm_b_glu_b", "moe_w_router",
        "moe_b_router", "moe_w_gate_up", "moe_b_gate_up", "moe_w_down", "moe_b_down"]
_SHAPES = {"norm_gain": [2, 2, D], "ada_w": [2, D, 6 * D], "ada_b": [2, 6 * D], "attn_w_qkv": [1, D, QKV],
           "attn_b_qkv": [1, QKV], "attn_q_gain": [1, 64], "attn_k_gain": [1, 64], "attn_sinks": [1, 32],
           "attn_w_o": [1, D, D], "attn_b_o": [1, D], "ssm_lam_re": [1, 128, 64], "ssm_lam_im": [1, 128, 64],
           "ssm_log_dt": [1, 128], "ssm_b_re": [1, 128, 64, 16], "ssm_b_im": [1, 128, 64, 16],
           "ssm_c_re": [1, 128, 16, 64], "ssm_c_im": [1, 128, 16, 64], "ssm_d": [1, D], "ssm_w_glu_a": [1, D, D],
           "ssm_b_glu_a": [1, D], "ssm_w_glu_b": [1, D, D], "ssm_b_glu_b": [1, D], "moe_w_router": [2, D, 32],
           "moe_b_router": [2, 32], "moe_w_gate_up": [2, 32, D, 2 * D], "moe_b_gate_up": [2, 32, 2 * D],
           "moe_w_down": [2, 32, D, D], "moe_b_down": [2, 32, D]}


def build_full():
    nc = bass.Bass("TRN2", target_bir_lowering=False)
    dt = lambda name, shape, kind="ExternalInput": nc.dram_tensor(name, list(shape), F32, kind=kind).ap()
    x = dt("x", [S, D]); c = dt("c", [D]); biasmask = dt("biasmask", [32, 128, 256])
    a = {n: dt(n, _SHAPES[n]) for n in _REP}
    out = dt("out", [S, D], kind="ExternalOutput")
    scr = lambda name, shape: nc.dram_tensor(name, list(shape), F32, kind="Internal").ap()
    modrow = scr("modrow", [2, 6 * D]); xa0 = scr("xa0", [S, D]); xb0 = scr("xb0", [S, D]); xa1 = scr("xa1", [S, D]); hbuf = scr("hbuf", [S, D])
    P = Prog(nc)
    P.ident = P.sbuf("ident", [128, 128], F32)
    P.op("pool", lambda e: e.memset(P.ident[:], 1.0), writes=["ident"])
    P.op("pool", lambda e: e.affine_select(P.ident[:], P.ident[:], [[-1, 128]], ALU.is_equal, 0.0, base=0, channel_multiplier=1), reads=["ident"], writes=["ident"])
    P.barrier_all()
    m0 = P.mark()
    emit_adaln(P, nc, c, a["ada_w"], a["ada_b"], modrow)
    P.barrier_all(); P.release(m0)
    emit_attn(P, nc, x, xa0, modrow, a["norm_gain"], a["attn_w_qkv"], a["attn_b_qkv"], a["attn_q_gain"], a["attn_k_gain"],
              a["attn_sinks"], a["attn_w_o"], a["attn_b_o"], biasmask)
    P.barrier_all(); P.release(m0)
    P.prefix = 'm0_'
    emit_moe(P, nc, 0, xa0, xb0, modrow, a["norm_gain"], a["moe_w_router"], a["moe_b_router"], a["moe_w_gate_up"],
             a["moe_b_gate_up"], a["moe_w_down"], a["moe_b_down"])
    P.barrier_all(); P.release(m0)
    P.prefix = 's_'
    emit_ssm(P, nc, xb0, xa1, hbuf, modrow, a["norm_gain"], a["ssm_lam_re"], a["ssm_lam_im"], a["ssm_log_dt"], a["ssm_b_re"],
             a["ssm_b_im"], a["ssm_c_re"], a["ssm_c_im"], a["ssm_d"], a["ssm_w_glu_a"], a["ssm_b_glu_a"], a["ssm_w_glu_b"], a["ssm_b_glu_b"])
    P.barrier_all(); P.release(m0)
    P.prefix = 'm1_'
    emit_moe(P, nc, 1, xa1, out, modrow, a["norm_gain"], a["moe_w_router"], a["moe_b_router"], a["moe_w_gate_up"],
             a["moe_b_gate_up"], a["moe_w_down"], a["moe_b_down"])
    P.barrier_all()
    P.emit()
    P.close()
    return nc


def kernel(**inputs):
    n_cores = int(inputs.pop("_n_cores", 8)) if "_n_cores" in inputs else 8
    x = np.asarray(inputs["x"], dtype=np.float32)
    c = np.asarray(inputs["c"], dtype=np.float32)
    base = {n: np.ascontiguousarray(np.asarray(inputs[n], dtype=np.float32)) for n in _REP}
    base["biasmask"] = make_biasmask(np.asarray(inputs["rel_bias"], dtype=np.float32))
    nc = build_full()
    in_maps = [dict(base, x=np.ascontiguousarray(x[b]), c=np.ascontiguousarray(c[b])) for b in range(n_cores)]
    res = run_bass_kernel_spmd(nc, in_maps, core_ids=list(range(n_cores)))
    return np.stack([res.results[b]["out"] for b in range(n_cores)], axis=0).astype(np.float32)
```

```python
import math
import copy
import numpy as np
import concourse.bass as bass
import concourse.mybir as mybir
from concourse.bass_utils import run_bass_kernel_spmd

F32 = mybir.dt.float32
F32R = mybir.dt.float32r
I32 = mybir.dt.int32
U32 = mybir.dt.uint32
ALU = mybir.AluOpType
AF = mybir.ActivationFunctionType
AX = mybir.AxisListType

ENGS = ("pe", "act", "dve", "pool", "sp")
SAME_ENGINE_SYNC = {"pe": False, "act": True, "dve": True, "pool": True, "sp": False}


class Prog:
    def __init__(self, nc):
        self.nc = nc
        self.items = {e: [] for e in ENGS}
        self.count = {e: 0 for e in ENGS}
        self.sem = {}
        self.waited = {e: {} for e in ENGS}
        self.last_write = {}
        self.reads_since = {}
        self.dma_count = {}
        self.ctx = []
        for e in ENGS:
            self.sem[e] = self._newsem("eng_" + e)

    def _newsem(self, name):
        cm = self.nc.semaphore(name)
        s = cm.__enter__()
        self.ctx.append(cm)
        return s

    def sbuf(self, name, shape, dt):
        cm = self.nc.sbuf_tensor(getattr(self, 'prefix', '') + name, list(shape), dt)
        t = cm.__enter__()
        self.ctx.append(cm)
        return t

    def psum(self, name, shape, dt):
        cm = self.nc.psum_tensor(getattr(self, 'prefix', '') + name, list(shape), dt)
        t = cm.__enter__()
        self.ctx.append(cm)
        return t

    def _deps(self, reads, writes):
        deps = {}
        def add(k, v):
            if deps.get(k, 0) < v:
                deps[k] = v
        for r in reads:
            if r in self.last_write:
                add(*self.last_write[r])
        for w in writes:
            if w in self.last_write:
                add(*self.last_write[w])
            for k, v in self.reads_since.get(w, {}).items():
                add(k, v)
        return deps

    def _emit_waits(self, eng, deps):
        for k, v in deps.items():
            if k == eng and not SAME_ENGINE_SYNC[eng]:
                continue
            if self.waited[eng].get(k, 0) >= v:
                continue
            self.waited[eng][k] = v
            self.items[eng].append(("wait", self.sem[k], v))

    def _record(self, key, val, reads, writes):
        for r in reads:
            self.reads_since.setdefault(r, {})
            if self.reads_since[r].get(key, 0) < val:
                self.reads_since[r][key] = val
        for w in writes:
            self.last_write[w] = (key, val)
            self.reads_since[w] = {}

    def op(self, eng, fn, reads=(), writes=()):
        deps = self._deps(reads, writes)
        self._emit_waits(eng, deps)
        self.count[eng] += 1
        self.items[eng].append(("op", fn, self.sem[eng], 1))
        self._record(eng, self.count[eng], reads, writes)

    def dma(self, eng, chan, fn, reads=(), writes=()):
        key = "dma_" + chan
        if key not in self.sem:
            self.sem[key] = self._newsem(key)
            self.dma_count[key] = 0
        deps = self._deps(reads, writes)
        self._emit_waits(eng, deps)
        self.dma_count.setdefault(key, 0)
        self.dma_count[key] += 16
        self.items[eng].append(("op", fn, self.sem[key], 16))
        self._record(key, self.dma_count[key], reads, writes)

    def raw(self, eng, fn, reads=(), writes=()):
        self.op(eng, fn, reads, writes)

    def _snap(self):
        return copy.deepcopy((self.count, self.waited, self.last_write, self.reads_since, self.dma_count))

    def _restore(self, st):
        self.count, self.waited, self.last_write, self.reads_since, self.dma_count = copy.deepcopy(st)

    def guard(self, cnt_ap, thresh, fnA, fnB, cnt_res):
        self.barrier_all()
        for e in ENGS:
            self._emit_waits(e, self._deps([cnt_res], []))
        st0 = self._snap()
        main_items = self.items
        self.items = {e: [] for e in ENGS}
        fnA()
        itA = self.items; cA = dict(self.count); dA = dict(self.dma_count)
        self._restore(st0)
        self.items = {e: [] for e in ENGS}
        fnB()
        itB = self.items; cB = dict(self.count); dB = dict(self.dma_count)
        assert dA == dB, "branches must issue identical DMA counts per channel"
        self.items = main_items
        start = st0[0]
        for e in ENGS:
            m = max(cA[e], cB[e])
            for its_all, c in ((itA, cA[e]), (itB, cB[e])):
                d = m - c
                if d > 0:
                    its = its_all[e]
                    idx = min(i for i, it in enumerate(its) if it[0] == "op" and it[3] == 1)
                    it = its[idx]
                    its[idx] = ("op", it[1], it[2], 1 + d)
                    sem_e = self.sem[e]
                    for e2 in ENGS:
                        l2 = its_all[e2]
                        for i, it2 in enumerate(l2):
                            if it2[0] == "wait" and it2[1] is sem_e and it2[2] > start[e]:
                                l2[i] = ("wait", it2[1], it2[2] + d)
            self.count[e] = m
        for e in ENGS:
            self.items[e].append(("if", cnt_ap, thresh, itA[e], itB[e]))
        self.barrier_all()

    def barrier_all(self, engs=ENGS):
        allk = {}
        for e in ENGS:
            if self.count[e]:
                allk[e] = self.count[e]
        for k, v in self.dma_count.items():
            if v:
                allk[k] = v
        for e in engs:
            self._emit_waits(e, dict(allk))

    def emit(self):
        nc = self.nc
        items = self.items
        with nc.Block() as block:
            def run(engine, lst):
                for it in lst:
                    if it[0] == "wait":
                        engine.wait_ge(it[1], it[2])
                    elif it[0] == "if":
                        self._gr = getattr(self, "_gr", 0) + 1
                        with engine.register("gr%d" % self._gr) as gr:
                            engine.reg_load(gr, it[1])
                            with engine.If_lt(gr, it[2]):
                                run(engine, it[3])
                            with engine.Else():
                                run(engine, it[4])
                    else:
                        ins = it[1](engine)
                        ins.then_inc(it[2], it[3])

            @block.tensor
            def _(e):
                run(e, items["pe"])

            @block.scalar
            def _(e):
                run(e, items["act"])

            @block.vector
            def _(e):
                run(e, items["dve"])

            @block.gpsimd
            def _(e):
                run(e, items["pool"])

            @block.sync
            def _(e):
                run(e, items["sp"])

    def mark(self):
        return len(self.ctx)

    def release(self, m):
        while len(self.ctx) > m:
            self.ctx.pop().__exit__(None, None, None)

    def close(self):
        for cm in reversed(self.ctx):
            cm.__exit__(None, None, None)
        self.ctx = []

BF16 = mybir.dt.bfloat16

D = 2048
S = 2048
NT = 16
QKV = 2560


def emit_adaln(P, nc, c_ap, ada_w, ada_b, modrow):
    ident = P.ident
    c16 = P.sbuf("c16", [16, 128], F32)
    condT = P.sbuf("condT", [128, 16], F32)
    pc = P.psum("pc", [128, 16], F32)
    P.dma("sp", "c", lambda e: e.dma_start(out=c16[:], in_=c_ap.rearrange("(k p) -> k p", p=128)), writes=["c16"])
    P.op("pe", lambda e: e.transpose(pc[:], c16[:], ident[:16, :16]), reads=["c16", "ident"], writes=["pc"])
    P.op("act", lambda e: e.activation(out=condT[:], in_=pc[:], func=AF.Silu), reads=["pc"], writes=["condT"])
    wb = [P.sbuf(f"adaw{i}", [128, 16, 512], F32) for i in range(2)]
    pm = [P.psum(f"pm{i}", [1, 512], F32) for i in range(2)]
    bt = [P.sbuf(f"adab{i}", [1, 512], F32) for i in range(2)]
    mt = [P.sbuf(f"mrow{i}", [1, 512], F32) for i in range(2)]
    it = 0
    for l in range(2):
        for cb in range(24):
            b = it % 2
            cs = slice(cb * 512, (cb + 1) * 512)
            P.dma("sp", f"adaw{b}", lambda e, b=b, l=l, cs=cs: e.dma_start(
                out=wb[b][:], in_=ada_w[l, :, cs].rearrange("(k p) n -> p k n", p=128)),
                writes=[f"adaw{b}"])
            P.dma("sp", f"adab{b}", lambda e, b=b, l=l, cs=cs: e.dma_start(out=bt[b][:], in_=ada_b[l:l + 1, cs]), writes=[f"adab{b}"])
            for k in range(16):
                P.op("pe", lambda e, b=b, k=k: e.matmul(pm[b][:], condT[:, k:k + 1], wb[b][:, k, :], start=(k == 0), stop=(k == 15)),
                     reads=["condT", f"adaw{b}"], writes=[f"pm{b}"])
            P.op("dve", lambda e, b=b: e.tensor_tensor(mt[b][:], pm[b][:], bt[b][:], ALU.add),
                 reads=[f"pm{b}", f"adab{b}"], writes=[f"mrow{b}"])
            P.dma("sp", f"mrow{b}", lambda e, b=b, l=l, cs=cs: e.dma_start(out=modrow[l:l + 1, cs], in_=mt[b][:]), reads=[f"mrow{b}"], writes=["d_modrow"])
            it += 1


def emit_attn(P, nc, x, xa, modrow, norm_gain, w_qkv, b_qkv, q_gain, k_gain, sinks, w_o, b_o, biasmask):
    ident = P.ident
    A = P.sbuf("A", [128, D], F32)
    Bt = P.sbuf("Bt", [128, D], F32)
    G1 = P.sbuf("G1", [128, D], F32)
    bq = P.sbuf("bq", [128, QKV], F32)
    bo = P.sbuf("bo", [128, D], F32)
    qg = P.sbuf("qg", [128, 64], F32)
    kg = P.sbuf("kg", [128, 64], F32)
    snk = P.sbuf("snk", [128, 32], F32)
    bm = P.sbuf("bm", [128, 32, 256], F32)
    P.dma("sp", "c0", lambda e: e.dma_start(out=Bt[:], in_=modrow[0, 0:D].partition_broadcast(128)), reads=["d_modrow"], writes=["Bt"])
    P.dma("sp", "c1", lambda e: e.dma_start(out=A[:], in_=modrow[0, D:2 * D].partition_broadcast(128)), reads=["d_modrow"], writes=["A"])
    P.dma("sp", "c2", lambda e: e.dma_start(out=G1[:], in_=modrow[0, 2 * D:3 * D].partition_broadcast(128)), reads=["d_modrow"], writes=["G1"])
    P.dma("sp", "c3", lambda e: e.dma_start(out=bo[:], in_=norm_gain[0, 0, :].partition_broadcast(128)), writes=["bo"])
    P.op("dve", lambda e: e.scalar_tensor_tensor(A[:], A[:], 1.0, bo[:], ALU.add, ALU.mult), reads=["A", "bo"], writes=["A"])
    P.dma("sp", "c3", lambda e: e.dma_start(out=bo[:], in_=b_o[0, :].partition_broadcast(128)), reads=["bo"], writes=["bo"])
    P.dma("sp", "c4", lambda e: e.dma_start(out=bq[:], in_=b_qkv[0, :].partition_broadcast(128)), writes=["bq"])
    P.dma("sp", "c5", lambda e: e.dma_start(out=qg[:], in_=q_gain[0, :].partition_broadcast(128)), writes=["qg"])
    P.dma("sp", "c6", lambda e: e.dma_start(out=kg[:], in_=k_gain[0, :].partition_broadcast(128)), writes=["kg"])
    P.dma("sp", "c7", lambda e: e.dma_start(out=snk[:], in_=sinks[0, :].partition_broadcast(128)), writes=["snk"])
    P.dma("sp", "c8", lambda e: e.dma_start(out=bm[:], in_=biasmask.rearrange("h q k -> q h k")), writes=["bm"])
    P.op("dve", lambda e: e.tensor_scalar(qg[:], qg[:], 0.125, None, ALU.mult), reads=["qg"], writes=["qg"])

    xt = P.sbuf("xt", [128, D], F32)
    ht = P.sbuf("ht", [128, D], F32)
    junk = P.sbuf("junk", [128, QKV], F32)
    hT = P.sbuf("hT", [128, 16, 128], F32)
    ss = P.sbuf("ss", [128, 1], F32)
    rs = P.sbuf("rs", [128, 1], F32)
    qkv = P.sbuf("qkv", [128, QKV], F32)
    sq36 = P.sbuf("sq36", [128, 36], F32)
    kT = P.sbuf("kT", [64, 4, 256], F32)
    vv = P.sbuf("vv", [128, 2, 256], F32)
    qT = P.sbuf("qT", [64, 128], F32)
    pe_ = P.sbuf("pexp", [128, 256], F32)
    pT = P.sbuf("pT", [128, 2, 128], F32)
    mx = P.sbuf("mx", [128, 1], F32)
    nm = P.sbuf("nm", [128, 1], F32)
    sm = P.sbuf("sm", [128, 1], F32)
    es = P.sbuf("es", [128, 1], F32)
    osb = P.sbuf("osb", [128, D], F32)
    wch = [P.sbuf(f"wch{i}", [128, 16, 256], F32) for i in range(2)]
    ptr = P.psum("ptr", [128, 4, 128], F32)
    pmm = [P.psum(f"pmm{i}", [128, 256], F32) for i in range(2)]
    plg = P.psum("plg", [128, 256], F32)
    ppt = P.psum("ppt", [128, 2, 128], F32)
    po = P.psum("po", [128, 64], F32)
    pq = P.psum("pq", [64, 128], F32)

    P.op("dve", lambda e: e.memset(kT[:], 0.0), writes=["kT"])
    P.op("dve", lambda e: e.memset(vv[:], 0.0), writes=["vv"])
    wi = 0
    for t in range(NT):
        rows = slice(t * 128, (t + 1) * 128)
        P.dma("sp", "xt", lambda e, rows=rows: e.dma_start(out=xt[:], in_=x[rows, :]), writes=["xt"])
        P.op("act", lambda e: e.activation(out=junk[:, 0:D], in_=xt[:], func=AF.Square, accum_out=ss[:]), reads=["xt"], writes=["junk", "ss"])
        P.op("dve", lambda e: e.tensor_scalar(rs[:], ss[:], 1.0 / D, 1e-5, ALU.mult, ALU.add), reads=["ss"], writes=["rs"])
        P.op("act", lambda e: e.activation(out=rs[:], in_=rs[:], func=AF.Sqrt), reads=["rs"], writes=["rs"])
        P.op("dve", lambda e: e.reciprocal(rs[:], rs[:]), reads=["rs"], writes=["rs"])
        P.op("dve", lambda e: e.scalar_tensor_tensor(ht[:], xt[:], rs[:], A[:], ALU.mult, ALU.mult), reads=["xt", "rs", "A"], writes=["ht"])
        P.op("dve", lambda e: e.tensor_tensor(ht[:], ht[:], Bt[:], ALU.add), reads=["ht", "Bt"], writes=["ht"])
        for q in range(4):
            for i in range(4):
                k = q * 4 + i
                P.op("pe", lambda e, k=k, i=i: e.transpose(ptr[:, i, :], ht[:, k * 128:(k + 1) * 128], ident[:]), reads=["ht", "ident"], writes=["ptr"])
            P.op("act", lambda e, q=q: e.copy(out=hT[:, q * 4:(q + 1) * 4, :], in_=ptr[:]), reads=["ptr"], writes=["hT"])
        for cb in range(10):
            b = wi % 2
            wi += 1
            P.dma("sp", f"wch{b}", lambda e, b=b, cb=cb: e.dma_start(
                out=wch[b][:], in_=w_qkv[0, :, cb * 256:(cb + 1) * 256].rearrange("(k p) n -> p k n", p=128)), writes=[f"wch{b}"])
            for k in range(16):
                P.op("pe", lambda e, b=b, k=k: e.matmul(pmm[b][:], hT[:, k, :], wch[b][:, k, :], start=(k == 0), stop=(k == 15)),
                     reads=["hT", f"wch{b}"], writes=[f"pmm{b}"])
            P.op("dve", lambda e, b=b, cb=cb: e.tensor_tensor(qkv[:, cb * 256:(cb + 1) * 256], pmm[b][:], bq[:, cb * 256:(cb + 1) * 256], ALU.add),
                 reads=[f"pmm{b}", "bq"], writes=["qkv"])
        P.op("dve", lambda e: e.tensor_tensor(junk[:, 0:2304], qkv[:, 0:2304], qkv[:, 0:2304], ALU.mult), reads=["qkv"], writes=["junk"])
        P.op("dve", lambda e: e.tensor_reduce(sq36[:], junk[:, 0:2304].rearrange("p (h d) -> p h d", d=64), AX.X, ALU.add), reads=["junk"], writes=["sq36"])
        P.op("dve", lambda e: e.tensor_scalar(sq36[:], sq36[:], 1.0 / 64, 1e-5, ALU.mult, ALU.add), reads=["sq36"], writes=["sq36"])
        P.op("act", lambda e: e.activation(out=sq36[:], in_=sq36[:], func=AF.Sqrt), reads=["sq36"], writes=["sq36"])
        P.op("dve", lambda e: e.reciprocal(sq36[:], sq36[:]), reads=["sq36"], writes=["sq36"])
        for h in range(36):
            g = qg if h < 32 else kg
            gname = "qg" if h < 32 else "kg"
            P.op("dve", lambda e, h=h, g=g: e.scalar_tensor_tensor(qkv[:, h * 64:(h + 1) * 64], qkv[:, h * 64:(h + 1) * 64], sq36[:, h:h + 1], g[:], ALU.mult, ALU.mult),
                 reads=["qkv", "sq36", gname], writes=["qkv"])
        P.op("dve", lambda e: e.tensor_copy(kT[:, :, 0:128], kT[:, :, 128:256]), reads=["kT"], writes=["kT"])
        P.op("dve", lambda e: e.tensor_copy(vv[:, 0, :], vv[:, 1, :]), reads=["vv"], writes=["vv"])
        P.op("dve", lambda e: e.tensor_copy(vv[:, 1, :], qkv[:, 2304:2560]), reads=["qkv", "vv"], writes=["vv"])
        for kh in range(4):
            P.op("pe", lambda e, kh=kh: e.transpose(pq[:], qkv[:, 2048 + kh * 64:2048 + (kh + 1) * 64], ident[:]), reads=["qkv", "ident"], writes=["pq"])
            P.op("act", lambda e, kh=kh: e.copy(out=kT[:, kh, 128:256], in_=pq[:]), reads=["pq", "kT"], writes=["kT"])
        for h in range(32):
            kh = h // 8
            P.op("pe", lambda e, h=h: e.transpose(pq[:], qkv[:, h * 64:(h + 1) * 64], ident[:]), reads=["qkv", "ident"], writes=["pq"])
            P.op("act", lambda e: e.copy(out=qT[:], in_=pq[:]), reads=["pq"], writes=["qT"])
            P.op("pe", lambda e, kh=kh: e.matmul(plg[:], qT[:], kT[:, kh, :], start=True, stop=True), reads=["qT", "kT"], writes=["plg"])
            P.op("dve", lambda e, h=h: e.tensor_tensor(pe_[:], plg[:], bm[:, h, :], ALU.add), reads=["plg", "bm"], writes=["pexp"])
            if t == 0:
                P.op("dve", lambda e: e.memset(pe_[:, 0:128], -30000.0), reads=["pexp"], writes=["pexp"])
            P.op("dve", lambda e: e.reduce_max(mx[:], pe_[:], AX.X), reads=["pexp"], writes=["mx"])
            P.op("dve", lambda e, h=h: e.tensor_scalar(nm[:], mx[:], snk[:, h:h + 1], -1.0, ALU.max, ALU.mult), reads=["mx", "snk"], writes=["nm"])
            P.op("act", lambda e: e.activation(out=pe_[:], in_=pe_[:], func=AF.Exp, bias=nm[:], accum_out=sm[:]), reads=["pexp", "nm"], writes=["pexp", "sm"])
            P.op("act", lambda e, h=h: e.activation(out=es[:], in_=snk[:, h:h + 1], func=AF.Exp, bias=nm[:]), reads=["snk", "nm"], writes=["es"])
            P.op("dve", lambda e: e.tensor_tensor(sm[:], sm[:], es[:], ALU.add), reads=["sm", "es"], writes=["sm"])
            P.op("dve", lambda e: e.reciprocal(sm[:], sm[:]), reads=["sm"], writes=["sm"])
            for j in range(2):
                P.op("pe", lambda e, j=j: e.transpose(ppt[:, j, :], pe_[:, j * 128:(j + 1) * 128], ident[:]), reads=["pexp", "ident"], writes=["ppt"])
            P.op("act", lambda e: e.copy(out=pT[:], in_=ppt[:]), reads=["ppt"], writes=["pT"])
            for j in range(2):
                P.op("pe", lambda e, j=j, kh=kh: e.matmul(po[:], pT[:, j, :], vv[:, j, kh * 64:(kh + 1) * 64], start=(j == 0), stop=(j == 1)),
                     reads=["pT", "vv"], writes=["po"])
            P.op("dve", lambda e, h=h: e.tensor_scalar(osb[:, h * 64:(h + 1) * 64], po[:], sm[:], None, ALU.mult), reads=["po", "sm"], writes=["osb"])
        for q in range(4):
            for i in range(4):
                k = q * 4 + i
                P.op("pe", lambda e, k=k, i=i: e.transpose(ptr[:, i, :], osb[:, k * 128:(k + 1) * 128], ident[:]), reads=["osb", "ident"], writes=["ptr"])
            P.op("act", lambda e, q=q: e.copy(out=hT[:, q * 4:(q + 1) * 4, :], in_=ptr[:]), reads=["ptr"], writes=["hT"])
        for cb in range(8):
            b = wi % 2
            wi += 1
            P.dma("sp", f"wch{b}", lambda e, b=b, cb=cb: e.dma_start(
                out=wch[b][:], in_=w_o[0, :, cb * 256:(cb + 1) * 256].rearrange("(k p) n -> p k n", p=128)), writes=[f"wch{b}"])
            for k in range(16):
                P.op("pe", lambda e, b=b, k=k: e.matmul(pmm[b][:], hT[:, k, :], wch[b][:, k, :], start=(k == 0), stop=(k == 15)),
                     reads=["hT", f"wch{b}"], writes=[f"pmm{b}"])
            cs = slice(cb * 256, (cb + 1) * 256)
            P.op("dve", lambda e, b=b, cs=cs: e.tensor_tensor(ht[:, cs], pmm[b][:], bo[:, cs], ALU.add), reads=[f"pmm{b}", "bo"], writes=["ht"])
            P.op("dve", lambda e, cs=cs: e.tensor_tensor(ht[:, cs], ht[:, cs], G1[:, cs], ALU.mult), reads=["ht", "G1"], writes=["ht"])
            P.op("dve", lambda e, cs=cs: e.tensor_tensor(ht[:, cs], ht[:, cs], xt[:, cs], ALU.add), reads=["ht", "xt"], writes=["ht"])
        P.dma("sp", "xa", lambda e, rows=rows: e.dma_start(out=xa[rows, :], in_=ht[:]), reads=["ht"], writes=["d_xa"])


def make_biasmask(rel_bias):
    ql = np.arange(128)[:, None]
    kl = np.arange(256)[None, :]
    dist = ql + 128 - kl
    n = np.maximum(dist, 0)
    max_exact = 16
    large = max_exact + (np.log(np.maximum(n, 1) / max_exact) / np.log(128 / max_exact) * (32 - max_exact)).astype(np.int32)
    large = np.minimum(large, 31)
    bucket = np.where(n < max_exact, n, large).astype(np.int32)
    valid = (dist >= 0) & (dist < 128)
    bm = np.ascontiguousarray(np.transpose(rel_bias[bucket], (2, 0, 1))).astype(np.float32)
    bm[:, ~valid] = -30000.0
    return bm


STOP = 99
NP = 4
TP = 512


GUARD = True


def emit_moe(P, nc, l, xin, xout, modrow, norm_gain, w_router, b_router, w_gu, b_gu, w_d, b_d, mode='full', nexp=32):
    ident = P.ident
    wg_s = nc.dram_tensor(f"wg_s{l}", [32, 8, 128, 16 * 256], BF16, kind="Internal").ap()
    wl_s = nc.dram_tensor(f"wl_s{l}", [32, 8, 128, 16 * 256], BF16, kind="Internal").ap()
    wd_s = nc.dram_tensor(f"wd_s{l}", [32, 8, 128, 16 * 256], BF16, kind="Internal").ap()
    A = P.sbuf("mA", [128, D], F32)
    brt = P.sbuf("brt", [128, 32], F32)
    wr = P.sbuf("wr", [128, 16, 32], F32)
    identb = P.sbuf("identb", [128, 128], BF16)
    ones_b = P.sbuf("ones_b", [1, 128], BF16)
    P.op("dve", lambda e: e.tensor_copy(identb[:], ident[:]), reads=["ident"], writes=["identb"])
    P.op("dve", lambda e: e.memset(ones_b[:], 1.0), writes=["ones_b"])
    P.dma("sp", "m3", lambda e: e.dma_start(out=brt[:], in_=b_router[l, :].partition_broadcast(128)), writes=["brt"])
    P.dma("sp", "m4", lambda e: e.dma_start(out=wr[:], in_=w_router[l].rearrange("(k p) n -> p k n", p=128)), writes=["wr"])
    xt = P.sbuf("mxt", [128, D], F32)
    ht = P.sbuf("mht", [128, D], F32)
    ss = P.sbuf("mss", [128, 1], F32)
    rs = P.sbuf("mrs", [128, 1], F32)
    hT = P.sbuf("hTb", [128, 16, TP], BF16)
    actT = P.sbuf("actT", [128, 16, TP], BF16)
    acc = P.sbuf("acc", [128, TP // 128, D], F32)
    hTf = acc[:, 0, :].rearrange("p (k t) -> p k t", k=16)
    Gm = P.sbuf("Gm", [128, TP // 128, 32], F32)
    lg = P.sbuf("lg", [128, 32], F32)
    mx8 = P.sbuf("mx8", [128, 8], F32)
    nmx = P.sbuf("nmx", [128, 1], F32)
    msk = P.sbuf("msk", [128, 32], F32)
    ssum = P.sbuf("ssum", [128, 1], F32)
    bg32 = P.sbuf("bg32", [32, 128], F32)
    bgT = [P.sbuf(f"bgT{i}", [128, 32], F32) for i in range(2)]
    bd_b = [P.sbuf(f"bd_b{i}", [1, D], BF16) for i in range(2)]
    wg = [P.sbuf(f"wg{i}", [128, 16, 256], BF16) for i in range(2)]
    wl = [P.sbuf(f"wl{i}", [128, 16, 256], BF16) for i in range(2)]
    wd = [P.sbuf(f"wd{i}", [128, 16, 256], BF16) for i in range(2)]
    CAP = 128
    hTok = P.sbuf("hTok", [128, TP // 128, D], BF16)
    Mall = P.sbuf("Mall", [128, TP // 128, 32], F32)
    rank = P.sbuf("rank", [128, TP // 128, 32], F32)
    cnt_i = P.sbuf("cnt_i", [128, 32], I32)
    onesf = P.sbuf("onesf", [128, 128], F32)
    Lst = P.sbuf("Lst", [128, 128], F32)
    iota_c = P.sbuf("iota_c", [128, CAP], F32)
    Sm = P.sbuf("Sm", [128, TP // 128, CAP], BF16)
    SGm = P.sbuf("SGm", [128, TP // 128, CAP], BF16)
    STm = P.sbuf("STm", [128, CAP // 128, TP // 128, 128], BF16)
    hTc_full = P.sbuf("hTc", [128, 16, 256], BF16)
    Bt = hTc_full[:].rearrange("p k n -> p (k n)").bitcast(F32)
    hTc = hTc_full[:, :, 0:CAP]
    yc = P.sbuf("yc", [128, CAP // 128, 256], BF16)
    P.op("pool", lambda e: e.memset(onesf[:], 1.0), writes=["onesf"])
    P.op("pool", lambda e: e.memset(Lst[:], 1.0), writes=["Lst"])
    P.op("pool", lambda e: e.affine_select(Lst[:], Lst[:], [[1, 128]], ALU.is_gt, 0.0, base=0, channel_multiplier=-1), reads=["Lst"], writes=["Lst"])
    P.op("pool", lambda e: e.iota(iota_c[:], [[1, CAP]], base=0, channel_multiplier=0, allow_small_or_imprecise_dtypes=True), writes=["iota_c"])
    t1 = [P.sbuf(f"t1_{i}", [128, TP], F32) for i in range(2)]
    t2 = [P.sbuf(f"t2_{i}", [128, TP], F32) for i in range(2)]
    sg = [P.sbuf(f"sg_{i}", [128, TP], F32) for i in range(2)]
    ptr = P.psum("mptr", [128, 4, 128], F32)
    pg = [P.psum(f"pg{i}", [128, TP], F32) for i in range(2)]
    pl = [P.psum(f"pl{i}", [128, TP], F32) for i in range(2)]
    pd = [P.psum(f"pd{i}", [128, 512], F32) for i in range(2)]
    psm = P.psum("mpsm", [128, 64], F32)
    plg = psm[:, 0:32]
    pbg = psm[:, 32:64]

    wi = 0; di = 0; ui = 0; pi = 0; ei = 0
    for ps_ in range(NP):
        P.dma("sp", "m1", lambda e: e.dma_start(out=A[:], in_=modrow[l, 4 * D:5 * D].partition_broadcast(128)), reads=["d_modrow", "mA"], writes=["mA"])
        P.dma("sp", "m0", lambda e: e.dma_start(out=Bt, in_=modrow[l, 3 * D:4 * D].partition_broadcast(128)), reads=["d_modrow", "hTc"], writes=["hTc"])
        P.dma("sp", "m5", lambda e: e.dma_start(out=ht[:], in_=norm_gain[l, 1, :].partition_broadcast(128)), reads=["mht"], writes=["mht"])
        P.op("dve", lambda e: e.scalar_tensor_tensor(A[:], A[:], 1.0, ht[:], ALU.add, ALU.mult), reads=["mA", "mht"], writes=["mA"])
        for tt in range(TP // 128):
            rows = slice(ps_ * TP + tt * 128, ps_ * TP + (tt + 1) * 128)
            P.dma("sp", "mxt", lambda e, rows=rows: e.dma_start(out=xt[:], in_=xin[rows, :]), writes=["mxt"])
            P.op("act", lambda e: e.activation(out=ht[:], in_=xt[:], func=AF.Square, accum_out=ss[:]), reads=["mxt"], writes=["mht", "mss"])
            P.op("dve", lambda e: e.tensor_scalar(rs[:], ss[:], 1.0 / D, 1e-5, ALU.mult, ALU.add), reads=["mss"], writes=["mrs"])
            P.op("act", lambda e: e.activation(out=rs[:], in_=rs[:], func=AF.Sqrt), reads=["mrs"], writes=["mrs"])
            P.op("dve", lambda e: e.reciprocal(rs[:], rs[:]), reads=["mrs"], writes=["mrs"])
            P.op("dve", lambda e: e.scalar_tensor_tensor(ht[:], xt[:], rs[:], A[:], ALU.mult, ALU.mult), reads=["mxt", "mrs", "mA"], writes=["mht"])
            P.op("dve", lambda e: e.tensor_tensor(ht[:], ht[:], Bt, ALU.add), reads=["mht", "hTc"], writes=["mht"])
            P.op("pool", lambda e, tt=tt: e.tensor_copy(hTok[:, tt, :], ht[:]), reads=["mht"], writes=["hTok"])
            if STOP <= 1:
                P.dma("sp", "mxo", lambda e, rows=rows: e.dma_start(out=xout[rows, :], in_=ht[:]), reads=["mht"], writes=["d_xout"])
                continue
            for q in range(4):
                for i in range(4):
                    k = q * 4 + i
                    P.op("pe", lambda e, k=k, i=i: e.transpose(ptr[:, i, :], ht[:, k * 128:(k + 1) * 128], ident[:]), reads=["mht", "ident"], writes=["mptr"])
                P.op("act", lambda e, q=q: e.copy(out=hTf[:, q * 4:(q + 1) * 4, :], in_=ptr[:]), reads=["mptr"], writes=["acc"])
                P.op("dve", lambda e, q=q, tt=tt: e.tensor_copy(hT[:, q * 4:(q + 1) * 4, tt * 128:(tt + 1) * 128], hTf[:, q * 4:(q + 1) * 4, :]), reads=["acc"], writes=["hTb"])
            if STOP <= 2:
                P.dma("sp", "mxo", lambda e, rows=rows: e.dma_start(out=xout[rows, :], in_=hTf[:].rearrange("p k t -> p (k t)")), reads=["acc", "hTb"], writes=["d_xout"])
                continue
            for k in range(16):
                P.op("pe", lambda e, k=k: e.matmul(plg, hTf[:, k, :], wr[:, k, :], start=(k == 0), stop=(k == 15)), reads=["acc", "wr"], writes=["mpsm"])
            P.op("dve", lambda e: e.tensor_tensor(lg[:], plg, brt[:], ALU.add), reads=["mpsm", "brt"], writes=["lg"])
            if STOP <= 3:
                P.dma("sp", "mxo", lambda e, rows=rows: e.dma_start(out=xout[rows, 0:32], in_=lg[:]), reads=["lg"], writes=["d_xout"])
                continue
            P.op("dve", lambda e: e.max(mx8[:], lg[:]), reads=["lg"], writes=["mx8"])
            P.op("dve", lambda e: e.tensor_scalar(msk[:], lg[:], mx8[:, 3:4], None, ALU.is_ge), reads=["lg", "mx8"], writes=["msk"])
            P.op("dve", lambda e, tt=tt: e.tensor_copy(Mall[:, tt, :], msk[:]), reads=["msk"], writes=["Mall"])
            P.op("dve", lambda e: e.tensor_scalar(nmx[:], mx8[:, 0:1], -1.0, None, ALU.mult), reads=["mx8"], writes=["nmx"])
            P.op("act", lambda e: e.activation(out=lg[:], in_=lg[:], func=AF.Exp, bias=nmx[:]), reads=["lg", "nmx"], writes=["lg"])
            P.op("dve", lambda e: e.tensor_tensor(lg[:], lg[:], msk[:], ALU.mult), reads=["lg", "msk"], writes=["lg"])
            P.op("dve", lambda e: e.reduce_sum(ssum[:], lg[:], AX.X), reads=["lg"], writes=["ssum"])
            P.op("dve", lambda e: e.reciprocal(ssum[:], ssum[:]), reads=["ssum"], writes=["ssum"])
            P.op("dve", lambda e, tt=tt: e.tensor_scalar(Gm[:, tt, :], lg[:], ssum[:], None, ALU.mult), reads=["lg", "ssum"], writes=["Gm"])
        if mode == 'route' and STOP <= 3:
            continue
        if mode == 'route':
            for tt in range(TP // 128):
                rows = slice(ps_ * TP + tt * 128, ps_ * TP + (tt + 1) * 128)
                P.dma("sp", "mxo", lambda e, rows=rows, tt=tt: e.dma_start(out=xout[rows, 0:32], in_=Gm[:, tt, :]), reads=["Gm"], writes=["d_xout"])
            continue
        P.op("pool", lambda e: e.memset(acc[:], 0.0), reads=["acc"], writes=["acc"])
        NTL = TP // 128
        for tt in range(NTL):
            for t2_ in range(tt):
                P.op("pe", lambda e, t2_=t2_: e.matmul(plg, onesf[:], Mall[:, t2_, :], start=(t2_ == 0), stop=False), reads=["onesf", "Mall"], writes=["mpsm"])
            P.op("pe", lambda e, tt=tt: e.matmul(plg, Lst[:], Mall[:, tt, :], start=(tt == 0), stop=True), reads=["Lst", "Mall"], writes=["mpsm"])
            P.op("dve", lambda e, tt=tt: e.tensor_copy(rank[:, tt, :], plg), reads=["mpsm"], writes=["rank"])
        for tt in range(NTL):
            P.op("pe", lambda e, tt=tt: e.matmul(plg, onesf[:], Mall[:, tt, :], start=(tt == 0), stop=(tt == NTL - 1)), reads=["onesf", "Mall"], writes=["mpsm"])
        P.op("dve", lambda e: e.tensor_copy(cnt_i[:], plg), reads=["mpsm"], writes=["cnt_i"])
        for ex in range(nexp):
            eb = ei % 2; ei += 1
            P.dma("sp", "bg32", lambda e, ex=ex: e.dma_start(out=bg32[:], in_=b_gu[l, ex, :].rearrange("(c p) -> c p", p=128)), writes=["bg32"])
            P.op("pe", lambda e: e.transpose(pbg, bg32[:], ident[:32, :32]), reads=["bg32", "ident"], writes=["mpsm"])
            P.op("act", lambda e, eb=eb: e.copy(out=bgT[eb][:], in_=pbg), reads=["mpsm"], writes=[f"bgT{eb}"])
            P.dma("pool", f"bd_b{eb}", lambda e, ex=ex, eb=eb: e.dma_start(out=bd_b[eb][:], in_=b_d[l, ex:ex + 1, :]), writes=[f"bd_b{eb}"])
            ctr = {"wi": wi, "di": di, "ui": ui, "pi": pi}

            def ffn(compact, ex=ex, eb=eb, ctr=ctr):
                wi = ctr["wi"]; di = ctr["di"]; ui = ctr["ui"]; pi = ctr["pi"]
                N = CAP if compact else TP
                hsrc = hTc if compact else hT
                if compact:
                    for tt in range(NTL):
                        P.op("dve", lambda e, tt=tt: e.tensor_scalar(Sm[:, tt, :], iota_c[:], rank[:, tt, ex:ex + 1], Mall[:, tt, ex:ex + 1], ALU.is_equal, ALU.mult),
                             reads=["iota_c", "rank", "Mall"], writes=["Sm"])
                        P.op("dve", lambda e, tt=tt: e.tensor_scalar(SGm[:, tt, :], Sm[:, tt, :], Gm[:, tt, ex:ex + 1], None, ALU.mult), reads=["Sm", "Gm"], writes=["SGm"])
                    ptb = ptr[:].rearrange("p a b -> p (a b)").bitcast(BF16).rearrange("p (a b) -> p a b", b=128)
                    for sb in range(CAP // 128):
                        for tt in range(NTL):
                            P.op("pe", lambda e, sb=sb, tt=tt: e.transpose(ptb[:, sb * NTL + tt, :], SGm[:, tt, sb * 128:(sb + 1) * 128], identb[:]), reads=["SGm", "identb"], writes=["mptr"])
                    P.op("act", lambda e: e.copy(out=STm[:].rearrange("p s t k -> p (s t) k"), in_=ptb[:, 0:(CAP // 128) * NTL, :]), reads=["mptr"], writes=["STm"])
                    for k2 in range(8):
                        u = ui % 2; ui += 1
                        for kk in range(2):
                            k = k2 * 2 + kk
                            for tt in range(NTL):
                                P.op("pe", lambda e, u=u, k=k, kk=kk, tt=tt: e.matmul(pg[u][:, kk * CAP:(kk + 1) * CAP], hTok[:, tt, k * 128:(k + 1) * 128], Sm[:, tt, :], start=(tt == 0), stop=(tt == NTL - 1)),
                                     reads=["hTok", "Sm"], writes=[f"pg{u}"])
                        P.op("act", lambda e, u=u, k2=k2: e.copy(out=hTc[:, k2 * 2:k2 * 2 + 2, :], in_=pg[u][:, 0:2 * CAP].rearrange("p (a b) -> p a b", a=2)), reads=[f"pg{u}"], writes=["hTc"])
                for c2 in range(8):
                    b = wi % 2; wi += 1
                    if ps_ == 0:
                        P.dma("pool", f"wg{b}", lambda e, b=b, c2=c2: e.dma_start(
                            out=wg[b][:], in_=w_gu[l, ex, :, c2 * 256:(c2 + 1) * 256].rearrange("(k p) n -> p k n", p=128)), writes=[f"wg{b}"])
                        P.dma("pool", f"wl{b}", lambda e, b=b, c2=c2: e.dma_start(
                            out=wl[b][:], in_=w_gu[l, ex, :, D + c2 * 256:D + (c2 + 1) * 256].rearrange("(k p) n -> p k n", p=128)), writes=[f"wl{b}"])
                        P.dma("sp", f"wgw{b}", lambda e, b=b, c2=c2: e.dma_start(out=wg_s[ex, c2], in_=wg[b][:].rearrange("p k n -> p (k n)")), reads=[f"wg{b}"], writes=["d_wgs"])
                        P.dma("sp", f"wlw{b}", lambda e, b=b, c2=c2: e.dma_start(out=wl_s[ex, c2], in_=wl[b][:].rearrange("p k n -> p (k n)")), reads=[f"wl{b}"], writes=["d_wls"])
                    else:
                        P.dma("sp", f"wg{b}", lambda e, b=b, c2=c2: e.dma_start(out=wg[b][:].rearrange("p k n -> p (k n)"), in_=wg_s[ex, c2]), reads=["d_wgs"], writes=[f"wg{b}"])
                        P.dma("sp", f"wl{b}", lambda e, b=b, c2=c2: e.dma_start(out=wl[b][:].rearrange("p k n -> p (k n)"), in_=wl_s[ex, c2]), reads=["d_wls"], writes=[f"wl{b}"])
                    for sub in range(2):
                        u = ui % 2; ui += 1
                        ch = c2 * 2 + sub
                        hname = "hTc" if compact else "hTb"
                        for k in range(16):
                            P.op("pe", lambda e, u=u, b=b, k=k, sub=sub: e.matmul(pg[u][:, 0:N], wg[b][:, k, sub * 128:(sub + 1) * 128], hsrc[:, k, :], start=(k == 0), stop=(k == 15)),
                                 reads=[f"wg{b}", hname], writes=[f"pg{u}"])
                        for k in range(16):
                            P.op("pe", lambda e, u=u, b=b, k=k, sub=sub: e.matmul(pl[u][:, 0:N], wl[b][:, k, sub * 128:(sub + 1) * 128], hsrc[:, k, :], start=(k == 0), stop=(k == 15)),
                                 reads=[f"wl{b}", hname], writes=[f"pl{u}"])
                        P.op("dve", lambda e, u=u, ch=ch: e.tensor_scalar(t1[u][:, 0:N], pg[u][:, 0:N], bgT[eb][:, ch:ch + 1], 7.0, ALU.add, ALU.min),
                             reads=[f"pg{u}", f"bgT{eb}"], writes=[f"t1_{u}"])
                        P.op("act", lambda e, u=u: e.activation(out=sg[u][:, 0:N], in_=t1[u][:, 0:N], func=AF.Sigmoid, scale=1.702), reads=[f"t1_{u}"], writes=[f"sg_{u}"])
                        P.op("dve", lambda e, u=u, ch=ch: e.tensor_scalar(t2[u][:, 0:N], pl[u][:, 0:N], bgT[eb][:, 16 + ch:16 + ch + 1], 7.0, ALU.add, ALU.min),
                             reads=[f"pl{u}", f"bgT{eb}"], writes=[f"t2_{u}"])
                        P.op("dve", lambda e, u=u: e.tensor_scalar(t2[u][:, 0:N], t2[u][:, 0:N], -7.0, 1.0, ALU.max, ALU.add), reads=[f"t2_{u}"], writes=[f"t2_{u}"])
                        P.op("dve", lambda e, u=u: e.tensor_tensor(t1[u][:, 0:N], t1[u][:, 0:N], sg[u][:, 0:N], ALU.mult), reads=[f"t1_{u}", f"sg_{u}"], writes=[f"t1_{u}"])
                        P.op("dve", lambda e, u=u, ch=ch: e.tensor_tensor(actT[:, ch, 0:N], t1[u][:, 0:N], t2[u][:, 0:N], ALU.mult), reads=[f"t1_{u}", f"t2_{u}"], writes=["actT"])
                for cc in range(8):
                    b = di % 2; di += 1
                    if ps_ == 0:
                        P.dma("pool", f"wd{b}", lambda e, b=b, cc=cc: e.dma_start(
                            out=wd[b][:], in_=w_d[l, ex, :, cc * 256:(cc + 1) * 256].rearrange("(k p) n -> p k n", p=128)), writes=[f"wd{b}"])
                        P.dma("sp", f"wdw{b}", lambda e, b=b, cc=cc: e.dma_start(out=wd_s[ex, cc], in_=wd[b][:].rearrange("p k n -> p (k n)")), reads=[f"wd{b}"], writes=["d_wds"])
                    else:
                        P.dma("sp", f"wd{b}", lambda e, b=b, cc=cc: e.dma_start(out=wd[b][:].rearrange("p k n -> p (k n)"), in_=wd_s[ex, cc]), reads=["d_wds"], writes=[f"wd{b}"])
                    ccs = slice(cc * 256, (cc + 1) * 256)
                    if not compact:
                        for tt in range(NTL):
                            u = pi % 2; pi += 1
                            for i in range(16):
                                P.op("pe", lambda e, u=u, b=b, i=i, tt=tt: e.matmul(pd[u][:, 0:256], actT[:, i, tt * 128:(tt + 1) * 128], wd[b][:, i, :], start=(i == 0), stop=False),
                                     reads=["actT", f"wd{b}"], writes=[f"pd{u}"])
                            P.op("pe", lambda e, u=u, ccs=ccs: e.matmul(pd[u][:, 0:256], ones_b[:], bd_b[eb][:, ccs], start=False, stop=True),
                                 reads=["ones_b", f"bd_b{eb}"], writes=[f"pd{u}"])
                            P.op("dve", lambda e, u=u, tt=tt, ccs=ccs: e.scalar_tensor_tensor(acc[:, tt, ccs], pd[u][:, 0:256], Gm[:, tt, ex:ex + 1], acc[:, tt, ccs], ALU.mult, ALU.add),
                                 reads=[f"pd{u}", "Gm", "acc"], writes=["acc"])
                    else:
                        for sb in range(CAP // 128):
                            u = pi % 2; pi += 1
                            for i in range(16):
                                P.op("pe", lambda e, u=u, b=b, i=i, sb=sb: e.matmul(pd[u][:, 0:256], actT[:, i, sb * 128:(sb + 1) * 128], wd[b][:, i, :], start=(i == 0), stop=False),
                                     reads=["actT", f"wd{b}"], writes=[f"pd{u}"])
                            P.op("pe", lambda e, u=u, ccs=ccs: e.matmul(pd[u][:, 0:256], ones_b[:], bd_b[eb][:, ccs], start=False, stop=True),
                                 reads=["ones_b", f"bd_b{eb}"], writes=[f"pd{u}"])
                            P.op("act", lambda e, u=u, sb=sb: e.copy(out=yc[:, sb, :], in_=pd[u][:, 0:256]), reads=[f"pd{u}"], writes=["yc"])
                        for tt in range(NTL):
                            u = ui % 2; ui += 1
                            for sb in range(CAP // 128):
                                P.op("pe", lambda e, u=u, sb=sb, tt=tt: e.matmul(pg[u][:, 0:256], STm[:, sb, tt, :], yc[:, sb, :], start=(sb == 0), stop=(sb == CAP // 128 - 1)),
                                     reads=["STm", "yc"], writes=[f"pg{u}"])
                            P.op("dve", lambda e, u=u, tt=tt, ccs=ccs: e.tensor_tensor(acc[:, tt, ccs], acc[:, tt, ccs], pg[u][:, 0:256], ALU.add), reads=[f"pg{u}", "acc"], writes=["acc"])
                ctr2 = {"wi": wi, "di": di, "ui": ui, "pi": pi}
                return ctr2

            res = {}
            def fa():
                res["a"] = ffn(True)
            def fb():
                res["b"] = ffn(False)
            if GUARD:
                P.guard(cnt_i[0:1, ex:ex + 1], CAP + 1, fa, fb, "cnt_i")
            else:
                fb()
                res["a"] = res["b"]
            wi = max(res["a"]["wi"], res["b"]["wi"]); di = max(res["a"]["di"], res["b"]["di"])
            ui = 0; pi = 0
        P.dma("sp", "m1", lambda e: e.dma_start(out=A[:], in_=modrow[l, 5 * D:6 * D].partition_broadcast(128)), reads=["d_modrow", "mA"], writes=["mA"])
        for tt in range(TP // 128):
            rows = slice(ps_ * TP + tt * 128, ps_ * TP + (tt + 1) * 128)
            P.dma("sp", "mxt", lambda e, rows=rows: e.dma_start(out=xt[:], in_=xin[rows, :]), writes=["mxt"])
            P.op("dve", lambda e, tt=tt: e.tensor_tensor(ht[:], acc[:, tt, :], A[:], ALU.mult), reads=["acc", "mA"], writes=["mht"])
            P.op("dve", lambda e: e.tensor_tensor(ht[:], ht[:], xt[:], ALU.add), reads=["mht", "mxt"], writes=["mht"])
            P.dma("sp", "mxo", lambda e, rows=rows: e.dma_start(out=xout[rows, :], in_=ht[:]), reads=["mht"], writes=["d_xout%d" % l])

TWO_PI = 2.0 * math.pi


def _wrap_sincos(P, frac_src, n, tagp, tmp_i, tmp_f, g, sin_out, cos_out, u, u_name=None, ti_name=None):
    u_name = u_name or (tagp + "u"); ti_name = ti_name or (tagp + "ti")
    P.op("dve", lambda e: e.tensor_copy(tmp_i, u), reads=[u_name], writes=[ti_name])
    P.op("dve", lambda e: e.tensor_copy(tmp_f, tmp_i), reads=[ti_name], writes=[tagp + "tf"])
    P.op("dve", lambda e: e.tensor_tensor(tmp_f, u, tmp_f, ALU.subtract), reads=[u_name, tagp + "tf"], writes=[tagp + "tf"])
    P.op("dve", lambda e: e.scalar_tensor_tensor(g, tmp_f, 0.5, tmp_f, ALU.is_gt, ALU.subtract), reads=[tagp + "tf"], writes=[tagp + "g"])
    P.op("dve", lambda e: e.scalar_tensor_tensor(tmp_f, g, 0.5, g, ALU.is_gt, ALU.subtract), reads=[tagp + "g"], writes=[tagp + "tf"])
    P.op("act", lambda e: e.activation(out=sin_out, in_=tmp_f, func=AF.Sin, scale=TWO_PI), reads=[tagp + "tf"], writes=[tagp + "sin"])
    P.op("dve", lambda e: e.tensor_scalar(tmp_f, tmp_f, 0.25, None, ALU.add), reads=[tagp + "tf", tagp + "sin"], writes=[tagp + "tf"])
    P.op("dve", lambda e: e.scalar_tensor_tensor(g, tmp_f, 0.5, tmp_f, ALU.is_gt, ALU.subtract), reads=[tagp + "tf"], writes=[tagp + "g"])
    P.op("act", lambda e: e.activation(out=cos_out, in_=g, func=AF.Sin, scale=-TWO_PI), reads=[tagp + "g"], writes=[tagp + "cos"])


def emit_ssm(P, nc, xin, xout, hbuf, modrow, norm_gain, lam_re, lam_im, log_dt, b_re, b_im, c_re, c_im, d_skip,
             w_a, b_a, w_b, b_b, debug_y=None):
    ident = P.ident
    m_pre = P.mark()
    A = P.sbuf("sA", [128, D], F32)
    Bt = P.sbuf("sB", [128, D], F32)
    xt = P.sbuf("sxt", [128, D], F32)
    ht = P.sbuf("sht", [128, D], F32)
    ss = P.sbuf("sss", [128, 1], F32)
    rs = P.sbuf("srs", [128, 1], F32)
    P.dma("sp", "s0", lambda e: e.dma_start(out=Bt[:], in_=modrow[1, 0:D].partition_broadcast(128)), reads=["d_modrow"], writes=["sB"])
    P.dma("sp", "s1", lambda e: e.dma_start(out=A[:], in_=modrow[1, D:2 * D].partition_broadcast(128)), reads=["d_modrow"], writes=["sA"])
    P.dma("sp", "s2", lambda e: e.dma_start(out=ht[:], in_=norm_gain[1, 0, :].partition_broadcast(128)), writes=["sht"])
    P.op("dve", lambda e: e.scalar_tensor_tensor(A[:], A[:], 1.0, ht[:], ALU.add, ALU.mult), reads=["sA", "sht"], writes=["sA"])
    for t in range(16):
        rows = slice(t * 128, (t + 1) * 128)
        P.dma("sp", "sxt", lambda e, rows=rows: e.dma_start(out=xt[:], in_=xin[rows, :]), writes=["sxt"])
        P.op("act", lambda e: e.activation(out=ht[:], in_=xt[:], func=AF.Square, accum_out=ss[:]), reads=["sxt"], writes=["sht", "sss"])
        P.op("dve", lambda e: e.tensor_scalar(rs[:], ss[:], 1.0 / D, 1e-5, ALU.mult, ALU.add), reads=["sss"], writes=["srs"])
        P.op("act", lambda e: e.activation(out=rs[:], in_=rs[:], func=AF.Sqrt), reads=["srs"], writes=["srs"])
        P.op("dve", lambda e: e.reciprocal(rs[:], rs[:]), reads=["srs"], writes=["srs"])
        P.op("dve", lambda e: e.scalar_tensor_tensor(ht[:], xt[:], rs[:], A[:], ALU.mult, ALU.mult), reads=["sxt", "srs", "sA"], writes=["sht"])
        P.op("dve", lambda e: e.tensor_tensor(ht[:], ht[:], Bt[:], ALU.add), reads=["sht", "sB"], writes=["sht"])
        P.dma("sp", "shb", lambda e, rows=rows: e.dma_start(out=hbuf[rows, :], in_=ht[:]), reads=["sht"], writes=["d_hbuf"])
    P.barrier_all()
    P.release(m_pre)

    ptr = P.psum("sptr", [128, 4, 128], F32)
    pxr = [P.psum(f"pxr{i}", [128, 512], F32) for i in range(2)]
    pxi = [P.psum(f"pxi{i}", [128, 512], F32) for i in range(2)]
    pyc = [P.psum(f"pyc{i}", [128, 512], F32) for i in range(2)]

    l64 = P.sbuf("l64", [64, 3, 128], F32)
    dt2 = P.sbuf("dt2", [64, 2], F32)
    P.dma("sp", "p0", lambda e: e.dma_start(out=l64[:, 0, :], in_=lam_re[0].rearrange("(j a) p -> j (a p)", a=2)), writes=["l64"])
    P.dma("sp", "p1", lambda e: e.dma_start(out=l64[:, 1, :], in_=lam_im[0].rearrange("(j a) p -> j (a p)", a=2)), writes=["l64"])
    P.dma("sp", "p2", lambda e: e.dma_start(out=dt2[:], in_=log_dt[0].rearrange("(j a) -> j a", a=2)), writes=["dt2"])
    P.op("dve", lambda e: e.tensor_copy(l64[:, 2, 0:64], dt2[:, 0:1].to_broadcast([64, 64])), reads=["dt2", "l64"], writes=["l64"])
    P.op("dve", lambda e: e.tensor_copy(l64[:, 2, 64:128], dt2[:, 1:2].to_broadcast([64, 64])), reads=["dt2", "l64"], writes=["l64"])
    prm = P.sbuf("prm", [128, 3, 64], F32)
    for i in range(3):
        P.op("pe", lambda e, i=i: e.transpose(ptr[:, i, 0:64], l64[:, i, :], ident[:64, :64]), reads=["l64", "ident"], writes=["sptr"])
    P.op("act", lambda e: e.copy(out=prm[:], in_=ptr[:, 0:3, 0:64]), reads=["sptr"], writes=["prm"])
    lr = prm[:, 0, :]; li = prm[:, 1, :]
    q = P.sbuf("q", [128, 12, 64], F32)
    qi = P.sbuf("qi", [128, 64], I32)
    dtc, rr, thf, sn, cs, abr, abi, den, fre, fim, tA, tB = [q[:, i, :] for i in range(12)]
    P.op("act", lambda e: e.activation(out=dtc, in_=prm[:, 2, :], func=AF.Exp), reads=["prm"], writes=["q_dt"])
    P.op("dve", lambda e: e.tensor_tensor(rr, lr, dtc, ALU.mult), reads=["prm", "q_dt"], writes=["q_r"])
    P.op("act", lambda e: e.activation(out=rr, in_=rr, func=AF.Exp), reads=["q_r"], writes=["q_r"])
    P.op("dve", lambda e: e.scalar_tensor_tensor(thf, li, 1.0 / TWO_PI, dtc, ALU.mult, ALU.mult), reads=["prm", "q_dt"], writes=["pp_u"])
    _wrap_sincos(P, None, 64, "pp_", qi[:], tA, tB, sn, cs, thf)
    P.op("dve", lambda e: e.tensor_tensor(abr, rr, cs, ALU.mult), reads=["q_r", "pp_cos"], writes=["q_abr"])
    P.op("dve", lambda e: e.tensor_tensor(abi, rr, sn, ALU.mult), reads=["q_r", "pp_sin"], writes=["q_abi"])
    P.op("dve", lambda e: e.tensor_tensor(den, lr, lr, ALU.mult), reads=["prm"], writes=["q_den"])
    P.op("dve", lambda e: e.tensor_tensor(tA, li, li, ALU.mult), reads=["prm", "pp_tf"], writes=["pp_tf"])
    P.op("dve", lambda e: e.tensor_tensor(den, den, tA, ALU.add), reads=["q_den", "pp_tf"], writes=["q_den"])
    P.op("dve", lambda e: e.reciprocal(den, den), reads=["q_den"], writes=["q_den"])
    P.op("dve", lambda e: e.tensor_scalar(abr, abr, -1.0, None, ALU.add), reads=["q_abr"], writes=["q_abr"])
    P.op("dve", lambda e: e.tensor_tensor(tA, abr, lr, ALU.mult), reads=["q_abr", "prm", "pp_tf"], writes=["pp_tf"])
    P.op("dve", lambda e: e.tensor_tensor(tB, abi, li, ALU.mult), reads=["q_abi", "prm", "pp_g"], writes=["pp_g"])
    P.op("dve", lambda e: e.tensor_tensor(tA, tA, tB, ALU.add), reads=["pp_tf", "pp_g"], writes=["pp_tf"])
    P.op("dve", lambda e: e.tensor_tensor(fre, tA, den, ALU.mult), reads=["pp_tf", "q_den"], writes=["q_fre"])
    P.op("dve", lambda e: e.tensor_tensor(tA, abi, lr, ALU.mult), reads=["q_abi", "prm", "pp_tf"], writes=["pp_tf"])
    P.op("dve", lambda e: e.tensor_tensor(tB, abr, li, ALU.mult), reads=["q_abr", "prm", "pp_g"], writes=["pp_g"])
    P.op("dve", lambda e: e.tensor_tensor(tA, tA, tB, ALU.subtract), reads=["pp_tf", "pp_g"], writes=["pp_tf"])
    P.op("dve", lambda e: e.tensor_tensor(fim, tA, den, ALU.mult), reads=["pp_tf", "q_den"], writes=["q_fim"])
    P.op("dve", lambda e: e.scalar_tensor_tensor(thf, li, 1.0 / TWO_PI, dtc, ALU.mult, ALU.mult), reads=["prm", "q_dt", "pp_u"], writes=["pp_u"])

    Bre = P.sbuf("Bre", [128, 64, 16], F32)
    Bim = P.sbuf("Bim", [128, 64, 16], F32)
    bbr = P.sbuf("bbr", [128, 64, 16], F32)
    bbi = P.sbuf("bbi", [128, 64, 16], F32)
    for a in range(2):
        P.dma("sp", f"pb{a}", lambda e, a=a: e.dma_start(out=Bre[a * 64:(a + 1) * 64, :, :], in_=b_re[0].rearrange("(j a) p c -> a p j c", a=2)[a]), writes=["Bre"])
        P.dma("sp", f"pc{a}", lambda e, a=a: e.dma_start(out=Bim[a * 64:(a + 1) * 64, :, :], in_=b_im[0].rearrange("(j a) p c -> a p j c", a=2)[a]), writes=["Bim"])
    freb = fre.unsqueeze(2).to_broadcast([128, 64, 16])
    fimb = fim.unsqueeze(2).to_broadcast([128, 64, 16])
    P.op("dve", lambda e: e.tensor_tensor(bbr[:], Bre[:], freb, ALU.mult), reads=["Bre", "q_fre"], writes=["bbr"])
    P.op("dve", lambda e: e.tensor_tensor(bbi[:], Bim[:], fimb, ALU.mult), reads=["Bim", "q_fim"], writes=["bbi"])
    P.op("dve", lambda e: e.tensor_tensor(bbr[:], bbr[:], bbi[:], ALU.subtract), reads=["bbr", "bbi"], writes=["bbr"])
    P.op("dve", lambda e: e.tensor_tensor(bbi[:], Bre[:], fimb, ALU.mult), reads=["Bre", "q_fim", "bbi"], writes=["bbi"])
    P.op("dve", lambda e: e.tensor_tensor(Bre[:], Bim[:], freb, ALU.mult), reads=["Bim", "q_fre", "Bre"], writes=["Bre"])
    P.op("dve", lambda e: e.tensor_tensor(bbi[:], bbi[:], Bre[:], ALU.add), reads=["bbi", "Bre"], writes=["bbi"])

    d16 = P.sbuf("d16", [16, 128], F32)
    dT = P.sbuf("dT", [128, 16], F32)
    P.dma("sp", "p3", lambda e: e.dma_start(out=d16[:], in_=d_skip[0].rearrange("(k p) -> k p", p=128)), writes=["d16"])
    P.op("pe", lambda e: e.transpose(ptr[:, 3, 0:16], d16[:], ident[:16, :16]), reads=["d16", "ident"], writes=["sptr"])
    P.op("act", lambda e: e.copy(out=dT[:], in_=ptr[:, 3, 0:16]), reads=["sptr"], writes=["dT"])

    iot = P.sbuf("iot", [128, S], F32)
    P.op("pool", lambda e: e.iota(iot[:], [[1, S]], base=0, channel_multiplier=0, allow_small_or_imprecise_dtypes=True), writes=["iot"])

    XR = P.sbuf("XR", [128, 4, 128], F32)
    XI = P.sbuf("XI", [128, 4, 128], F32)
    BBr = P.sbuf("BBr", [128, 4, 128], F32)
    BBi = P.sbuf("BBi", [128, 4, 128], F32)
    CCr = P.sbuf("CCr", [128, 4, 128], F32)
    CCi = P.sbuf("CCi", [128, 4, 128], F32)
    for tname, tt_ in (("XR", XR), ("XI", XI), ("CCr", CCr), ("CCi", CCi)):
        P.op("pool", lambda e, tt_=tt_: e.memset(tt_[:], 0.0), writes=[tname])
    Cld = P.sbuf("Cld", [128, 2, 2, 64], F32)
    Td = P.sbuf("Td", [128, 2, 128], F32)
    hld = P.sbuf("hld", [128, 16, 128], F32)
    uT = P.sbuf("uT", [128, S], F32)
    yacc = P.sbuf("yacc", [128, S], F32)
    ygT = P.sbuf("ygT", [128, 16, S], BF16)
    cosT = P.sbuf("cosT", [128, S], F32)
    sinT = P.sbuf("sinT", [128, S], F32)
    tf_ = P.sbuf("tf_", [128, S], F32)
    gg = P.sbuf("gg", [128, S], F32)
    wre = P.sbuf("wre", [128, S], F32)
    wim = P.sbuf("wim", [128, S], F32)
    qre = P.sbuf("qre", [128, S], F32)
    qim = P.sbuf("qim", [128, S], F32)

    bi = 0; ci = 0
    for jk in range(16):
        for jj in range(4):
            j = jk * 4 + jj
            r0 = jj * 32
            P.op("dve", lambda e, j=j, jj=jj, r0=r0: e.tensor_copy(XR[0:64, jj, r0:r0 + 16], bbr[0:64, j, :]), reads=["bbr", "XR"], writes=["XR"])
            P.op("dve", lambda e, j=j, jj=jj, r0=r0: e.tensor_copy(XR[64:128, jj, r0 + 16:r0 + 32], bbr[64:128, j, :]), reads=["bbr", "XR"], writes=["XR"])
            P.op("dve", lambda e, j=j, jj=jj, r0=r0: e.tensor_copy(XI[0:64, jj, r0:r0 + 16], bbi[0:64, j, :]), reads=["bbi", "XI"], writes=["XI"])
            P.op("dve", lambda e, j=j, jj=jj, r0=r0: e.tensor_copy(XI[64:128, jj, r0 + 16:r0 + 32], bbi[64:128, j, :]), reads=["bbi", "XI"], writes=["XI"])
        for jj in range(4):
            P.op("pe", lambda e, jj=jj: e.transpose(ptr[:, jj, :], XR[:, jj, :], ident[:]), reads=["XR", "ident"], writes=["sptr"])
        P.op("act", lambda e: e.copy(out=BBr[:], in_=ptr[:]), reads=["sptr"], writes=["BBr"])
        for jj in range(4):
            P.op("pe", lambda e, jj=jj: e.transpose(ptr[:, jj, :], XI[:, jj, :], ident[:]), reads=["XI", "ident"], writes=["sptr"])
        P.op("act", lambda e: e.copy(out=BBi[:], in_=ptr[:]), reads=["sptr"], writes=["BBi"])
        crows = slice(jk * 128, (jk + 1) * 128)
        for dup in range(2):
            P.dma("sp", "cld", lambda e, dup=dup, crows=crows: e.dma_start(out=Cld[:, 0, dup, :], in_=c_re[0].rearrange("g c p -> (g c) p")[crows, :]), writes=["Cld"])
            P.dma("sp", "cld", lambda e, dup=dup, crows=crows: e.dma_start(out=Cld[:, 1, dup, :], in_=c_im[0].rearrange("g c p -> (g c) p")[crows, :]), writes=["Cld"])
        for ri in range(2):
            P.op("pe", lambda e, ri=ri: e.transpose(ptr[:, ri, :], Cld[:, ri, :, :].rearrange("p d q -> p (d q)"), ident[:]), reads=["Cld", "ident"], writes=["sptr"])
        P.op("act", lambda e: e.copy(out=Td[:], in_=ptr[:, 0:2, :]), reads=["sptr"], writes=["Td"])
        for jj in range(4):
            r0 = jj * 32
            P.op("dve", lambda e, jj=jj, r0=r0: e.tensor_copy(CCr[0:64, jj, r0:r0 + 16], Td[0:64, 0, r0:r0 + 16]), reads=["Td", "CCr"], writes=["CCr"])
            P.op("dve", lambda e, jj=jj, r0=r0: e.tensor_copy(CCr[64:128, jj, r0 + 16:r0 + 32], Td[64:128, 0, r0 + 16:r0 + 32]), reads=["Td", "CCr"], writes=["CCr"])
            P.op("dve", lambda e, jj=jj, r0=r0: e.tensor_scalar(CCi[0:64, jj, r0:r0 + 16], Td[0:64, 1, r0:r0 + 16], -1.0, None, ALU.mult), reads=["Td", "CCi"], writes=["CCi"])
            P.op("dve", lambda e, jj=jj, r0=r0: e.tensor_scalar(CCi[64:128, jj, r0 + 16:r0 + 32], Td[64:128, 1, r0 + 16:r0 + 32], -1.0, None, ALU.mult), reads=["Td", "CCi"], writes=["CCi"])
        P.dma("sp", "hld", lambda e, jk=jk: e.dma_start(out=hld[:], in_=hbuf[:, jk * 128:(jk + 1) * 128].rearrange("(t p) f -> p t f", p=128)), reads=["d_hbuf"], writes=["hld"])
        for qd in range(4):
            for i in range(4):
                t = qd * 4 + i
                P.op("pe", lambda e, t=t, i=i: e.transpose(ptr[:, i, :], hld[:, t, :], ident[:]), reads=["hld", "ident"], writes=["sptr"])
            P.op("act", lambda e, qd=qd: e.copy(out=uT[:, qd * 512:(qd + 1) * 512], in_=ptr[:].rearrange("p a b -> p (a b)")), reads=["sptr"], writes=["uT"])
        P.op("dve", lambda e, jk=jk: e.tensor_scalar(yacc[:], uT[:], dT[:, jk:jk + 1], None, ALU.mult), reads=["uT", "dT"], writes=["yacc"])
        for jj in range(4):
            j = jk * 4 + jj
            r0 = jj * 32
            P.op("dve", lambda e, j=j: e.tensor_scalar(qre[:], iot[:], thf[:, j:j + 1], None, ALU.mult), reads=["iot", "pp_u"], writes=["qre"])
            _wrap_sincos(P, None, S, "tt_", qim[:].bitcast(I32), tf_[:], gg[:], sinT[:], cosT[:], qre[:], u_name="qre", ti_name="qim")
            for n in range(4):
                b = bi % 2; bi += 1
                ns = slice(n * 512, (n + 1) * 512)
                P.op("pe", lambda e, b=b, jj=jj, r0=r0, ns=ns: e.matmul(pxr[b][:], BBr[min(r0, 64):r0 + 32, jj, :], uT[min(r0, 64):r0 + 32, ns], start=True, stop=True),
                     reads=["BBr", "uT"], writes=[f"pxr{b}"])
                P.op("pe", lambda e, b=b, jj=jj, r0=r0, ns=ns: e.matmul(pxi[b][:], BBi[min(r0, 64):r0 + 32, jj, :], uT[min(r0, 64):r0 + 32, ns], start=True, stop=True),
                     reads=["BBi", "uT"], writes=[f"pxi{b}"])
                P.op("dve", lambda e, b=b, ns=ns: e.tensor_tensor(wre[:, ns], pxr[b][:], cosT[:, ns], ALU.mult), reads=[f"pxr{b}", "tt_cos"], writes=["wre"])
                P.op("dve", lambda e, b=b, ns=ns: e.tensor_tensor(gg[:, ns], pxi[b][:], sinT[:, ns], ALU.mult), reads=[f"pxi{b}", "tt_sin", "tt_g"], writes=["tt_g"])
                P.op("dve", lambda e, ns=ns: e.tensor_tensor(wre[:, ns], wre[:, ns], gg[:, ns], ALU.add), reads=["wre", "tt_g"], writes=["wre"])
                P.op("dve", lambda e, b=b, ns=ns: e.tensor_tensor(wim[:, ns], pxi[b][:], cosT[:, ns], ALU.mult), reads=[f"pxi{b}", "tt_cos"], writes=["wim"])
                P.op("dve", lambda e, b=b, ns=ns: e.tensor_tensor(gg[:, ns], pxr[b][:], sinT[:, ns], ALU.mult), reads=[f"pxr{b}", "tt_sin", "tt_g"], writes=["tt_g"])
                P.op("dve", lambda e, ns=ns: e.tensor_tensor(wim[:, ns], wim[:, ns], gg[:, ns], ALU.subtract), reads=["wim", "tt_g"], writes=["wim"])
            rb = rr[:, j:j + 1].to_broadcast([128, S])
            P.op("dve", lambda e, rb=rb: e.tensor_tensor_scan(qre[:], rb, wre[:], 0.0, ALU.mult, ALU.add), reads=["wre", "q_r"], writes=["qre"])
            P.op("dve", lambda e, rb=rb: e.tensor_tensor_scan(qim[:], rb, wim[:], 0.0, ALU.mult, ALU.add), reads=["wim", "q_r"], writes=["qim"])
            P.op("dve", lambda e: e.tensor_tensor(wre[:], qre[:], cosT[:], ALU.mult), reads=["qre", "tt_cos", "wre"], writes=["wre"])
            P.op("dve", lambda e: e.tensor_tensor(gg[:], qim[:], sinT[:], ALU.mult), reads=["qim", "tt_sin", "tt_g"], writes=["tt_g"])
            P.op("dve", lambda e: e.tensor_tensor(wre[:], wre[:], gg[:], ALU.subtract), reads=["wre", "tt_g"], writes=["wre"])
            P.op("dve", lambda e: e.tensor_tensor(wim[:], qre[:], sinT[:], ALU.mult), reads=["qre", "tt_sin", "wim"], writes=["wim"])
            P.op("dve", lambda e: e.tensor_tensor(gg[:], qim[:], cosT[:], ALU.mult), reads=["qim", "tt_cos", "tt_g"], writes=["tt_g"])
            P.op("dve", lambda e: e.tensor_tensor(wim[:], wim[:], gg[:], ALU.add), reads=["wim", "tt_g"], writes=["wim"])
            for n in range(4):
                c = ci % 2; ci += 1
                ns = slice(n * 512, (n + 1) * 512)
                P.op("pe", lambda e, c=c, jj=jj, ns=ns: e.matmul(pyc[c][:], CCr[:, jj, :], wre[:, ns], start=True, stop=False), reads=["CCr", "wre"], writes=[f"pyc{c}"])
                P.op("pe", lambda e, c=c, jj=jj, ns=ns: e.matmul(pyc[c][:], CCi[:, jj, :], wim[:, ns], start=False, stop=True), reads=["CCi", "wim"], writes=[f"pyc{c}"])
                P.op("dve", lambda e, c=c, ns=ns: e.tensor_tensor(yacc[:, ns], yacc[:, ns], pyc[c][:], ALU.add), reads=["yacc", f"pyc{c}"], writes=["yacc"])
        if debug_y is not None:
            P.dma("sp", "dbg", lambda e, jk=jk: e.dma_start(out=debug_y[jk * 128:(jk + 1) * 128, :], in_=yacc[:]), reads=["yacc"], writes=["d_dbg"])
        P.op("dve", lambda e: e.tensor_tensor(gg[:], yacc[:], yacc[:], ALU.mult), reads=["yacc", "tt_g"], writes=["tt_g"])
        P.op("dve", lambda e: e.tensor_scalar(gg[:], gg[:], 0.044715, 1.0, ALU.mult, ALU.add), reads=["tt_g"], writes=["tt_g"])
        P.op("dve", lambda e: e.tensor_tensor(gg[:], gg[:], yacc[:], ALU.mult), reads=["tt_g", "yacc"], writes=["tt_g"])
        P.op("act", lambda e: e.activation(out=gg[:], in_=gg[:], func=AF.Tanh, scale=0.7978845608028654), reads=["tt_g"], writes=["tt_g"])
        P.op("dve", lambda e: e.tensor_scalar(gg[:], gg[:], 1.0, 0.5, ALU.add, ALU.mult), reads=["tt_g"], writes=["tt_g"])
        P.op("dve", lambda e, jk=jk: e.tensor_tensor(ygT[:, jk, :], gg[:], yacc[:], ALU.mult), reads=["tt_g", "yacc"], writes=["ygT"])

    P.barrier_all()
    G1 = wre; ba_t = wim; bb_t = gg; xcol = [cosT, sinT]; ocol = [qre, qim]
    P.dma("sp", "g0", lambda e: e.dma_start(out=G1[:], in_=modrow[1, 2 * D:3 * D].partition_broadcast(128)), reads=["d_modrow", "wre"], writes=["wre"])
    P.dma("sp", "g1", lambda e: e.dma_start(out=ba_t[:], in_=b_a[0, :].partition_broadcast(128)), reads=["wim"], writes=["wim"])
    P.dma("sp", "g2", lambda e: e.dma_start(out=bb_t[:], in_=b_b[0, :].partition_broadcast(128)), reads=["tt_g"], writes=["tt_g"])
    wa = [tf_[:].bitcast(BF16).rearrange("p (k n) -> p k n", k=16)]
    wb = [iot[:].bitcast(BF16).rearrange("p (k n) -> p k n", k=16)]
    gi = 0
    CW = 256
    for cb in range(D // CW):
        cs_ = slice(cb * CW, (cb + 1) * CW)
        P.dma("pool", "wa0", lambda e, cs_=cs_: e.dma_start(out=wa[0], in_=w_a[0, :, cs_].rearrange("(k p) n -> p k n", p=128)), writes=["wa0"])
        P.dma("pool", "wb0", lambda e, cs_=cs_: e.dma_start(out=wb[0], in_=w_b[0, :, cs_].rearrange("(k p) n -> p k n", p=128)), writes=["wb0"])
        for t in range(16):
            b = gi % 2; gi += 1
            rows = slice(t * 128, (t + 1) * 128)
            xc = xcol[b]; oc = ocol[b]
            P.dma("sp", f"gx{b}", lambda e, rows=rows, cs_=cs_, xc=xc: e.dma_start(out=xc[:, 0:CW], in_=xin[rows, cs_]), reads=[f"xc{b}"], writes=[f"xc{b}"])
            for k in range(16):
                P.op("pe", lambda e, b=b, k=k, rows=rows: e.matmul(pxr[b][:, 0:CW], ygT[:, k, rows], wa[0][:, k, :], start=(k == 0), stop=(k == 15)), reads=["ygT", "wa0"], writes=[f"pxr{b}"])
            for k in range(16):
                P.op("pe", lambda e, b=b, k=k, rows=rows: e.matmul(pxi[b][:, 0:CW], ygT[:, k, rows], wb[0][:, k, :], start=(k == 0), stop=(k == 15)), reads=["ygT", "wb0"], writes=[f"pxi{b}"])
            P.op("dve", lambda e, b=b, cs_=cs_, oc=oc: e.tensor_tensor(oc[:, 512:512 + CW], pxi[b][:, 0:CW], bb_t[:, cs_], ALU.add), reads=[f"pxi{b}", "tt_g"], writes=[f"oc{b}"])
            P.op("act", lambda e, oc=oc, b=b: e.activation(out=oc[:, 512:512 + CW], in_=oc[:, 512:512 + CW], func=AF.Sigmoid), reads=[f"oc{b}"], writes=[f"oc{b}"])
            P.op("dve", lambda e, b=b, cs_=cs_, oc=oc: e.tensor_tensor(oc[:, 0:CW], pxr[b][:, 0:CW], ba_t[:, cs_], ALU.add), reads=[f"pxr{b}", "wim", f"oc{b}"], writes=[f"oc{b}"])
            P.op("dve", lambda e, oc=oc, b=b: e.tensor_tensor(oc[:, 0:CW], oc[:, 0:CW], oc[:, 512:512 + CW], ALU.mult), reads=[f"oc{b}"], writes=[f"oc{b}"])
            P.op("dve", lambda e, oc=oc, b=b, cs_=cs_: e.tensor_tensor(oc[:, 0:CW], oc[:, 0:CW], G1[:, cs_], ALU.mult), reads=[f"oc{b}", "wre"], writes=[f"oc{b}"])
            P.op("dve", lambda e, oc=oc, xc=xc, b=b: e.tensor_tensor(oc[:, 0:CW], oc[:, 0:CW], xc[:, 0:CW], ALU.add), reads=[f"oc{b}", f"xc{b}"], writes=[f"oc{b}"])
            P.dma("sp", f"go{b}", lambda e, rows=rows, cs_=cs_, oc=oc: e.dma_start(out=xout[rows, cs_], in_=oc[:, 0:CW]), reads=[f"oc{b}"], writes=["d_xout_ssm"])


_REP = ["norm_gain", "ada_w", "ada_b", "attn_w_qkv", "attn_b_qkv", "attn_q_gain", "attn_k_gain", "attn_sinks",
        "attn_w_o", "attn_b_o", "ssm_lam_re", "ssm_lam_im", "ssm_log_dt", "ssm_b_re", "ssm_b_im", "ssm_c_re",
        "ssm_c_im", "ssm_d", "ssm_w_glu_a", "ssm_b_glu_a", "ssm_w_glu_b", "ssm_b_glu_b", "moe_w_router",
        "moe_b_router", "moe_w_gate_up", "moe_b_gate_up", "moe_w_down", "moe_b_down"]
_SHAPES = {"norm_gain": [2, 2, D], "ada_w": [2, D, 6 * D], "ada_b": [2, 6 * D], "attn_w_qkv": [1, D, QKV],
           "attn_b_qkv": [1, QKV], "attn_q_gain": [1, 64], "attn_k_gain": [1, 64], "attn_sinks": [1, 32],
           "attn_w_o": [1, D, D], "attn_b_o": [1, D], "ssm_lam_re": [1, 128, 64], "ssm_lam_im": [1, 128, 64],
           "ssm_log_dt": [1, 128], "ssm_b_re": [1, 128, 64, 16], "ssm_b_im": [1, 128, 64, 16],
           "ssm_c_re": [1, 128, 16, 64], "ssm_c_im": [1, 128, 16, 64], "ssm_d": [1, D], "ssm_w_glu_a": [1, D, D],
           "ssm_b_glu_a": [1, D], "ssm_w_glu_b": [1, D, D], "ssm_b_glu_b": [1, D], "moe_w_router": [2, D, 32],
           "moe_b_router": [2, 32], "moe_w_gate_up": [2, 32, D, 2 * D], "moe_b_gate_up": [2, 32, 2 * D],
           "moe_w_down": [2, 32, D, D], "moe_b_down": [2, 32, D]}


def build_full():
    nc = bass.Bass("TRN2", target_bir_lowering=False)
    dt = lambda name, shape, kind="ExternalInput": nc.dram_tensor(name, list(shape), F32, kind=kind).ap()
    x = dt("x", [S, D]); c = dt("c", [D]); biasmask = dt("biasmask", [32, 128, 256])
    a = {n: dt(n, _SHAPES[n]) for n in _REP}
    out = dt("out", [S, D], kind="ExternalOutput")
    scr = lambda name, shape: nc.dram_tensor(name, list(shape), F32, kind="Internal").ap()
    modrow = scr("modrow", [2, 6 * D]); xa0 = scr("xa0", [S, D]); xb0 = scr("xb0", [S, D]); xa1 = scr("xa1", [S, D]); hbuf = scr("hbuf", [S, D])
    P = Prog(nc)
    P.ident = P.sbuf("ident", [128, 128], F32)
    P.op("pool", lambda e: e.memset(P.ident[:], 1.0), writes=["ident"])
    P.op("pool", lambda e: e.affine_select(P.ident[:], P.ident[:], [[-1, 128]], ALU.is_equal, 0.0, base=0, channel_multiplier=1), reads=["ident"], writes=["ident"])
    P.barrier_all()
    m0 = P.mark()
    emit_adaln(P, nc, c, a["ada_w"], a["ada_b"], modrow)
    P.barrier_all(); P.release(m0)
    emit_attn(P, nc, x, xa0, modrow, a["norm_gain"], a["attn_w_qkv"], a["attn_b_qkv"], a["attn_q_gain"], a["attn_k_gain"],
              a["attn_sinks"], a["attn_w_o"], a["attn_b_o"], biasmask)
    P.barrier_all(); P.release(m0)
    P.prefix = 'm0_'
    emit_moe(P, nc, 0, xa0, xb0, modrow, a["norm_gain"], a["moe_w_router"], a["moe_b_router"], a["moe_w_gate_up"],
             a["moe_b_gate_up"], a["moe_w_down"], a["moe_b_down"])
    P.barrier_all(); P.release(m0)
    P.prefix = 's_'
    emit_ssm(P, nc, xb0, xa1, hbuf, modrow, a["norm_gain"], a["ssm_lam_re"], a["ssm_lam_im"], a["ssm_log_dt"], a["ssm_b_re"],
             a["ssm_b_im"], a["ssm_c_re"], a["ssm_c_im"], a["ssm_d"], a["ssm_w_glu_a"], a["ssm_b_glu_a"], a["ssm_w_glu_b"], a["ssm_b_glu_b"])
    P.barrier_all(); P.release(m0)
    P.prefix = 'm1_'
    emit_moe(P, nc, 1, xa1, out, modrow, a["norm_gain"], a["moe_w_router"], a["moe_b_router"], a["moe_w_gate_up"],
             a["moe_b_gate_up"], a["moe_w_down"], a["moe_b_down"])
    P.barrier_all()
    P.emit()
    P.close()
    return nc


def kernel(**inputs):
    n_cores = int(inputs.pop("_n_cores", 8)) if "_n_cores" in inputs else 8
    x = np.asarray(inputs["x"], dtype=np.float32)
    c = np.asarray(inputs["c"], dtype=np.float32)
    base = {n: np.ascontiguousarray(np.asarray(inputs[n], dtype=np.float32)) for n in _REP}
    base["biasmask"] = make_biasmask(np.asarray(inputs["rel_bias"], dtype=np.float32))
    nc = build_full()
    in_maps = [dict(base, x=np.ascontiguousarray(x[b]), c=np.ascontiguousarray(c[b])) for b in range(n_cores)]
    res = run_bass_kernel_spmd(nc, in_maps, core_ids=list(range(n_cores)))
    return np.stack([res.results[b]["out"] for b in range(n_cores)], axis=0).astype(np.float32)
```
